# Optimizing a Trainium2 kernel written in Bass

```python
import math
import jax, jax.numpy as jnp
from jax import lax
import numpy as np

D_MODEL = 1024
BATCH = 16
SEQ = 2048
DEPTH = 1

PLE_DIM = 256
D_MIX = D_MODEL
D_SSM = D_MIX // 2
SSM_GROUP = 16
N_SSM_GROUPS = D_SSM // SSM_GROUP
SSM_STATE = 64
D_SGU = D_MIX - D_SSM
SGU_CHUNK = 128
N_SGU_HEADS = 4
SGU_HEAD_DIM = D_SGU // N_SGU_HEADS
D_IN = D_SSM + 2 * D_SGU
PEER_HEADS = 8
PEER_NKEYS = 128
PEER_NEXPERTS = PEER_NKEYS * PEER_NKEYS
PEER_TOPK = 16
PEER_HALF = 128
PEER_QDIM = 2 * PEER_HALF
PEER_BLOCK = 128
EPS = 1e-6

kernel_name = "hymba_s5_sgu_peer_block"


def _gelu(x):
    return jax.nn.gelu(x, approximate=False)


def _rmsnorm(x, g):
    x32 = x.astype(jnp.float32)
    y = x32 * lax.rsqrt(jnp.mean(x32 * x32, axis=-1, keepdims=True) + EPS)
    return (y * g.astype(jnp.float32)).astype(x.dtype)


def _s5(xs, a_re, a_im, log_dt, b_re, b_im, c_re, c_im, d):
    bsz, s, _ = xs.shape
    f32 = jnp.float32
    u = xs.astype(f32).reshape(bsz, s, N_SSM_GROUPS, SSM_GROUP)
    lam_re, lam_im = a_re.astype(f32), a_im.astype(f32)
    dt = jnp.exp(log_dt.astype(f32))[:, None]
    mag = jnp.exp(lam_re * dt)
    ang = lam_im * dt
    abar_re, abar_im = mag * jnp.cos(ang), mag * jnp.sin(ang)
    nr, ni = abar_re - 1.0, abar_im
    den = lam_re * lam_re + lam_im * lam_im
    coef_re = (nr * lam_re + ni * lam_im) / den
    coef_im = (ni * lam_re - nr * lam_im) / den
    br, bi = b_re.astype(f32), b_im.astype(f32)
    bbar_re = coef_re[..., None] * br - coef_im[..., None] * bi
    bbar_im = coef_re[..., None] * bi + coef_im[..., None] * br
    bu_re = jnp.einsum('bsgc,gpc->sbgp', u, bbar_re)
    bu_im = jnp.einsum('bsgc,gpc->sbgp', u, bbar_im)
    a_s_re = jnp.broadcast_to(abar_re[None, None], (s, 1, N_SSM_GROUPS, SSM_STATE))
    a_s_im = jnp.broadcast_to(abar_im[None, None], (s, 1, N_SSM_GROUPS, SSM_STATE))

    def combine(e1, e2):
        a1r, a1i, b1r, b1i = e1
        a2r, a2i, b2r, b2i = e2
        ar = a1r * a2r - a1i * a2i
        ai = a1r * a2i + a1i * a2r
        bo_r = a2r * b1r - a2i * b1i + b2r
        bo_i = a2r * b1i + a2i * b1r + b2i
        return ar, ai, bo_r, bo_i

    _, _, h_re, h_im = lax.associative_scan(combine, (a_s_re, a_s_im, bu_re, bu_im), axis=0)
    y = (jnp.einsum('sbgp,gcp->bsgc', h_re, c_re.astype(f32))
         - jnp.einsum('sbgp,gcp->bsgc', h_im, c_im.astype(f32)))
    y = y + d.astype(f32).reshape(N_SSM_GROUPS, SSM_GROUP) * u
    return y.reshape(bsz, s, D_SSM).astype(xs.dtype)


def _sgu(u, v, ln_g, ln_b, ws, bs):
    bsz, s, _ = u.shape
    nc = s // SGU_CHUNK
    u = _gelu(u)
    v = _gelu(v)
    v32 = v.astype(jnp.float32).reshape(bsz, s, N_SGU_HEADS, SGU_HEAD_DIM)
    mu = jnp.mean(v32, axis=-1, keepdims=True)
    var = jnp.mean(jnp.square(v32 - mu), axis=-1, keepdims=True)
    vn = ((v32 - mu) * lax.rsqrt(var + EPS)
          * ln_g.astype(jnp.float32).reshape(N_SGU_HEADS, SGU_HEAD_DIM)
          + ln_b.astype(jnp.float32).reshape(N_SGU_HEADS, SGU_HEAD_DIM)).astype(u.dtype)
    vc = vn.reshape(bsz, nc, SGU_CHUNK, N_SGU_HEADS, SGU_HEAD_DIM)
    causal = jnp.tril(jnp.ones((SGU_CHUNK, SGU_CHUNK), dtype=bool))
    wsm = jnp.where(causal[None], ws, jnp.zeros_like(ws))
    mixed = jnp.einsum('hts,bcshd->bcthd', wsm, vc) + bs.T[None, None, :, :, None]
    return u * mixed.reshape(bsz, s, D_SGU).astype(u.dtype)


def _peer(xn, wq, keys, u_tab, v_tab):
    bsz, s, d = xn.shape
    t = bsz * s
    xt = xn.reshape(t, d)
    q = (xt @ wq).reshape(t, PEER_HEADS, 2, PEER_HALF)
    sc = jnp.einsum('thkd,hknd->thkn', q, keys).astype(jnp.float32)
    s1, i1 = lax.top_k(sc[:, :, 0], PEER_TOPK)
    s2, i2 = lax.top_k(sc[:, :, 1], PEER_TOPK)
    cand = (s1[..., :, None] + s2[..., None, :]).reshape(t, PEER_HEADS, PEER_TOPK * PEER_TOPK)
    top_s, ci = lax.top_k(cand, PEER_TOPK)
    ia, ib = ci // PEER_TOPK, ci % PEER_TOPK
    experts = (jnp.take_along_axis(i1, ia, axis=-1) * PEER_NKEYS
               + jnp.take_along_axis(i2, ib, axis=-1))
    gates = jax.nn.softmax(top_s, axis=-1).astype(xn.dtype)
    nb = t // PEER_BLOCK

    def block(args):
        xb, eb, gb = args
        ub = u_tab[eb]
        act = _gelu(jnp.einsum('td,thkd->thk', xb, ub)) * gb
        vb = v_tab[eb]
        return jnp.einsum('thk,thkd->td', act, vb)

    y = lax.map(block, (xt.reshape(nb, PEER_BLOCK, d),
                        experts.reshape(nb, PEER_BLOCK, PEER_HEADS, PEER_TOPK),
                        gates.reshape(nb, PEER_BLOCK, PEER_HEADS, PEER_TOPK)))
    return y.reshape(bsz, s, d)


def setup_inputs(seed: int = 0) -> dict:
    key = jax.random.key(seed)
    ks = jax.random.split(key, 32)
    n = jax.random.normal
    L, D = DEPTH, D_MODEL
    G, P, C = N_SSM_GROUPS, SSM_STATE, SSM_GROUP
    a_im = jnp.broadcast_to(jnp.pi * jnp.arange(P, dtype=jnp.float32), (L, G, P))
    return {
        "x": n(ks[0], (BATCH, SEQ, D), jnp.float32),
        "p": n(ks[1], (DEPTH, BATCH, SEQ, PLE_DIM), jnp.float32),
        "ln_mix_g": 1.0 + 0.02 * n(ks[2], (L, D), jnp.float32),
        "w_in": n(ks[3], (L, D, D_IN), jnp.float32) * D ** -0.5,
        "s5_a_re": -0.5 + 0.01 * n(ks[4], (L, G, P), jnp.float32),
        "s5_a_im": a_im + 0.01 * n(ks[5], (L, G, P), jnp.float32),
        "s5_log_dt": jax.random.uniform(ks[6], (L, G), jnp.float32, math.log(1e-3), math.log(1e-1)),
        "s5_b_re": n(ks[7], (L, G, P, C), jnp.float32) * (2 * C) ** -0.5,
        "s5_b_im": n(ks[8], (L, G, P, C), jnp.float32) * (2 * C) ** -0.5,
        "s5_c_re": n(ks[9], (L, G, C, P), jnp.float32) * (2 * P) ** -0.5,
        "s5_c_im": n(ks[10], (L, G, C, P), jnp.float32) * (2 * P) ** -0.5,
        "s5_d": n(ks[11], (L, D_SSM), jnp.float32),
        "glu_w": n(ks[12], (L, D_SSM, D_SSM), jnp.float32) * D_SSM ** -0.5,
        "glu_b": 0.02 * n(ks[13], (L, D_SSM), jnp.float32),
        "sgu_ln_g": 1.0 + 0.02 * n(ks[14], (L, D_SGU), jnp.float32),
        "sgu_ln_b": 0.02 * n(ks[15], (L, D_SGU), jnp.float32),
        "sgu_ws": n(ks[16], (L, N_SGU_HEADS, SGU_CHUNK, SGU_CHUNK), jnp.float32) * SGU_CHUNK ** -0.5,
        "sgu_b": 1.0 + 0.02 * n(ks[17], (L, N_SGU_HEADS, SGU_CHUNK), jnp.float32),
        "out_norm_ssm": 1.0 + 0.02 * n(ks[18], (L, D_SSM), jnp.float32),
        "out_norm_sgu": 1.0 + 0.02 * n(ks[19], (L, D_SGU), jnp.float32),
        "w_out": n(ks[20], (L, D_MIX, D), jnp.float32) * D_MIX ** -0.5,
        "ln_ffn_g": 1.0 + 0.02 * n(ks[21], (L, D), jnp.float32),
        "peer_wq": n(ks[22], (L, D, PEER_HEADS * PEER_QDIM), jnp.float32) * D ** -0.5,
        "peer_keys": n(ks[23], (L, PEER_HEADS, 2, PEER_NKEYS, PEER_HALF), jnp.float32) * PEER_HALF ** -0.5,
        "peer_u": n(ks[24], (L, PEER_NEXPERTS, D), jnp.float32) * D ** -0.5,
        "peer_v": n(ks[25], (L, PEER_NEXPERTS, D), jnp.float32) * PEER_HEADS ** -0.5,
        "ple_norm_g": 1.0 + 0.02 * n(ks[26], (L, D), jnp.float32),
        "ple_w_gate": n(ks[27], (L, D, D), jnp.float32) * D ** -0.5,
        "ple_w_proj": n(ks[28], (L, PLE_DIM, D), jnp.float32) * PLE_DIM ** -0.5,
        "final_g": 1.0 + 0.02 * n(ks[29], (D,), jnp.float32),
    }


def reference(x, p, ln_mix_g, w_in, s5_a_re, s5_a_im, s5_log_dt, s5_b_re, s5_b_im,
              s5_c_re, s5_c_im, s5_d, glu_w, glu_b, sgu_ln_g, sgu_ln_b, sgu_ws, sgu_b,
              out_norm_ssm, out_norm_sgu, w_out, ln_ffn_g, peer_wq, peer_keys, peer_u,
              peer_v, ple_norm_g, ple_w_gate, ple_w_proj, final_g):
    h = x
    for i in range(DEPTH):
        xn = _rmsnorm(h, ln_mix_g[i])
        z = xn @ w_in[i]
        xs = z[..., :D_SSM]
        u = z[..., D_SSM:D_SSM + D_SGU]
        v = z[..., D_SSM + D_SGU:]
        ys = _s5(xs, s5_a_re[i], s5_a_im[i], s5_log_dt[i], s5_b_re[i], s5_b_im[i],
                 s5_c_re[i], s5_c_im[i], s5_d[i])
        ys = _gelu(ys)
        ys = ys * jax.nn.sigmoid(ys @ glu_w[i] + glu_b[i])
        yg = _sgu(u, v, sgu_ln_g[i], sgu_ln_b[i], sgu_ws[i], sgu_b[i])
        mix = jnp.concatenate([_rmsnorm(ys, out_norm_ssm[i]),
                               _rmsnorm(yg, out_norm_sgu[i])], axis=-1)
        h = h + mix @ w_out[i]
        hn = _rmsnorm(h, ln_ffn_g[i])
        h = h + _peer(hn, peer_wq[i], peer_keys[i], peer_u[i], peer_v[i])
        gate = jax.nn.sigmoid(_rmsnorm(h, ple_norm_g[i]) @ ple_w_gate[i])
        h = h + gate * (p[i] @ ple_w_proj[i])
    return _rmsnorm(h, final_g)
```

```python
import math
import numpy as np
import concourse.bass as bass
import concourse.mybir as mybir
from concourse.bass_utils import run_bass_kernel_spmd

F32 = mybir.dt.float32
BF16 = mybir.dt.bfloat16
U32 = mybir.dt.uint32
I32 = mybir.dt.int32
AF = mybir.ActivationFunctionType
ALU = mybir.AluOpType
AX = mybir.AxisListType

NCORES = 8
D = 1024
SEQ = 2048
NB = 2
T = NB * SEQ
NT = T // 128
EPS = 1e-6
TWO_PI = 2.0 * math.pi


class Buf:
    __slots__ = ("name", "lw", "rd")

    def __init__(self, name=""):
        self.name = name
        self.lw = None
        self.rd = []


class Op:
    __slots__ = ("eng", "fn", "deps", "dma", "sem", "val", "sig", "sigidx", "prev_same_sem")

    def __init__(self, eng, fn, dma):
        self.eng = eng
        self.fn = fn
        self.deps = []
        self.dma = dma
        self.sem = None
        self.val = 0
        self.sig = False
        self.sigidx = 0
        self.prev_same_sem = 0


class Sched:
    ENGS = ("pe", "act", "dve", "pool", "sp")
    NDMA = 12
    NOSYNC = ("pe",)

    def __init__(self, nc, stack):
        self.nc = nc
        self.ops = []
        self.esem = {e: stack.enter_context(nc.semaphore("es_" + e)) for e in ("pe", "act", "dve", "pool")}
        self.ecount = {e: 0 for e in self.esem}
        self.dsem = {q: [stack.enter_context(nc.semaphore("ds_%s%d" % (q, i))) for i in range(self.NDMA)]
                     for q in ("sp", "act", "pool")}
        self.dcount = {q: 0 for q in self.dsem}
        self.waited = {e: {} for e in self.ENGS}
        self.phase = 0
        self.bufs = []

    def buf(self, name=""):
        b = Buf(name)
        self.bufs.append(b)
        return b

    def bufs_n(self, n, name=""):
        return [self.buf(name + str(i)) for i in range(n)]

    def _track(self, op, reads, writes):
        deps = []
        for b in reads:
            if b.lw is not None:
                deps.append(b.lw)
        for b in writes:
            if b.lw is not None:
                deps.append(b.lw)
            deps.extend(b.rd)
        for b in reads:
            b.rd.append(op)
        for b in writes:
            b.lw = op
            b.rd = []
        seen = set()
        for d in deps:
            if d is op or id(d) in seen:
                continue
            seen.add(id(d))
            if (not d.dma) and (not op.dma) and d.eng == op.eng and d.eng in self.NOSYNC:
                continue
            op.deps.append(d)
            d.sig = True

    def add(self, eng, fn, reads=(), writes=()):
        op = Op(eng, fn, False)
        self._track(op, reads, writes)
        self.ops.append(op)
        return op

    def dma(self, q, out, in_, reads=(), writes=(), **kw):
        op = Op(q, (lambda e, out=out, in_=in_, kw=kw: e.dma_start(out=out, in_=in_, **kw)), True)
        self._track(op, reads, writes)
        self.ops.append(op)
        return op

    def emit(self, name=None):
        nc = self.nc
        for op in self.ops:
            if op.dma:
                j = self.dcount[op.eng]
                self.dcount[op.eng] = j + 1
                op.sem = self.dsem[op.eng][j % self.NDMA]
                op.val = 16 * (j // self.NDMA + 1)
                op.prev_same_sem = 16 * (j // self.NDMA)
            elif op.sig:
                self.ecount[op.eng] += 1
                op.sem = self.esem[op.eng]
                op.val = self.ecount[op.eng]
        per = {e: [o for o in self.ops if o.eng == e] for e in self.ENGS}
        waited = self.waited
        dsem = self.dsem
        dcount = dict(self.dcount)
        NDMA = self.NDMA

        def run(e, ename):
            w = waited[ename]

            def wait(sem, val):
                k = sem.num
                if w.get(k, 0) >= val:
                    return
                w[k] = val
                e.wait_ge(sem, val)

            for op in per[ename]:
                for d in op.deps:
                    wait(d.sem, d.val)
                if op.dma:
                    if op.prev_same_sem:
                        wait(op.sem, op.prev_same_sem)
                    op.fn(e).then_inc(op.sem, 16)
                else:
                    ins = op.fn(e)
                    if op.sig:
                        ins.then_inc(op.sem, 1)
            if ename in dsem:
                n = dcount[ename]
                for i in range(NDMA):
                    cnt = (n - i + NDMA - 1) // NDMA if n > i else 0
                    if cnt:
                        wait(dsem[ename][i], 16 * cnt)

        with nc.Block() as block:
            @block.tensor
            def _(e):
                run(e, "pe")

            @block.scalar
            def _(e):
                run(e, "act")

            @block.vector
            def _(e):
                run(e, "dve")

            @block.gpsimd
            def _(e):
                run(e, "pool")

            @block.sync
            def _(e):
                run(e, "sp")
        self.ops = []
        self.phase += 1
        for b in self.bufs:
            b.lw = None
            b.rd = []


def _bc(ap, shape):
    return ap.to_broadcast(list(shape))


class K:
    def __init__(self, dbg=None):
        from contextlib import ExitStack
        self.dbg = dbg
        self.nc = nc = bass.Bass("TRN2", target_bir_lowering=False)
        self.root = ExitStack()
        self.S = Sched(nc, self.root)
        self.din = {}
        self.dq = 0

    def inp(self, name, shape):
        t = self.nc.dram_tensor(name, list(shape), F32, kind="ExternalInput").ap()
        self.din[name] = t
        return t

    def sb(self, stack, name, shape, dt=F32):
        return stack.enter_context(self.nc.sbuf_tensor(name, list(shape), dt))

    def ps(self, stack, name, shape, dt=F32):
        return stack.enter_context(self.nc.psum_tensor(name, list(shape), dt))

    def q(self):
        self.dq ^= 1
        return "sp" if self.dq else "act"

    def act(self, out, in_, func, reads, writes, **kw):
        return self.S.add("act", lambda e: e.activation(out=out, in_=in_, func=func, **kw), reads, writes)

    def mm(self, out, lhsT, rhs, start, stop, reads, writes, **kw):
        return self.S.add("pe", lambda e: e.matmul(out, lhsT, rhs, start=start, stop=stop, **kw), reads, writes)

    def tr(self, out, in_, ident, reads, writes):
        return self.S.add("pe", lambda e: e.transpose(out, in_, ident), reads, writes)

    def dve(self, fn, reads, writes):
        return self.S.add("dve", fn, reads, writes)

    def pool(self, fn, reads, writes):
        return self.S.add("pool", fn, reads, writes)

    def rstd(self, rstd, ss, scale, bufs_r, bufs_w):
        S = self.S
        self.dve(lambda e: e.tensor_scalar(out=rstd, in0=ss, scalar1=scale, scalar2=EPS, op0=ALU.mult, op1=ALU.add),
                 bufs_r, bufs_w)
        self.act(rstd, rstd, AF.Sqrt, bufs_w, bufs_w)
        self.dve(lambda e: e.reciprocal(out=rstd, in_=rstd), bufs_w, bufs_w)

    def load_weight(self, dst, dram2d, nk, n, gain, stage, stage_bufs, dst_buf, gain_buf):
        for kc in range(nk):
            st = stage[kc % 2]
            sbuf = stage_bufs[kc % 2]
            self.S.dma(self.q(), st[:, 0:n], dram2d[kc * 128:(kc + 1) * 128, :], [], [sbuf])
            if gain is None:
                self.dve(lambda e, st=st, kc=kc: e.tensor_copy(out=dst[:, kc, :], in_=st[:, 0:n]), [sbuf], [dst_buf])
            else:
                self.dve(lambda e, st=st, kc=kc: e.tensor_scalar(out=dst[:, kc, :], in0=st[:, 0:n],
                                                                 scalar1=gain[:, kc:kc + 1], scalar2=None,
                                                                 op0=ALU.mult),
                         [sbuf, gain_buf], [dst_buf])

    def sincos(self, ang, sin_o, cos_o, tmp, B):
        MAGIC = 12582912.0
        C1 = 6.28125
        C2 = TWO_PI - 6.28125
        d = self.dve
        d(lambda e: e.tensor_scalar(out=tmp, in0=ang, scalar1=1.0 / TWO_PI, scalar2=MAGIC, op0=ALU.mult, op1=ALU.add), B, B)
        d(lambda e: e.tensor_scalar(out=tmp, in0=tmp, scalar1=MAGIC, scalar2=None, op0=ALU.subtract), B, B)
        d(lambda e: e.scalar_tensor_tensor(out=ang, in0=tmp, scalar=-C1, in1=ang, op0=ALU.mult, op1=ALU.add), B, B)
        d(lambda e: e.scalar_tensor_tensor(out=ang, in0=tmp, scalar=-C2, in1=ang, op0=ALU.mult, op1=ALU.add), B, B)
        d(lambda e: e.tensor_scalar(out=ang, in0=ang, scalar1=math.pi, scalar2=-math.pi, op0=ALU.min, op1=ALU.max), B, B)
        self.act(sin_o, ang, AF.Sin, B, B)
        d(lambda e: e.scalar_tensor_tensor(out=tmp, in0=ang, scalar=-1.0, in1=ang, op0=ALU.mult, op1=ALU.max), B, B)
        self.act(cos_o, tmp, AF.Sin, B, B, scale=-1.0, bias=self.halfpi[:, 0:1])

    def build(self):
        from contextlib import ExitStack
        nc, S = self.nc, self.S
        inp = self.inp
        x = inp("x", [T, D])
        p_in = inp("p", [T, 256])
        ln_mix_g = inp("ln_mix_g", [1, D])
        w_in = inp("w_in", [1, D, 1536])
        a_re = inp("s5_a_re", [1, 32, 64]); a_im = inp("s5_a_im", [1, 32, 64])
        log_dt = inp("s5_log_dt", [1, 32])
        b_re = inp("s5_b_re", [1, 32, 64, 16]); b_im = inp("s5_b_im", [1, 32, 64, 16])
        c_re = inp("s5_c_re", [1, 32, 16, 64]); c_im = inp("s5_c_im", [1, 32, 16, 64])
        s5_d = inp("s5_d", [1, 512])
        glu_w = inp("glu_w", [1, 512, 512]); glu_b = inp("glu_b", [1, 512])
        sgu_ln_g = inp("sgu_ln_g", [1, 512]); sgu_ln_b = inp("sgu_ln_b", [1, 512])
        sgu_ws = inp("sgu_ws", [1, 4, 128, 128]); sgu_b = inp("sgu_b", [1, 4, 128])
        on_ssm = inp("out_norm_ssm", [1, 512]); on_sgu = inp("out_norm_sgu", [1, 512])
        w_out = inp("w_out", [1, D, D])
        ln_ffn_g = inp("ln_ffn_g", [1, D])
        peer_wq = inp("peer_wq", [1, D, 2048])
        peer_keys = inp("peer_keys", [1, 8, 2, 128, 128])
        peer_u = inp("peer_u", [1, 16384, D]); peer_v = inp("peer_v", [1, 16384, D])
        ple_norm_g = inp("ple_norm_g", [1, D])
        ple_w_gate = inp("ple_w_gate", [1, D, D]); ple_w_proj = inp("ple_w_proj", [1, 256, D])
        final_g = inp("final_g", [D])
        out = nc.dram_tensor("out", [T, D], F32, kind="ExternalOutput").ap()
        self.out = out
        dbg = self.dbg
        if dbg:
            self.dbg_out = {}

        def dbg_t(name, shape, dt=F32):
            t = nc.dram_tensor("dbg_" + name, list(shape), dt, kind="ExternalOutput").ap()
            self.dbg_out[name] = t
            return t

        h1_s = nc.dram_tensor("h1_s", [T, D], F32, kind="Internal").ap()
        hnT_s = nc.dram_tensor("hnT_s", [NT, 128, D], BF16, kind="Internal").ap()
        UT_s = nc.dram_tensor("UT_s", [128, 128, D], BF16, kind="Internal").ap()
        V_s = nc.dram_tensor("V_s", [128, 128, D], BF16, kind="Internal").ap()

        root = self.root
        sb, ps = self.sb, self.ps
        identF = sb(root, "identF", [128, 128], F32)
        identB = sb(root, "identB", [128, 128], BF16)
        onesB = sb(root, "onesB", [128, 128], BF16)
        self.halfpi = sb(root, "halfpi", [128, 1], F32)
        cB = S.buf("consts")
        S.add("pool", lambda e: e.memset(identF[:], 1.0), [], [cB])
        S.add("pool", lambda e: e.affine_select(out=identF[:], in_=identF[:], pattern=[[-1, 128]],
                                                compare_op=ALU.is_equal, fill=0.0, base=0, channel_multiplier=1), [cB], [cB])
        S.add("pool", lambda e: e.tensor_copy(out=identB[:], in_=identF[:]), [cB], [cB])
        S.add("pool", lambda e: e.memset(onesB[:], 1.0), [], [cB])
        S.add("pool", lambda e: e.memset(self.halfpi[:], math.pi / 2), [], [cB])
        S.emit()

        mixer = ExitStack()
        xsT_all = sb(mixer, "xsT_all", [128, 4, T], BF16)
        ygT_all = sb(mixer, "ygT_all", [128, 4, T], BF16)
        ss_sgu = sb(mixer, "ss_sgu", [128, NT], F32)
        xsB = S.bufs_n(4, "xsT")
        ygB = S.buf("ygT")
        ssgB = S.buf("ss_sgu")

        a1 = ExitStack()
        win = sb(a1, "win", [128, 8, 1536], BF16)
        stage = [sb(a1, "stage%d" % i, [128, 1536], F32) for i in range(2)]
        stageB = S.bufs_n(2, "stage")
        gmix = sb(a1, "gmix", [128, 8], F32)
        winB = S.buf("win"); gmixB = S.buf("gmix")
        S.dma("sp", gmix[:], ln_mix_g.rearrange("o (kc p) -> p (o kc)", p=128), [], [gmixB], allow_slow_non_contiguous=True)
        self.load_weight(win, w_in[0], 8, 1536, gmix, stage, stageB, winB, gmixB)
        wsmT = sb(a1, "wsmT", [128, 4, 128], BF16)
        Rt = sb(a1, "Rt", [128, 4, 128], F32)
        sg_g = sb(a1, "sg_g", [128, 4], F32)
        sg_b = sb(a1, "sg_b", [128, 4], F32)
        bs1 = sb(a1, "bs1", [1, 512], F32)
        bs1b = sb(a1, "bs1b", [1, 512], BF16)
        sguB = S.buf("sgu_consts")
        ws_t = sb(a1, "ws_t", [128, 4, 128], F32)
        ws_tB = S.buf("ws_t")
        pA = [ps(a1, "pA%d" % i, [128, 512], F32) for i in range(6)]
        pAB = S.bufs_n(6, "pA")
        pT = ps(a1, "pT", [128, 1024], BF16)
        pTB = S.buf("pT")
        S.dma("sp", sg_g[:], sgu_ln_g.rearrange("o (h d) -> d (o h)", d=128), [], [sguB], allow_slow_non_contiguous=True)
        S.dma("sp", sg_b[:], sgu_ln_b.rearrange("o (h d) -> d (o h)", d=128), [], [sguB], allow_slow_non_contiguous=True)
        S.dma("sp", bs1[:], sgu_b.rearrange("o h t -> o (h t)"), [], [sguB])
        for h in range(4):
            S.dma("act", ws_t[:, h, :], sgu_ws[0, h], [], [ws_tB])
        S.add("pool", lambda e: e.affine_select(out=ws_t[:], in_=ws_t[:], pattern=[[0, 4], [-1, 128]],
                                                compare_op=ALU.is_ge, fill=0.0, base=0, channel_multiplier=1),
              [ws_tB], [ws_tB])
        for h in range(4):
            self.tr(pA[0][:, h * 128:(h + 1) * 128], ws_t[:, h, :], identF[:], [ws_tB], [pAB[0]])
        self.dve(lambda e: e.tensor_copy(out=wsmT[:].rearrange("p h t -> p (h t)"), in_=pA[0][:]), [pAB[0]], [sguB])
        self.dve(lambda e: e.tensor_copy(out=bs1b[:], in_=bs1[:]), [sguB], [sguB])
        self.mm(pA[1][:], onesB[:], wsmT[:].rearrange("p h t -> p (h t)"), True, True, [sguB], [pAB[1]])
        self.mm(pA[2][:], onesB[0:1, :], bs1b[:], True, True, [sguB], [pAB[2]])
        self.dve(lambda e: e.tensor_copy(out=Rt[:].rearrange("p h t -> p (h t)"), in_=pA[2][:]), [pAB[2]], [sguB])
        for h in range(4):
            self.dve(lambda e, h=h: e.scalar_tensor_tensor(out=Rt[:, h, :], in0=pA[1][:, h * 128:(h + 1) * 128],
                                                            scalar=sg_b[:, h:h + 1], in1=Rt[:, h, :],
                                                            op0=ALU.mult, op1=ALU.add),
                     [pAB[1], sguB], [sguB])

        xt = [sb(a1, "xt%d" % i, [128, D], F32) for i in range(2)]
        xtB = S.bufs_n(2, "xt")
        junk = sb(a1, "junk", [128, D], BF16)
        junkB = S.buf("junk")
        st1 = [sb(a1, "st1_%d" % i, [128, 2], F32) for i in range(2)]
        st1B = S.bufs_n(2, "st1")
        xnb = [sb(a1, "xnb%d" % i, [128, D], BF16) for i in range(2)]
        xnbB = S.bufs_n(2, "xnb")
        xnT = [sb(a1, "xnT%d" % i, [128, 8, 128], BF16) for i in range(2)]
        xnTB = S.bufs_n(2, "xnT")
        gu = [sb(a1, "gu%d" % i, [128, 4, 128], F32) for i in range(2)]
        guB = S.bufs_n(2, "gu")
        gv = [sb(a1, "gv%d" % i, [128, 512], F32) for i in range(2)]
        gvB = S.bufs_n(2, "gv")
        bst = sb(a1, "bst", [128, 4, 6], F32); mv = sb(a1, "mv", [128, 4, 2], F32); lrs = sb(a1, "lrs", [128, 4], F32)
        lnB = S.buf("lnstats")
        vn = [sb(a1, "vn%d" % i, [128, 512], BF16) for i in range(2)]
        vnB = S.bufs_n(2, "vn")
        mt = sb(a1, "mt", [128, 4, 128], F32); mtB = S.buf("mt")
        ysq = sb(a1, "ysq", [128, 4, 128], BF16); ysqB = S.buf("ysq")

        NTB = 4
        ust = [sb(a1, "ust%d" % i, [128, D], F32) for i in range(NTB)]; ustB = S.bufs_n(NTB, "ust")
        vst = [sb(a1, "vst%d" % i, [128, D], F32) for i in range(NTB)]; vstB = S.bufs_n(NTB, "vst")
        ubf = [sb(a1, "ubf%d" % i, [128, D], BF16) for i in range(2)]; ubfB = S.bufs_n(2, "ubf")
        utb = [sb(a1, "utb%d" % i, [128, D], BF16) for i in range(3)]; utbB = S.bufs_n(3, "utb")
        vbf = [sb(a1, "vbf%d" % i, [128, D], BF16) for i in range(3)]; vbfB = S.bufs_n(3, "vbf")
        pTu = ps(a1, "pTu", [128, D], BF16); pTuB = S.buf("pTu")

        def t_load(tb_):
            if tb_ >= 128:
                return
            k4 = tb_ % NTB
            S.dma("sp", ust[k4][:], peer_u[0, tb_ * 128:(tb_ + 1) * 128, :], [], [ustB[k4]])
            S.dma("sp", vst[k4][:], peer_v[0, tb_ * 128:(tb_ + 1) * 128, :], [], [vstB[k4]])

        def t_block(tb_):
            t_load(tb_ + 2)
            k = tb_ % 2
            k4 = tb_ % NTB
            self.act(ubf[k][:], ust[k4][:], AF.Copy, [ustB[k4]], [ubfB[k]])
            for kc in range(8):
                self.tr(pTu[:, kc * 128:(kc + 1) * 128], ubf[k][:, kc * 128:(kc + 1) * 128], identB[:], [ubfB[k]], [pTuB])
            k3 = tb_ % 3
            self.dve(lambda e, k3=k3: e.tensor_copy(out=utb[k3][:], in_=pTu[:]), [pTuB], [utbB[k3]])
            S.add("pool", lambda e, k3=k3, k4=k4: e.tensor_copy(out=vbf[k3][:], in_=vst[k4][:]), [vstB[k4]], [vbfB[k3]])
            if tb_ >= 1:
                t_store(tb_ - 1)

        def t_store(tb_):
            k3 = tb_ % 3
            S.dma("act", UT_s[tb_], utb[k3][:], [utbB[k3]], [])
            S.dma("act", V_s[tb_], vbf[k3][:], [vbfB[k3]], [])

        t_load(0)
        t_load(1)
        def a1_front(i):
            b = i % 2
            cols = slice(i * 128, (i + 1) * 128)
            for tb_ in range(4 * i, 4 * i + 4):
                t_block(tb_)
            S.dma("sp", xt[b][:], x[i * 128:(i + 1) * 128, :], [], [xtB[b]])
            self.dve(lambda e, b=b: e.scalar_tensor_tensor(out=junk[:], in0=xt[b][:], scalar=1.0, in1=xt[b][:],
                                                           op0=ALU.mult, op1=ALU.mult, accum_out=st1[b][:, 0:1]),
                     [xtB[b]], [junkB, st1B[b]])
            self.rstd(st1[b][:, 1:2], st1[b][:, 0:1], 1.0 / D, [st1B[b]], [st1B[b]])
            self.act(xnb[b][:], xt[b][:], AF.Copy, [xtB[b], st1B[b]], [xnbB[b]], scale=st1[b][:, 1:2])
            for kc in range(8):
                self.tr(pT[:, kc * 128:(kc + 1) * 128], xnb[b][:, kc * 128:(kc + 1) * 128], identB[:], [xnbB[b]], [pTB])
            self.dve(lambda e, b=b: e.tensor_copy(out=xnT[b][:].rearrange("p k t -> p (k t)"), in_=pT[:]), [pTB], [xnTB[b]])

        def a1_back(i):
            b = i % 2
            cols = slice(i * 128, (i + 1) * 128)
            for c in range(4):
                for kc in range(8):
                    self.mm(pA[0][:, c * 128:(c + 1) * 128], win[:, kc, c * 128:(c + 1) * 128], xnT[b][:, kc, :],
                            kc == 0, kc == 7, [winB, xnTB[b]], [pAB[0]])
            self.act(xsT_all[:, :, cols], pA[0][:].rearrange("p (c t) -> p c t", c=4), AF.Copy, [pAB[0]], xsB)
            for c in range(4):
                for kc in range(8):
                    self.mm(pA[1][:, c * 128:(c + 1) * 128], win[:, kc, 512 + c * 128:512 + (c + 1) * 128], xnT[b][:, kc, :],
                            kc == 0, kc == 7, [winB, xnTB[b]], [pAB[1]])
            self.act(gu[b][:].rearrange("p c t -> p (c t)"), pA[1][:], AF.Gelu, [pAB[1]], [guB[b]])
            for kc in range(8):
                self.mm(pA[2][:], xnT[b][:, kc, :], win[:, kc, 1024:1536], kc == 0, kc == 7, [winB, xnTB[b]], [pAB[2]])
            self.act(gv[b][:], pA[2][:], AF.Gelu, [pAB[2]], [gvB[b]])
            for h in range(4):
                self.dve(lambda e, b=b, h=h: e.bn_stats(out=bst[:, h, :], in_=gv[b][:, h * 128:(h + 1) * 128]), [gvB[b]], [lnB])
            for h in range(4):
                self.dve(lambda e, h=h: e.bn_aggr(out=mv[:, h, :], in_=bst[:, h, :]), [lnB], [lnB])
            self.dve(lambda e: e.tensor_scalar(out=lrs[:], in0=mv[:, :, 1], scalar1=EPS, scalar2=None, op0=ALU.add), [lnB], [lnB])
            self.act(lrs[:], lrs[:], AF.Sqrt, [lnB], [lnB])
            self.dve(lambda e: e.reciprocal(out=lrs[:], in_=lrs[:]), [lnB], [lnB])
            for h in range(4):
                self.dve(lambda e, b=b, h=h: e.tensor_scalar(out=vn[b][:, h * 128:(h + 1) * 128], in0=gv[b][:, h * 128:(h + 1) * 128],
                                                             scalar1=mv[:, h, 0:1], scalar2=lrs[:, h:h + 1],
                                                             op0=ALU.subtract, op1=ALU.mult),
                         [gvB[b], lnB], [vnB[b]])
            for h in range(4):
                self.mm(pA[3][:, h * 128:(h + 1) * 128], vn[b][:, h * 128:(h + 1) * 128], wsmT[:, h, :], True, True,
                        [vnB[b], sguB], [pAB[3]])
            for h in range(4):
                self.dve(lambda e, h=h: e.scalar_tensor_tensor(out=mt[:, h, :], in0=pA[3][:, h * 128:(h + 1) * 128],
                                                                scalar=sg_g[:, h:h + 1], in1=Rt[:, h, :],
                                                                op0=ALU.mult, op1=ALU.add),
                         [pAB[3], sguB], [mtB])
            self.dve(lambda e, b=b, cols=cols: e.tensor_tensor(out=ygT_all[:, :, cols], in0=mt[:], in1=gu[b][:], op=ALU.mult),
                     [mtB, guB[b]], [ygB])
            self.act(ysq[:], ygT_all[:, :, cols], AF.Square, [ygB], [ysqB])
            for c in range(4):
                self.mm(pA[4][:, 0:1], ysq[:, c, :], onesB[:, 0:1], c == 0, c == 3, [ysqB], [pAB[4]])
            self.dve(lambda e, i=i: e.tensor_copy(out=ss_sgu[:, i:i + 1], in_=pA[4][:, 0:1]), [pAB[4]], [ssgB])
        a1_front(0)
        for i in range(NT):
            if i + 1 < NT:
                a1_front(i + 1)
            a1_back(i)
        t_store(127)
        if dbg == "a1":
            d1 = dbg_t("xsT", [128, 4, T], BF16); d2 = dbg_t("ygT", [128, 4, T], BF16); d3 = dbg_t("ss_sgu", [128, NT])
            S.dma("sp", d1, xsT_all[:], xsB, []); S.dma("sp", d2, ygT_all[:], [ygB], []); S.dma("sp", d3, ss_sgu[:], [ssgB], [])
        S.emit()
        a1.close()
        if dbg == "a1":
            mixer.close()
            return

        s5p = ExitStack()
        T0 = sb(s5p, "T0", [128, 32, 128], BF16)
        Wv = sb(s5p, "Wv", [128, 32, 2, 64], BF16)
        Wy = sb(s5p, "Wy", [128, 32, 2, 128], BF16)
        cosT = sb(s5p, "cosT", [128, 16, 256], F32)
        sinT = sb(s5p, "sinT", [128, 16, 256], F32)
        r8 = sb(s5p, "r8", [128, 16], F32)
        dtile = sb(s5p, "dtile", [128, 32], F32)
        s5cB = S.buf("s5consts")
        selB = S.buf("sel")
        pp = ExitStack()
        are = sb(pp, "are", [128, 16]); aim = sb(pp, "aim", [128, 16]); dtb = sb(pp, "dtb", [128, 16])
        lrdt = sb(pp, "lrdt", [128, 16]); ang1 = sb(pp, "ang1", [128, 16])
        braw = [sb(pp, "braw%d" % i, [128, 16, 16]) for i in range(2)]
        craw = [sb(pp, "craw%d" % i, [128, 16, 16]) for i in range(2)]
        tauI = sb(pp, "tauI", [128, 17], I32); tau = sb(pp, "tau", [128, 17])
        kI = sb(pp, "kI", [128, 256], I32); kF = sb(pp, "kF", [128, 256])
        E17 = sb(pp, "E17", [128, 16, 17]); A17 = sb(pp, "A17", [128, 16, 17]); tmp17 = sb(pp, "tmp17", [128, 16, 17])
        sn17 = sb(pp, "sn17", [128, 16, 17]); cs17 = sb(pp, "cs17", [128, 16, 17])
        pw = [sb(pp, "pw%d" % i, [128, 16, 17]) for i in range(2)]
        sm = [sb(pp, "sm%d" % i, [128, 16]) for i in range(8)]
        Bb = [sb(pp, "Bb%d" % i, [128, 16, 16]) for i in range(2)]
        tb = [sb(pp, "tb%d" % i, [128, 16, 16]) for i in range(2)]
        Xr = sb(pp, "Xr", [128, 8, 8, 16]); Xi = sb(pp, "Xi", [128, 8, 8, 16])
        Yr = sb(pp, "Yr", [128, 8, 8, 16]); Yi = sb(pp, "Yi", [128, 8, 8, 16])
        t1 = sb(pp, "t1", [128, 8, 8, 16]); t2 = sb(pp, "t2", [128, 8, 8, 16])
        mask = sb(pp, "mask", [128, 128])
        Zb = [sb(pp, "Zb%d" % i, [128, 8, 128], BF16) for i in range(2)]
        pSb = ps(pp, "pSb", [128, 2, 1024], BF16); pSbB = S.bufs_n(2, "pSb")
        phr = sb(pp, "phr", [128, 16]); pht = sb(pp, "pht", [128, 16])
        pS = [ps(pp, "pS%d" % i, [128, 512], F32) for i in range(2)]
        pSB = S.bufs_n(2, "pS")
        PB = S.buf("s5prep")
        XB = S.buf("X"); YB = S.buf("Y")
        NC = dict(allow_slow_non_contiguous=True)
        for par in range(2):
            rows = slice(par * 64, (par + 1) * 64)
            S.dma(self.q(), are[rows, :], a_re[0].rearrange("(gp par) p -> par p gp", par=2)[par], [], [PB], **NC)
            S.dma(self.q(), aim[rows, :], a_im[0].rearrange("(gp par) p -> par p gp", par=2)[par], [], [PB], **NC)
            S.dma(self.q(), dtb[rows, :], bass.AP(log_dt.tensor, par, [[0, 64], [2, 16]]), [], [PB], **NC)
            S.dma(self.q(), braw[0][rows], b_re[0].rearrange("(gp par) p c -> par p gp c", par=2)[par], [], [PB])
            S.dma(self.q(), braw[1][rows], b_im[0].rearrange("(gp par) p c -> par p gp c", par=2)[par], [], [PB])
            for gp in range(16):
                S.dma(self.q(), craw[0][rows, gp, :], c_re[0, 2 * gp + par].rearrange("c p -> p c"), [], [PB], **NC)
                S.dma(self.q(), craw[1][rows, gp, :], c_im[0, 2 * gp + par].rearrange("c p -> p c"), [], [PB], **NC)
        for t in range(8):
            S.dma(self.q(), dtile[16 * t:16 * (t + 1), :], s5_d[0].rearrange("(g c) -> c g", c=16), [], [s5cB], **NC)
        S.add("pool", lambda e: e.iota(tauI[:, 0:8], pattern=[[-1, 8]], base=0, channel_multiplier=0), [], [PB])
        S.add("pool", lambda e: e.iota(tauI[:, 8:17], pattern=[[1, 9]], base=0, channel_multiplier=0), [PB], [PB])
        S.add("pool", lambda e: e.iota(kI[:], pattern=[[1, 256]], base=0, channel_multiplier=0), [PB], [PB])
        S.add("pool", lambda e: e.memset(mask[:], 1.0), [PB], [PB])
        S.add("pool", lambda e: e.affine_select(out=mask[:], in_=mask[:], pattern=[[16, 8], [0, 16]], compare_op=ALU.is_ge,
                                                fill=0.0, base=15, channel_multiplier=-1), [PB], [PB])
        if dbg == "s5p0":
            S.dma("sp", dbg_t("craw", [128, 16, 16]), craw[0][:], [PB], [])
            S.dma("sp", dbg_t("dtb", [128, 16]), dtb[:], [PB], [])
            S.dma("sp", dbg_t("mask", [128, 128]), mask[:], [PB], [])
            S.dma("sp", dbg_t("dtile", [128, 32]), dtile[:], [s5cB], [])
            S.emit(); pp.close(); s5p.close(); mixer.close()
            return
        P1 = [PB]
        d = lambda fn: self.dve(fn, P1, P1)
        TT = lambda o, a, b, op: d(lambda e: e.tensor_tensor(out=o, in0=a, in1=b, op=op))
        d(lambda e: e.tensor_copy(out=tau[:], in_=tauI[:]))
        d(lambda e: e.tensor_copy(out=kF[:], in_=kI[:]))
        self.act(dtb[:], dtb[:], AF.Exp, P1, P1)
        TT(lrdt[:], are[:], dtb[:], ALU.mult)
        TT(ang1[:], aim[:], dtb[:], ALU.mult)
        s17 = [128, 16, 17]
        taub = _bc(tau[:].unsqueeze(1), s17)
        TT(E17[:], _bc(lrdt[:].unsqueeze(2), s17), taub, ALU.mult)
        self.act(E17[:], E17[:], AF.Exp, P1, P1)
        TT(A17[:], _bc(ang1[:].unsqueeze(2), s17), taub, ALU.mult)
        self.sincos(A17[:], sn17[:], cs17[:], tmp17[:], P1)
        TT(pw[0][:], E17[:], cs17[:], ALU.mult)
        TT(pw[1][:], E17[:], sn17[:], ALU.mult)
        d(lambda e: e.tensor_copy(out=r8[:], in_=E17[:, :, 16]))
        if dbg == "s5p1":
            S.dma("sp", dbg_t("pw0", [128, 16, 17]), pw[0][:], [PB], [])
            S.dma("sp", dbg_t("pw1", [128, 16, 17]), pw[1][:], [PB], [])
            S.emit(); pp.close(); s5p.close(); mixer.close()
            return
        abr, abi = pw[0][:, :, 9], pw[1][:, :, 9]
        nr, den, cre, cim, u1, u2 = [m[:] for m in sm[:6]]
        d(lambda e: e.tensor_scalar(out=nr, in0=abr, scalar1=-1.0, scalar2=None, op0=ALU.add))
        TT(den, are[:], are[:], ALU.mult); TT(u1, aim[:], aim[:], ALU.mult); TT(den, den, u1, ALU.add)
        d(lambda e: e.reciprocal(out=den, in_=den))
        TT(u1, nr, are[:], ALU.mult); TT(u2, abi, aim[:], ALU.mult); TT(cre, u1, u2, ALU.add); TT(cre, cre, den, ALU.mult)
        TT(u1, abi, are[:], ALU.mult); TT(u2, nr, aim[:], ALU.mult); TT(cim, u1, u2, ALU.subtract); TT(cim, cim, den, ALU.mult)
        s16 = [128, 16, 16]
        creb, cimb = _bc(cre.unsqueeze(2), s16), _bc(cim.unsqueeze(2), s16)
        TT(tb[0][:], creb, braw[0][:], ALU.mult); TT(tb[1][:], cimb, braw[1][:], ALU.mult); TT(Bb[0][:], tb[0][:], tb[1][:], ALU.subtract)
        TT(tb[0][:], creb, braw[1][:], ALU.mult); TT(tb[1][:], cimb, braw[0][:], ALU.mult); TT(Bb[1][:], tb[0][:], tb[1][:], ALU.add)
        s4 = [128, 8, 8, 16]

        def cmul(o_re, o_im, ar, ai, br, bi, neg_im=False, rw=P1):
            dd = lambda fn: self.dve(fn, rw, rw)
            dd(lambda e: e.tensor_tensor(out=t1[:], in0=ar, in1=br, op=ALU.mult))
            dd(lambda e: e.tensor_tensor(out=t2[:], in0=ai, in1=bi, op=ALU.mult))
            dd(lambda e: e.tensor_tensor(out=o_re, in0=t1[:], in1=t2[:], op=ALU.subtract))
            dd(lambda e: e.tensor_tensor(out=t1[:], in0=ar, in1=bi, op=ALU.mult))
            dd(lambda e: e.tensor_tensor(out=t2[:], in0=ai, in1=br, op=ALU.mult))
            if neg_im:
                dd(lambda e: e.scalar_tensor_tensor(out=o_im, in0=t1[:], scalar=-1.0, in1=t2[:], op0=ALU.mult, op1=ALU.subtract))
            else:
                dd(lambda e: e.tensor_tensor(out=o_im, in0=t1[:], in1=t2[:], op=ALU.add))

        RW = [PB, XB, YB, s5cB]
        self.dve(lambda e: e.memset(Wy[:], 0.0), RW, RW)
        for hf in range(2):
            gs = slice(8 * hf, 8 * hf + 8)
            Nre = _bc(pw[0][:, gs, 0:8].unsqueeze(3), s4); Nim = _bc(pw[1][:, gs, 0:8].unsqueeze(3), s4)
            Pre = _bc(pw[0][:, gs, 8:16].unsqueeze(3), s4); Pim = _bc(pw[1][:, gs, 8:16].unsqueeze(3), s4)
            P1re = _bc(pw[0][:, gs, 9:17].unsqueeze(3), s4); P1im = _bc(pw[1][:, gs, 9:17].unsqueeze(3), s4)
            Bre = _bc(Bb[0][:, gs].unsqueeze(2), s4); Bim = _bc(Bb[1][:, gs].unsqueeze(2), s4)
            Cre = _bc(craw[0][:, gs].unsqueeze(2), s4); Cim = _bc(craw[1][:, gs].unsqueeze(2), s4)
            cmul(Xr[:], Xi[:], Nre, Nim, Bre, Bim, rw=RW)
            cmul(Yr[:], Yi[:], Pre, Pim, Cre, Cim, neg_im=True, rw=RW)
            if dbg == "s5p2":
                S.dma("sp", dbg_t("Xr", [128, 8, 8, 16]), Xr[:], RW, [])
                S.dma("sp", dbg_t("Yi", [128, 8, 8, 16]), Yi[:], RW, [])
                S.emit(); pp.close(); s5p.close(); mixer.close()
                return
            for par in range(2):
                rows = slice(par * 64, (par + 1) * 64)
                for r in range(2):
                    for gi in range(4):
                        gp = 4 * r + gi
                        self.mm(pS[par][:, gi * 128:(gi + 1) * 128], Xr[rows, gp].rearrange("p s c -> p (s c)"),
                                Yr[rows, gp].rearrange("p s c -> p (s c)"), True, False, RW, [pSB[par]])
                        self.mm(pS[par][:, gi * 128:(gi + 1) * 128], Xi[rows, gp].rearrange("p s c -> p (s c)"),
                                Yi[rows, gp].rearrange("p s c -> p (s c)"), False, True, RW, [pSB[par]])
                    gsl2 = slice(16 * hf + 8 * r + par, 16 * hf + 8 * r + 8, 2)
                    self.dve(lambda e, par=par, gsl2=gsl2: e.tensor_tensor(out=T0[:, gsl2, :], in0=pS[par][:].rearrange("p (g m) -> p g m", g=4),
                                                                          in1=_bc(mask[:].unsqueeze(1), [128, 4, 128]), op=ALU.mult),
                             [pSB[par], PB], [s5cB])
            if dbg == "s5p3":
                S.dma("sp", dbg_t("T0", [128, 32, 128], BF16), T0[:], RW, [])
                S.emit(); pp.close(); s5p.close(); mixer.close()
                return
            s3 = [128, 8, 128]
            P7re = _bc(pw[0][:, gs, 15:16], s3); P7im = _bc(pw[1][:, gs, 15:16], s3)
            t1v, t2v = t1[:].rearrange("p g s c -> p g (s c)"), t2[:].rearrange("p g s c -> p g (s c)")
            Xrv, Xiv = Xr[:].rearrange("p g s c -> p g (s c)"), Xi[:].rearrange("p g s c -> p g (s c)")
            Zr, Zi = Zb[0][:], Zb[1][:]
            dz = lambda fn: self.dve(fn, RW, RW)
            dz(lambda e, t1v=t1v, P7re=P7re, Xrv=Xrv: e.tensor_tensor(out=t1v, in0=P7re, in1=Xrv, op=ALU.mult))
            dz(lambda e, t2v=t2v, P7im=P7im, Xiv=Xiv: e.tensor_tensor(out=t2v, in0=P7im, in1=Xiv, op=ALU.mult))
            dz(lambda e, Zr=Zr, t1v=t1v, t2v=t2v: e.tensor_tensor(out=Zr, in0=t1v, in1=t2v, op=ALU.subtract))
            dz(lambda e, t1v=t1v, P7re=P7re, Xiv=Xiv: e.tensor_tensor(out=t1v, in0=P7re, in1=Xiv, op=ALU.mult))
            dz(lambda e, t2v=t2v, P7im=P7im, Xrv=Xrv: e.tensor_tensor(out=t2v, in0=P7im, in1=Xrv, op=ALU.mult))
            dz(lambda e, Zi=Zi, t1v=t1v, t2v=t2v: e.tensor_tensor(out=Zi, in0=t1v, in1=t2v, op=ALU.add))
            for par in range(2):
                rows = slice(par * 64, (par + 1) * 64)
                for r in range(2):
                    for gi in range(4):
                        gp = 4 * r + gi
                        self.tr(pSb[:, par, gi * 128:gi * 128 + 64], Zb[0][rows, gp, :], identB[rows, rows], RW, [pSbB[par]])
                        self.tr(pSb[:, par, gi * 128 + 64:(gi + 1) * 128], Zb[1][rows, gp, :], identB[rows, rows], RW, [pSbB[par]])
                    gsl2 = slice(16 * hf + 8 * r + par, 16 * hf + 8 * r + 8, 2)
                    self.dve(lambda e, par=par, gsl2=gsl2: e.tensor_copy(out=Wv[:, gsl2], in_=pSb[:, par, 0:512].rearrange("p (g r m) -> p g r m", g=4, r=2)),
                             [pSbB[par]], [s5cB])
            P1re = _bc(pw[0][:, gs, 9:17].unsqueeze(3), s4); P1im = _bc(pw[1][:, gs, 9:17].unsqueeze(3), s4)
            cmul(Xr[:], Xi[:], P1re, P1im, Cre, Cim, neg_im=True, rw=RW)
            for par in range(2):
                rows = slice(par * 64, (par + 1) * 64)
                gsl2 = slice(16 * hf + par, 16 * hf + 16, 2)
                for ri, src in enumerate((Xr, Xi)):
                    self.dve(lambda e, rows=rows, gsl2=gsl2, ri=ri, src=src: e.tensor_copy(
                        out=Wy[rows, gsl2, ri, :], in_=src[rows].rearrange("p g s c -> p g (s c)")), RW, RW)
        d(lambda e: e.tensor_scalar(out=phr[:], in0=ang1[:], scalar1=8.0, scalar2=None, op0=ALU.mult))
        MAGIC = 12582912.0
        d(lambda e: e.tensor_scalar(out=pht[:], in0=phr[:], scalar1=1.0 / TWO_PI, scalar2=MAGIC, op0=ALU.mult, op1=ALU.add))
        d(lambda e: e.tensor_scalar(out=pht[:], in0=pht[:], scalar1=MAGIC, scalar2=None, op0=ALU.subtract))
        d(lambda e: e.scalar_tensor_tensor(out=phr[:], in0=pht[:], scalar=-6.28125, in1=phr[:], op0=ALU.mult, op1=ALU.add))
        d(lambda e: e.scalar_tensor_tensor(out=phr[:], in0=pht[:], scalar=-(TWO_PI - 6.28125), in1=phr[:], op0=ALU.mult, op1=ALU.add))
        sh = [128, 4, 256]
        angb = t1[:].rearrange("p g s c -> p (g s c)").rearrange("p (a k) -> p a k", k=256)
        tmpb = t2[:].rearrange("p g s c -> p (g s c)").rearrange("p (a k) -> p a k", k=256)
        for hf in range(4):
            gsl = slice(4 * hf, 4 * hf + 4)
            self.dve(lambda e, gsl=gsl: e.tensor_tensor(out=angb, in0=_bc(phr[:, gsl].unsqueeze(2), sh),
                                                        in1=_bc(kF[:].unsqueeze(1), sh), op=ALU.mult), RW, RW)
            self.sincos(angb, sinT[:, gsl, :], cosT[:, gsl, :], tmpb, RW)
        if dbg == "s5p":
            for nm, tt, dt_ in (("T0", T0, BF16), ("Wv", Wv, BF16), ("Wy", Wy, BF16), ("cosT", cosT, F32), ("sinT", sinT, F32)):
                S.dma("sp", dbg_t(nm, list(tt[:].shape), dt_), tt[:], [s5cB, PB], [])
            S.dma("sp", dbg_t("pw0", [128, 16, 17]), pw[0][:], [PB], [])
            S.dma("sp", dbg_t("pw1", [128, 16, 17]), pw[1][:], [PB], [])
        S.emit()
        pp.close()
        if dbg == "s5p":
            s5p.close(); mixer.close()
            return

        sm_ = ExitStack()
        Sel = sb(sm_, "Sel", [128, 8, 8, 128], BF16)
        SelO = sb(sm_, "SelO", [128, 8, 8, 128], BF16)
        S.add("pool", lambda e: e.memset(Sel[:], 0.0), [], [selB])
        S.add("pool", lambda e: e.memset(SelO[:], 0.0), [], [selB])
        idv = identB[:].rearrange("p (g c) -> p g c", c=16)
        for s_ in range(8):
            S.add("pool", lambda e, s_=s_: e.tensor_copy(out=Sel[:, :, s_, 16 * s_:16 * (s_ + 1)], in_=idv), [selB], [selB])
            S.add("pool", lambda e, s_=s_: e.tensor_copy(out=SelO[:, s_, :, 16 * s_:16 * (s_ + 1)], in_=idv), [selB], [selB])
        ublk = [[sb(sm_, "ublk%d%d" % (i, j), [128, 512], BF16) for j in range(2)] for i in range(2)]
        ublkB = [S.bufs_n(2, "ublk%d" % i) for i in range(2)]
        yblk = sb(sm_, "yblk", [128, 8, 512], BF16); yblkB = S.bufs_n(8, "yblk")
        Gm = [sb(sm_, "Gm%d" % i, [128, 2, 256]) for i in range(2)]
        Sc = [sb(sm_, "Sc%d" % i, [128, 2, 256]) for i in range(2)]
        tq = [sb(sm_, "tq%d" % i, [128, 2, 256]) for i in range(2)]
        Hp = [sb(sm_, "Hp%d" % i, [128, 2, 512], BF16) for i in range(2)]
        HpB = S.bufs_n(2, "Hp")
        wB = S.buf("s5work")
        pU = [ps(sm_, "pU%d" % i, [128, 512]) for i in range(2)]; pUB = S.bufs_n(2, "pU")
        pV = ps(sm_, "pV", [128, 2, 512]); pVB = S.buf("pV")
        pY = [ps(sm_, "pY%d" % i, [128, 512]) for i in range(2)]; pYB = S.bufs_n(2, "pY")
        pO = [ps(sm_, "pO%d" % i, [128, 512]) for i in range(2)]; pOB = S.bufs_n(2, "pO")
        self.dve(lambda e: e.memset(Sc[0][:], 0.0), [wB], [wB])
        self.dve(lambda e: e.memset(Sc[1][:], 0.0), [wB], [wB])
        def xv_of(c):
            return xsT_all[:, c, :].rearrange("p (b j s) -> p b j s", b=2, s=8)

        def st_R(gp):
            c = gp // 4; db = gp % 2; xv = xv_of(c)
            for par in range(2):
                g = 2 * gp + par
                gl = g % 8
                for s_ in range(8):
                    self.mm(pU[par][:], Sel[:, gl, s_, :], xv[:, :, :, s_], s_ == 0, s_ == 7, [selB, xsB[c]], [pUB[par]])
                self.act(ublk[db][par][:], pU[par][:], AF.Copy, [pUB[par]], [ublkB[db][par]])

        def st_V(gp):
            db = gp % 2
            for par in range(2):
                g = 2 * gp + par
                for ri in range(2):
                    self.mm(pV[par * 64:(par + 1) * 64, ri, :], Wv[:, g, ri, :], ublk[db][par][:], True, True,
                            [s5cB, ublkB[db][par]], [pVB])

        def dv(fn, r=()):
            self.dve(fn, [wB, s5cB] + list(r), [wB])

        def st_M(gp):
            cm = _bc(cosT[:, gp, 1:256].unsqueeze(1), [128, 2, 255]); sn = _bc(sinT[:, gp, 1:256].unsqueeze(1), [128, 2, 255])
            Vre = pV[:, 0, :].rearrange("p (b j) -> p b j", b=2)[:, :, 0:255]
            Vim = pV[:, 1, :].rearrange("p (b j) -> p b j", b=2)[:, :, 0:255]
            dv(lambda e: e.tensor_tensor(out=tq[0][:, :, 0:255], in0=Vre, in1=cm, op=ALU.mult), [pVB])
            dv(lambda e: e.tensor_tensor(out=tq[1][:, :, 0:255], in0=Vim, in1=sn, op=ALU.mult), [pVB])
            dv(lambda e: e.tensor_tensor(out=Gm[0][:, :, 0:255], in0=tq[0][:, :, 0:255], in1=tq[1][:, :, 0:255], op=ALU.add))
            dv(lambda e: e.tensor_tensor(out=tq[0][:, :, 0:255], in0=Vim, in1=cm, op=ALU.mult), [pVB])
            dv(lambda e: e.tensor_tensor(out=tq[1][:, :, 0:255], in0=Vre, in1=sn, op=ALU.mult), [pVB])
            dv(lambda e: e.tensor_tensor(out=Gm[1][:, :, 0:255], in0=tq[0][:, :, 0:255], in1=tq[1][:, :, 0:255], op=ALU.subtract))

        def st_SD(gp):
            db = gp % 2
            r8b = _bc(r8[:, gp:gp + 1], [128, 255])
            for ri in range(2):
                for b_ in range(2):
                    dv(lambda e, ri=ri, b_=b_: e.tensor_tensor_scan(out=Sc[ri][:, b_, 1:256], data0=r8b, data1=Gm[ri][:, b_, 0:255],
                                                                     initial=0.0, op0=ALU.mult, op1=ALU.add))
            cd = _bc(cosT[:, gp, :].unsqueeze(1), [128, 2, 256]); sd = _bc(sinT[:, gp, :].unsqueeze(1), [128, 2, 256])
            Hv = Hp[db][:].rearrange("p r (b j) -> p r b j", b=2)
            dv(lambda e: e.tensor_tensor(out=tq[0][:], in0=Sc[0][:], in1=cd, op=ALU.mult))
            dv(lambda e: e.tensor_tensor(out=tq[1][:], in0=Sc[1][:], in1=sd, op=ALU.mult))
            self.dve(lambda e: e.tensor_tensor(out=Hv[:, 0], in0=tq[0][:], in1=tq[1][:], op=ALU.subtract), [wB], [wB, HpB[db]])
            dv(lambda e: e.tensor_tensor(out=tq[0][:], in0=Sc[1][:], in1=cd, op=ALU.mult))
            dv(lambda e: e.tensor_tensor(out=tq[1][:], in0=Sc[0][:], in1=sd, op=ALU.mult))
            self.dve(lambda e: e.tensor_tensor(out=Hv[:, 1], in0=tq[0][:], in1=tq[1][:], op=ALU.add), [wB], [wB, HpB[db]])

        def st_Y(gp):
            db = gp % 2
            for par in range(2):
                g = 2 * gp + par
                gl = g % 8
                self.mm(pY[par][:], T0[:, g, :], ublk[db][par][:], True, False, [s5cB, ublkB[db][par]], [pYB[par]])
                self.mm(pY[par][:], Wy[:, g, 0, :], Hp[db][:, 0, :], False, False, [s5cB, HpB[db]], [pYB[par]])
                self.mm(pY[par][:], Wy[:, g, 1, :], Hp[db][:, 1, :], False, True, [s5cB, HpB[db]], [pYB[par]])
                self.dve(lambda e, par=par, g=g, gl=gl: e.scalar_tensor_tensor(
                    out=yblk[:, gl, :], in0=ublk[db][par][:], scalar=dtile[:, g:g + 1], in1=pY[par][:], op0=ALU.mult, op1=ALU.add),
                    [ublkB[db][par], pYB[par], s5cB], [yblkB[gl]])

        def st_O(c):
            xv = xv_of(c)
            for t_ in range(8):
                po = pO[t_ % 2]; poB = pOB[t_ % 2]
                for gl in range(8):
                    self.mm(po[:], SelO[:, gl, t_, :], yblk[:, gl, :], gl == 0, gl == 7, [selB, yblkB[gl]], [poB])
                self.act(xv[:, :, :, t_], po[:].rearrange("p (b j) -> p b j", b=2), AF.Gelu, [poB], [xsB[c]])

        st_R(0)
        st_V(0)
        for gp in range(16):
            st_M(gp)
            if gp + 1 < 16:
                st_R(gp + 1)
                st_V(gp + 1)
            st_SD(gp)
            st_Y(gp)
            if gp % 4 == 3:
                st_O(gp // 4)
        if dbg == "s5":
            S.dma("sp", dbg_t("gysT", [128, 4, T], BF16), xsT_all[:], xsB, [])
        S.emit()
        sm_.close(); s5p.close()
        if dbg == "s5":
            mixer.close()
            return

        a2 = ExitStack()
        wout = sb(a2, "wout", [128, 8, 1024], BF16); woutB = S.buf("wout")
        glub = sb(a2, "glub", [128, 4, 512], BF16); glubB = S.buf("glub")
        stage2 = [sb(a2, "stage2_%d" % i, [128, 1024], F32) for i in range(2)]; stage2B = S.bufs_n(2, "stage2")
        gout = sb(a2, "gout", [128, 8], F32); goutB = S.buf("gout")
        gb4 = sb(a2, "gb4", [128, 4], F32)
        gffn = sb(a2, "gffn", [128, D], F32); gffnB = S.buf("gffn")
        S.dma("sp", gout[:, 0:4], on_ssm.rearrange("o (kc p) -> p (o kc)", p=128), [], [goutB], **NC)
        S.dma("sp", gout[:, 4:8], on_sgu.rearrange("o (kc p) -> p (o kc)", p=128), [], [goutB], **NC)
        S.dma("sp", gb4[:], glu_b.rearrange("o (kc p) -> p (o kc)", p=128), [], [goutB], **NC)
        S.dma("act", gffn[:], bass.AP(ln_ffn_g.tensor, 0, [[0, 128], [1, D]]), [], [gffnB])
        self.load_weight(wout, w_out[0], 8, 1024, gout, stage2, stage2B, woutB, goutB)
        self.load_weight(glub, glu_w[0], 4, 512, None, stage2, stage2B, glubB, None)
        pG = ps(a2, "pG", [128, 512]); pGB = S.buf("pG")
        pSS = ps(a2, "pSS", [128, 512]); pSSB = S.buf("pSS")
        pA2 = [ps(a2, "pA2_%d" % i, [128, 1024]) for i in range(2)]; pA2B = S.bufs_n(2, "pA2")
        pT2 = ps(a2, "pT2", [128, 1024], BF16); pT2B = S.buf("pT2")
        sg = sb(a2, "sg", [128, 4, 128], F32); sgB = S.buf("sg")
        ysg = [sb(a2, "ysg%d" % i, [128, 4, 128], BF16) for i in range(2)]; ysgB = S.bufs_n(2, "ysg")
        ysq2 = sb(a2, "ysq2", [128, 4, 128], BF16); ysq2B = S.buf("ysq2")
        rs = [sb(a2, "rs%d" % i, [128, 4], F32) for i in range(2)]; rsB = S.bufs_n(2, "rs")
        xt2 = [sb(a2, "xt2_%d" % i, [128, D], F32) for i in range(2)]; xt2B = S.bufs_n(2, "xt2")
        h1t = [sb(a2, "h1t%d" % i, [128, D], F32) for i in range(3)]; h1B = S.bufs_n(3, "h1t")
        junk2 = sb(a2, "junk2", [128, D], BF16); junk2B = S.buf("junk2")
        hnb = [sb(a2, "hnb%d" % i, [128, D], BF16) for i in range(2)]; hnbB = S.bufs_n(2, "hnb")
        hnT = [sb(a2, "hnT%d" % i, [128, D], BF16) for i in range(3)]; hnTB = S.bufs_n(3, "hnT")
        def a2_front(i):
            b = i % 2
            cols = slice(i * 128, (i + 1) * 128)
            S.dma("sp", xt2[b][:], x[i * 128:(i + 1) * 128, :], [], [xt2B[b]])
            for c in range(4):
                for kc in range(4):
                    self.mm(pG[:, c * 128:(c + 1) * 128], glub[:, kc, c * 128:(c + 1) * 128], xsT_all[:, kc, cols],
                            kc == 0, kc == 3, [glubB, xsB[kc]], [pGB])
            for c in range(4):
                self.act(sg[:, c, :], pG[:, c * 128:(c + 1) * 128], AF.Sigmoid, [pGB, goutB], [sgB], bias=gb4[:, c:c + 1])
            self.dve(lambda e, b=b, cols=cols: e.tensor_tensor(out=ysg[b][:], in0=sg[:], in1=xsT_all[:, :, cols], op=ALU.mult),
                     [sgB] + xsB, [ysgB[b]])
            self.act(ysq2[:], ysg[b][:], AF.Square, [ysgB[b]], [ysq2B])
            for c in range(4):
                self.mm(pSS[:, 0:1], ysq2[:, c, :], onesB[:, 0:1], c == 0, c == 3, [ysq2B], [pSSB])
            self.dve(lambda e, b=b: e.tensor_copy(out=rs[b][:, 0:1], in_=pSS[:, 0:1]), [pSSB], [rsB[b]])
            self.dve(lambda e, b=b, i=i: e.tensor_copy(out=rs[b][:, 1:2], in_=ss_sgu[:, i:i + 1]), [ssgB], [rsB[b]])
            self.rstd(rs[b][:, 0:2], rs[b][:, 0:2], 1.0 / 512, [rsB[b]], [rsB[b]])

        def a2_back(i):
            b = i % 2
            c3 = i % 3
            cols = slice(i * 128, (i + 1) * 128)
            for half in range(2):
                hs = slice(half * 512, (half + 1) * 512)
                for kc in range(4):
                    self.mm(pA2[0][:, hs], ysg[b][:, kc, :], wout[:, kc, hs], kc == 0, kc == 3, [ysgB[b], woutB], [pA2B[0]])
            for half in range(2):
                hs = slice(half * 512, (half + 1) * 512)
                for kc in range(4):
                    self.mm(pA2[1][:, hs], ygT_all[:, kc, cols], wout[:, 4 + kc, hs], kc == 0, kc == 3, [ygB, woutB], [pA2B[1]])
            self.dve(lambda e, b=b: e.scalar_tensor_tensor(out=h1t[c3][:], in0=pA2[0][:], scalar=rs[b][:, 0:1], in1=xt2[b][:],
                                                           op0=ALU.mult, op1=ALU.add), [pA2B[0], rsB[b], xt2B[b]], [h1B[c3]])
            self.dve(lambda e, b=b: e.scalar_tensor_tensor(out=h1t[c3][:], in0=pA2[1][:], scalar=rs[b][:, 1:2], in1=h1t[c3][:],
                                                           op0=ALU.mult, op1=ALU.add), [pA2B[1], rsB[b], h1B[c3]], [h1B[c3]])
            self.dve(lambda e, b=b: e.scalar_tensor_tensor(out=junk2[:], in0=h1t[c3][:], scalar=1.0, in1=h1t[c3][:],
                                                           op0=ALU.mult, op1=ALU.mult, accum_out=rs[b][:, 2:3]),
                     [h1B[c3]], [junk2B, rsB[b]])
            self.rstd(rs[b][:, 3:4], rs[b][:, 2:3], 1.0 / D, [rsB[b]], [rsB[b]])
            self.dve(lambda e, b=b: e.scalar_tensor_tensor(out=hnb[b][:], in0=h1t[c3][:], scalar=rs[b][:, 3:4], in1=gffn[:],
                                                           op0=ALU.mult, op1=ALU.mult), [h1B[c3], rsB[b], gffnB], [hnbB[b]])
            for kc in range(8):
                self.tr(pT2[:, kc * 128:(kc + 1) * 128], hnb[b][:, kc * 128:(kc + 1) * 128], identB[:], [hnbB[b]], [pT2B])
            self.act(hnT[c3][:], pT2[:], AF.Copy, [pT2B], [hnTB[c3]])
            if i >= 1:
                a2_store(i - 1)

        def a2_store(i):
            c3 = i % 3
            S.dma("act", h1_s[i * 128:(i + 1) * 128, :], h1t[c3][:], [h1B[c3]], [])
            S.dma("act", hnT_s[i], hnT[c3][:], [hnTB[c3]], [])
        a2_front(0)
        for i in range(NT):
            if i + 1 < NT:
                a2_front(i + 1)
            a2_back(i)
        a2_store(NT - 1)
        S.emit()
        a2.close(); mixer.close()
        if dbg == "a2":
            d1 = dbg_t("h1", [T, D])
            with nc.sbuf_tensor("dbgbuf", [128, D], F32) as dbuf:
                dB = S.buf("dbuf")
                for i in range(NT):
                    S.dma("sp", dbuf[:], h1_s[i * 128:(i + 1) * 128, :], [], [dB])
                    S.dma("sp", d1[i * 128:(i + 1) * 128, :], dbuf[:], [dB], [])
                S.emit()
            return

        rt_s = nc.dram_tensor("rt_s", [3, 128, T], F32, kind="Internal").ap()
        rp = ExitStack()
        wq = sb(rp, "wq", [128, 8, 2048], BF16); wqB = S.buf("wq")
        stq = [sb(rp, "stq%d" % i, [128, 2048], F32) for i in range(2)]; stqB = S.bufs_n(2, "stq")
        keysT = sb(rp, "keysT", [128, 16, 128], BF16); keysB = S.buf("keysT")
        kst = [sb(rp, "kst%d" % i, [128, 128], F32) for i in range(2)]; kstB = S.bufs_n(2, "kst")
        iota16 = sb(rp, "iota16", [128, 16], F32); iotaI = sb(rp, "iotaI", [128, 16], I32); ioB = S.buf("iota16")
        S.add("pool", lambda e: e.iota(iotaI[:], pattern=[[1, 16]], base=0, channel_multiplier=0), [], [ioB])
        S.add("pool", lambda e: e.tensor_copy(out=iota16[:], in_=iotaI[:]), [ioB], [ioB])
        self.load_weight(wq, peer_wq[0], 8, 2048, None, stq, stqB, wqB, None)
        pR = [ps(rp, "pR%d" % i, [128, 2048]) for i in range(2)]; pRB = S.bufs_n(2, "pR")
        for hk in range(16):
            S.dma(self.q(), kst[hk % 2][:], peer_keys[0, hk // 2, hk % 2], [], [kstB[hk % 2]])
            self.tr(pR[0][:, hk * 128:(hk + 1) * 128], kst[hk % 2][:], identF[:], [kstB[hk % 2]], [pRB[0]])
        self.dve(lambda e: e.tensor_copy(out=keysT[:].rearrange("p a n -> p (a n)"), in_=pR[0][:]), [pRB[0]], [keysB])
        hnp = [sb(rp, "hnp%d" % i, [128, 2, D], BF16) for i in range(2)]; hnpB = S.bufs_n(2, "hnp")
        qT = sb(rp, "qT", [128, 16, 256], BF16); qTB = S.buf("qT")
        def two(name, shape, dt=F32):
            return [sb(rp, "%s_%d" % (name, i), shape, dt) for i in range(2)]
        sc4 = [two("sc%d" % q_, [128, 16, 128]) for q_ in range(2)]; scB4 = [S.bufs_n(2, "sc%d_" % q_) for q_ in range(2)]
        scw2 = two("scw", [128, 16, 128])
        m82 = two("m8", [128, 16, 16]); ix2 = two("ix", [128, 16, 16], U32); ixf2 = two("ixf", [128, 16, 16])
        cand2 = two("cand", [128, 8, 256]); candw2 = two("candw", [128, 8, 256])
        c82 = two("c8", [128, 8, 16]); cix2 = two("cix", [128, 8, 16], U32)
        cia2 = two("cia", [128, 8, 16], U32); cib2 = two("cib", [128, 8, 16], U32)
        iaf2 = two("iaf", [128, 8, 16]); ibf2 = two("ibf", [128, 8, 16])
        oh2 = two("oh", [128, 8, 16, 16])
        res32 = two("res3", [128, 3, 8, 16])
        esum2 = two("esum", [128, 8])
        rtT = [sb(rp, "rtT%d" % i, [128, 3, 128], F32) for i in range(2)]; rtTB = S.bufs_n(2, "rtT")
        kB2 = S.bufs_n(2, "topk"); candB2 = S.bufs_n(2, "cand")
        tkB2 = [[S.bufs_n(5, "tk%d_%d_" % (j_, hk)) for hk in range(16)] for j_ in range(2)]
        tcB2 = [[S.bufs_n(5, "tc%d_%d_" % (j_, h)) for h in range(8)] for j_ in range(2)]
        s44 = [128, 8, 16, 16]

        def sub_tile(ps_, j):
            sc, scB, scw, m8, ix, ixf = sc4[ps_ % 2][j], scB4[ps_ % 2][j], scw2[j], m82[j], ix2[j], ixf2[j]
            cand, candw, c8, cix, cia, cib = cand2[j], candw2[j], c82[j], cix2[j], cia2[j], cib2[j]
            iaf, ibf, oh, res3, esum = iaf2[j], ibf2[j], oh2[j], res32[j], esum2[j]
            kB, candB, hB, cB_ = kB2[j], candB2[j], tkB2[j], tcB2[j]
            tile_i = 2 * ps_ + j
            for hk in range(16):
                self.dve(lambda e, hk=hk: e.max(out=m8[:, hk, 0:8], in_=sc[:, hk, :]), [scB], [hB[hk][0]])
                if hk % 4 == 3:
                    yield
            for hk in range(16):
                self.dve(lambda e, hk=hk: e.match_replace(out=scw[:, hk, :], in_to_replace=m8[:, hk, 0:8], in_values=sc[:, hk, :], imm_value=-1e30),
                         [scB, hB[hk][0]], [hB[hk][1]])
                if hk % 4 == 3:
                    yield
            for hk in range(16):
                self.dve(lambda e, hk=hk: e.max(out=m8[:, hk, 8:16], in_=scw[:, hk, :]), [hB[hk][1]], [hB[hk][2]])
                if hk % 4 == 3:
                    yield
            for hk in range(16):
                self.dve(lambda e, hk=hk: e.max_index(out=ix[:, hk, 0:8], in_max=m8[:, hk, 0:8], in_values=sc[:, hk, :]),
                         [scB, hB[hk][0]], [hB[hk][3]])
                if hk % 4 == 3:
                    yield
            for hk in range(16):
                self.dve(lambda e, hk=hk: e.max_index(out=ix[:, hk, 8:16], in_max=m8[:, hk, 8:16], in_values=sc[:, hk, :]),
                         [scB, hB[hk][2]], [hB[hk][4]])
                if hk % 4 == 3:
                    yield
            allh = [bb for hk in range(16) for bb in hB[hk]]
            self.dve(lambda e: e.tensor_copy(out=ixf[:], in_=ix[:]), allh, [kB])
            yield
            m8v = m8[:].rearrange("p (h k) n -> p h k n", k=2)
            ixv = ixf[:].rearrange("p (h k) n -> p h k n", k=2)
            S.add("pool", lambda e: e.tensor_tensor(out=cand[:].rearrange("p h (a b) -> p h a b", b=16),
                                                    in0=_bc(m8v[:, :, 0, :].unsqueeze(3), s44), in1=_bc(m8v[:, :, 1, :].unsqueeze(2), s44), op=ALU.add),
                  allh, [candB])
            yield
            for h in range(8):
                self.dve(lambda e, h=h: e.max(out=c8[:, h, 0:8], in_=cand[:, h, :]), [candB], [cB_[h][0]])
                if h % 4 == 3:
                    yield
            for h in range(8):
                self.dve(lambda e, h=h: e.match_replace(out=candw[:, h, :], in_to_replace=c8[:, h, 0:8], in_values=cand[:, h, :], imm_value=-1e30),
                         [candB, cB_[h][0]], [cB_[h][1]])
                if h % 4 == 3:
                    yield
            for h in range(8):
                self.dve(lambda e, h=h: e.max(out=c8[:, h, 8:16], in_=candw[:, h, :]), [cB_[h][1]], [cB_[h][2]])
                if h % 4 == 3:
                    yield
            for h in range(8):
                self.dve(lambda e, h=h: e.max_index(out=cix[:, h, 0:8], in_max=c8[:, h, 0:8], in_values=cand[:, h, :]),
                         [candB, cB_[h][0]], [cB_[h][3]])
                if h % 4 == 3:
                    yield
            for h in range(8):
                self.dve(lambda e, h=h: e.max_index(out=cix[:, h, 8:16], in_max=c8[:, h, 8:16], in_values=cand[:, h, :]),
                         [candB, cB_[h][2]], [cB_[h][4]])
                if h % 4 == 3:
                    yield
            allc = [bb for h in range(8) for bb in cB_[h]]
            KB = [kB, scB] + allc

            def dk(fn):
                self.dve(fn, KB + [ioB], KB)
            g3 = res3[:, 2]
            dk(lambda e: e.tensor_tensor(out=g3, in0=c8[:], in1=_bc(c8[:, :, 0:1], [128, 8, 16]), op=ALU.subtract))
            yield
            self.act(g3, g3, AF.Exp, KB, KB)
            yield
            dk(lambda e: e.tensor_reduce(out=esum[:], in_=g3, axis=AX.X, op=ALU.add))
            yield
            dk(lambda e: e.reciprocal(out=esum[:], in_=esum[:]))
            yield
            dk(lambda e: e.tensor_tensor(out=g3, in0=g3, in1=_bc(esum[:].unsqueeze(2), [128, 8, 16]), op=ALU.mult))
            yield
            dk(lambda e: e.tensor_single_scalar(out=cia[:], in_=cix[:], scalar=4, op=ALU.logical_shift_right))
            yield
            dk(lambda e: e.tensor_single_scalar(out=cib[:], in_=cix[:], scalar=15, op=ALU.bitwise_and))
            yield
            dk(lambda e: e.tensor_copy(out=iaf[:], in_=cia[:]))
            yield
            dk(lambda e: e.tensor_copy(out=ibf[:], in_=cib[:]))
            yield
            io4 = _bc(iota16[:].unsqueeze(1).unsqueeze(1), s44)
            for w_, (sel_f, kk) in enumerate(((iaf, 0), (ibf, 1))):
                dk(lambda e, sel_f=sel_f: e.tensor_tensor(out=oh[:], in0=_bc(sel_f[:].unsqueeze(3), s44), in1=io4, op=ALU.is_equal))
                yield
                S.add("pool", lambda e, kk=kk: e.tensor_tensor(out=oh[:], in0=oh[:], in1=_bc(ixv[:, :, kk, :].unsqueeze(2), s44), op=ALU.mult),
                      KB + [ioB], KB)
                yield
                dk(lambda e, w_=w_: e.tensor_reduce(out=res3[:, w_], in_=oh[:], axis=AX.X, op=ALU.add))
                yield
            rb = tile_i % 2
            for w_ in range(3):
                self.tr(pR[j][:, w_ * 128:(w_ + 1) * 128], res3[:, w_].rearrange("p h k -> p (h k)"), identF[:], KB, [pRB[j]])
            self.act(rtT[rb][:].rearrange("p w t -> p (w t)"), pR[j][:, 0:384], AF.Copy, [pRB[j]], [rtTB[rb]])
            S.dma("sp", rt_s[:, :, tile_i * 128:(tile_i + 1) * 128].rearrange("w p t -> p w t"), rtT[rb][:], [rtTB[rb]], [])
            yield

        def r_front(ps_):
            b2 = ps_ % 2
            for j in range(2):
                S.dma("sp", hnp[b2][:, j, :], hnT_s[2 * ps_ + j], [], [hnpB[b2]])
            for j in range(2):
                for hk in range(16):
                    for kc in range(8):
                        self.mm(pR[j][:, hk * 128:(hk + 1) * 128], wq[:, kc, hk * 128:(hk + 1) * 128],
                                hnp[b2][:, j, kc * 128:(kc + 1) * 128], kc == 0, kc == 7, [wqB, hnpB[b2]], [pRB[j]])
            for j in range(2):
                self.act(qT[:, :, j * 128:(j + 1) * 128], pR[j][:].rearrange("p (a t) -> p a t", a=16), AF.Copy, [pRB[j]], [qTB])
            for j in range(2):
                for hk in range(16):
                    self.mm(pR[j][:, hk * 128:(hk + 1) * 128], qT[:, hk, j * 128:(j + 1) * 128], keysT[:, hk, :], True, True,
                            [qTB, keysB], [pRB[j]])
                self.act(sc4[b2][j][:].rearrange("p a n -> p (a n)"), pR[j][:], AF.Copy, [pRB[j]], [scB4[b2][j]])

        r_front(0)
        for ps_ in range(T // 256):
            if ps_ + 1 < T // 256:
                r_front(ps_ + 1)
            gens = [sub_tile(ps_, 0), sub_tile(ps_, 1)]
            live = [True, True]
            while any(live):
                for gi_ in range(2):
                    if live[gi_]:
                        try:
                            next(gens[gi_])
                        except StopIteration:
                            live[gi_] = False
        if dbg == "r":
            dd = dbg_t("rt", [3, 128, T])
            with nc.sbuf_tensor("dbgbuf2", [128, 3, T], F32) as dbuf:
                dB = S.buf("dbuf2")
                S.emit()
                S.dma("sp", dbuf[:], rt_s.rearrange("w p t -> p w t"), [], [dB])
                S.dma("sp", dd.rearrange("w p t -> p w t"), dbuf[:], [dB], [])
                S.emit()
            rp.close()
            return
        S.emit()
        rp.close()

        ep = ExitStack()
        wg = sb(ep, "wg", [128, 8, 1024], BF16); wgB = S.buf("wg")
        wpj = sb(ep, "wpj", [128, 2, 1024], BF16); wpjB = S.buf("wpj")
        ot_ = sb(ep, "ot", [128, D], F32); ot = [ot_, ot_]; otB_ = S.buf("ot"); otB = [otB_, otB_]
        stE = ot; stEB = otB
        gpl4 = sb(ep, "gpl4", [128, 8], F32); gplB = S.buf("gpl4")
        gfin = sb(ep, "gfin", [128, D], F32); gfinB = S.buf("gfin")
        iotaB = sb(ep, "iotaB", [128, 128], BF16); iotaI2 = sb(ep, "iotaI2", [128, 128], I32); io2B = S.buf("iotaB")
        S.add("pool", lambda e: e.iota(iotaI2[:], pattern=[[1, 128]], base=0, channel_multiplier=0), [], [io2B])
        S.add("pool", lambda e: e.tensor_copy(out=iotaB[:], in_=iotaI2[:]), [io2B], [io2B])
        S.dma("sp", gpl4[:], ple_norm_g.rearrange("o (kc p) -> p (o kc)", p=128), [], [gplB], **NC)
        S.dma("act", gfin[:], bass.AP(final_g.tensor, 0, [[0, 128], [1, D]]), [], [gfinB])
        self.load_weight(wg, ple_w_gate[0], 8, 1024, gpl4, stE, stEB, wgB, gplB)
        self.load_weight(wpj, ple_w_proj[0], 2, 1024, None, stE, stEB, wpjB, None)
        GTs = [sb(ep, "GT%d" % i, [128, 256, 128], BF16) for i in range(2)]; GTBs = S.bufs_n(2, "GT")
        rtp = [sb(ep, "rtp%d" % i, [128, 3, 256], F32) for i in range(2)]; rtpB = S.bufs_n(2, "rtp")
        hne_ = sb(ep, "hne", [128, 2, D], BF16); hne = [hne_, hne_]; hneB_ = S.buf("hne"); hneB = [hneB_, hneB_]
        NPQ = 8
        Pq = [sb(ep, "Pq%d" % i, [128, 128], BF16) for i in range(NPQ)]; PqB = S.bufs_n(NPQ, "Pq")
        Qq = [sb(ep, "Qq%d" % i, [128, 128], BF16) for i in range(NPQ)]; QqB = S.bufs_n(NPQ, "Qq")
        NST = 4
        ut = [sb(ep, "ut%d" % i, [128, D], BF16) for i in range(NST)]; utB = S.bufs_n(NST, "ut")
        vt = [sb(ep, "vt%d" % i, [128, D], BF16) for i in range(NST)]; vtB = S.bufs_n(NST, "vt")
        gs = [sb(ep, "gs%d" % i, [128, 256], BF16) for i in range(2)]; gsB = S.bufs_n(2, "gs")
        aT = [sb(ep, "aT%d" % i, [128, 256], BF16) for i in range(3)]; aTB = S.bufs_n(3, "aT")
        yacc = [ps(ep, "yacc%d" % i, [128, 1024]) for i in range(2)]; yaccB = S.bufs_n(2, "yacc")
        pWS = ps(ep, "pWS", [128, 1024]); pWSB = S.bufs_n(2, "pWS")
        pWG = ps(ep, "pWG", [128, 512]); pWGB = S.buf("pWG")
        pWC = ps(ep, "pWC", [128, 512]); pWCB = S.buf("pWC")
        pWCb = pWC[:].bitcast(BF16)
        pWD, pWDB, pWDb = pWC, pWCB, pWCb
        h1e = [sb(ep, "h1e%d" % i, [128, D], F32) for i in range(2)]; h1eB = S.bufs_n(2, "h1e")
        pte_ = sb(ep, "pte", [128, 256], F32); pte = [pte_, pte_]; pteB_ = S.buf("pte"); pteB = [pteB_, pteB_]
        rse = sb(ep, "rse", [128, 4], F32); rseB = S.buf("rse")
        h2n = sb(ep, "h2n", [128, D], BF16); h2nB = S.buf("h2n")
        h2nT = sb(ep, "h2nT", [128, D], BF16); h2nTB = S.buf("h2nT")
        pbf = sb(ep, "pbf", [128, 256], BF16); pbfB = S.buf("pbf")
        pTs = sb(ep, "pTs", [128, 256], BF16); pTsB = S.buf("pTs")
        gate = ot_; gateB = otB_
        junk3 = gate; junk3B = gateB
        def pass_loads(pq):
            bq = pq % 2
            S.dma("sp", rtp[bq][:], rt_s[:, :, pq * 256:(pq + 1) * 256].rearrange("w p t -> p w t"), [], [rtpB[bq]])

        def hne_load(pq):
            for j in range(2):
                S.dma("act", hne[0][:, j, :], hnT_s[2 * pq + j], [], [hneB[0]])

        def g_pq(pq, t4):
            bq = pq % 2
            for tt in range(4):
                t = 4 * t4 + tt
                r_ = t % NPQ
                self.dve(lambda e, r_=r_, t=t, bq=bq: e.tensor_scalar(out=Pq[r_][:], in0=iotaB[:], scalar1=rtp[bq][:, 0, t:t + 1],
                                                                     scalar2=rtp[bq][:, 2, t:t + 1], op0=ALU.is_equal, op1=ALU.mult),
                         [rtpB[bq], io2B], [PqB[r_]])
                self.dve(lambda e, r_=r_, t=t, bq=bq: e.tensor_scalar(out=Qq[r_][:], in0=iotaB[:], scalar1=rtp[bq][:, 1, t:t + 1],
                                                                     scalar2=None, op0=ALU.is_equal),
                         [rtpB[bq], io2B], [QqB[r_]])

        def g_mm(pq, t4):
            bq = pq % 2
            for tt in range(4):
                t = 4 * t4 + tt
                r_ = t % NPQ
                self.mm(pWG[:, tt * 128:(tt + 1) * 128], Qq[r_][:], Pq[r_][:], True, True,
                        [PqB[r_], QqB[r_]], [pWGB])
            self.act(GTs[bq][:, 4 * t4:4 * t4 + 4, :].rearrange("p t i -> p (t i)"), pWG[:], AF.Copy, [pWGB], [GTBs[bq]])

        def g_tokens(pq, t4):
            g_pq(pq, t4)
            g_mm(pq, t4)

        def epi(pq, sub):
            H = h1e[sub]; HB = h1eB[sub]
            ti = 2 * pq + sub
            rows_ = slice(ti * 128, (ti + 1) * 128)
            S.dma("act", pte[sub][:], p_in[rows_, :], [], [pteB[sub]])
            self.dve(lambda e: e.scalar_tensor_tensor(out=junk3[:], in0=H[:], scalar=1.0, in1=H[:], op0=ALU.mult, op1=ALU.mult,
                                                      accum_out=rse[:, 0:1]), [HB], [junk3B, rseB])
            yield
            self.rstd(rse[:, 1:2], rse[:, 0:1], 1.0 / D, [rseB], [rseB])
            yield
            self.act(h2n[:], H[:], AF.Copy, [HB, rseB], [h2nB], scale=rse[:, 1:2])
            yield
            for kc in range(8):
                self.tr(pWCb[:, kc * 128:(kc + 1) * 128], h2n[:, kc * 128:(kc + 1) * 128], identB[:], [h2nB], [pWCB])
            yield
            self.act(h2nT[:], pWCb[:, 0:1024], AF.Copy, [pWCB], [h2nTB])
            self.act(pbf[:], pte[sub][:], AF.Copy, [pteB[sub]], [pbfB])
            yield
            for kc in range(2):
                self.tr(pWDb[:, kc * 128:(kc + 1) * 128], pbf[:, kc * 128:(kc + 1) * 128], identB[:], [pbfB], [pWDB])
            yield
            self.dve(lambda e: e.tensor_copy(out=pTs[:], in_=pWDb[:, 0:256]), [pWDB], [pTsB])
            yield
            for half in range(2):
                hs = slice(half * 512, (half + 1) * 512)
                for kc in range(8):
                    self.mm(pWD[:], h2nT[:, kc * 128:(kc + 1) * 128], wg[:, kc, hs], kc == 0, kc == 7, [h2nTB, wgB], [pWDB])
                yield
                self.act(gate[:, hs], pWD[:], AF.Sigmoid, [pWDB], [gateB])
                yield
                for kc in range(2):
                    self.mm(pWC[:], pTs[:, kc * 128:(kc + 1) * 128], wpj[:, kc, hs], kc == 0, kc == 1, [pTsB, wpjB], [pWCB])
                yield
                self.dve(lambda e, hs=hs: e.tensor_tensor(out=gate[:, hs], in0=pWC[:], in1=gate[:, hs], op=ALU.mult), [pWCB, gateB], [gateB])
                yield
                self.dve(lambda e, hs=hs: e.tensor_tensor(out=H[:, hs], in0=H[:, hs], in1=gate[:, hs], op=ALU.add), [gateB, HB], [HB])
                yield
            self.dve(lambda e: e.scalar_tensor_tensor(out=junk3[:], in0=H[:], scalar=1.0, in1=H[:], op0=ALU.mult, op1=ALU.mult,
                                                      accum_out=rse[:, 2:3]), [HB], [junk3B, rseB])
            yield
            self.rstd(rse[:, 3:4], rse[:, 2:3], 1.0 / D, [rseB], [rseB])
            yield
            self.dve(lambda e: e.scalar_tensor_tensor(out=ot[sub][:], in0=H[:], scalar=rse[:, 3:4], in1=gfin[:],
                                                      op0=ALU.mult, op1=ALU.mult), [HB, rseB, gfinB], [otB[sub]])
            yield
            yield
            S.dma("sp", out[rows_, :], ot[sub][:], [otB[sub]], [])
            yield

        pend = []
        pass_loads(0)
        hne_load(0)
        for t4 in range(64):
            g_tokens(0, t4)
        for ps_ in range(T // 256):
            b2 = ps_ % 2
            GT = GTs[b2]; GTB = GTBs[b2]
            if ps_ + 1 < T // 256:
                pass_loads(ps_ + 1)
            def s_stage(blk):
                k = blk % NST
                S.dma("sp", ut[k][:], UT_s[blk], [], [utB[k]])
                S.dma("act", vt[k][:], V_s[blk], [], [vtB[k]])
                sb_ = blk % 2
                psx = pWS[:, sb_ * 512:sb_ * 512 + 256]
                for kc in range(8):
                    self.mm(psx.rearrange("p (j t) -> p j t", j=2), ut[k][:, kc * 128:(kc + 1) * 128], hne[b2][:, :, kc * 128:(kc + 1) * 128],
                            kc == 0, kc == 7, [utB[k], hneB[b2]], [pWSB[sb_]])
                self.act(gs[sb_][:], psx, AF.Gelu, [pWSB[sb_]], [gsB[sb_]])
                a3 = blk % 3
                self.dve(lambda e, sb_=sb_, a3=a3, blk=blk, GT=GT: e.tensor_tensor(out=aT[a3][:], in0=gs[sb_][:], in1=GT[:, :, blk], op=ALU.mult),
                         [gsB[sb_], GTB], [aTB[a3]])

            def v_stage(blk):
                k = blk % NST
                sb_ = blk % 2
                for sub in range(2):
                    for half in range(2):
                        self.mm(yacc[sub][:, half * 512:(half + 1) * 512], aT[blk % 3][:, sub * 128:(sub + 1) * 128],
                                vt[k][:, half * 512:(half + 1) * 512], blk == 0, blk == 127, [aTB[blk % 3], vtB[k]], [yaccB[sub]])

            nxt = ps_ + 1 < T // 256
            s_stage(0)
            if nxt:
                g_pq(ps_ + 1, 0)
            for blk in range(129):
                if blk >= 2 and blk % 2 == 0 and pend:
                    try:
                        next(pend[0])
                    except StopIteration:
                        pend.pop(0)
                if blk == 100:
                    for g_ in pend:
                        for _ in g_:
                            pass
                    pend = []
                    for sub in range(2):
                        ti_ = 2 * ps_ + sub
                        S.dma("sp", h1e[sub][:], h1_s[ti_ * 128:(ti_ + 1) * 128, :], [], [h1eB[sub]])
                if nxt and blk % 2 == 0 and blk // 2 + 1 < 64:
                    g_pq(ps_ + 1, blk // 2 + 1)
                if blk + 1 < 128:
                    s_stage(blk + 1)
                if blk == 126 and nxt:
                    hne_load(ps_ + 1)
                if blk >= 1:
                    v_stage(blk - 1)
                if nxt and blk % 2 == 1 and blk < 128:
                    g_mm(ps_ + 1, blk // 2)
            for sub in range(2):
                self.dve(lambda e, sub=sub: e.tensor_tensor(out=h1e[sub][:], in0=yacc[sub][:], in1=h1e[sub][:], op=ALU.add),
                         [yaccB[sub], h1eB[sub]], [h1eB[sub]])
            pend = [epi(ps_, 0), epi(ps_, 1)]
        for g_ in pend:
            for _ in g_:
                pass
        S.emit()
        ep.close()


_W_NAMES = ["ln_mix_g", "w_in", "s5_a_re", "s5_a_im", "s5_log_dt", "s5_b_re", "s5_b_im", "s5_c_re", "s5_c_im", "s5_d",
            "glu_w", "glu_b", "sgu_ln_g", "sgu_ln_b", "sgu_ws", "sgu_b", "out_norm_ssm", "out_norm_sgu", "w_out",
            "ln_ffn_g", "peer_wq", "peer_keys", "peer_u", "peer_v", "ple_norm_g", "ple_w_gate", "ple_w_proj", "final_g"]


def _run(inputs, dbg=None, cores=NCORES):
    k = K(dbg)
    k.build()
    w = {n: np.ascontiguousarray(np.asarray(inputs[n], dtype=np.float32)) for n in _W_NAMES}
    x = np.asarray(inputs["x"], dtype=np.float32).reshape(16 * SEQ, D)
    p = np.asarray(inputs["p"], dtype=np.float32).reshape(16 * SEQ, 256)
    in_maps = []
    for c in range(cores):
        m = {n: w[n] for n in k.din if n in w}
        m["x"] = np.ascontiguousarray(x[c * T:(c + 1) * T])
        m["p"] = np.ascontiguousarray(p[c * T:(c + 1) * T])
        in_maps.append({n: m[n] for n in k.din})
    res = run_bass_kernel_spmd(k.nc, in_maps, core_ids=list(range(cores)))
    return res


def kernel(**inputs):
    res = _run(inputs)
    outs = [np.asarray(r["out"], dtype=np.float32).reshape(NB, SEQ, D) for r in res.results]
    return np.concatenate(outs, axis=0)
```

```python
import math
import numpy as np
import concourse.bass as bass
import concourse.mybir as mybir
from concourse.bass_utils import run_bass_kernel_spmd

F32 = mybir.dt.float32
BF16 = mybir.dt.bfloat16
U32 = mybir.dt.uint32
I32 = mybir.dt.int32
AF = mybir.ActivationFunctionType
ALU = mybir.AluOpType
AX = mybir.AxisListType

NCORES = 8
D = 1024
SEQ = 2048
NB = 2
T = NB * SEQ
NT = T // 128
EPS = 1e-6
TWO_PI = 2.0 * math.pi


class Buf:
    __slots__ = ("name", "lw", "rd")

    def __init__(self, name=""):
        self.name = name
        self.lw = None
        self.rd = []


class Op:
    __slots__ = ("eng", "fn", "deps", "dma", "sem", "val", "sig", "sigidx", "prev_same_sem")

    def __init__(self, eng, fn, dma):
        self.eng = eng
        self.fn = fn
        self.deps = []
        self.dma = dma
        self.sem = None
        self.val = 0
        self.sig = False
        self.sigidx = 0
        self.prev_same_sem = 0


class Sched:
    ENGS = ("pe", "act", "dve", "pool", "sp")
    NDMA = 12
    NOSYNC = ("pe",)

    def __init__(self, nc, stack):
        self.nc = nc
        self.ops = []
        self.esem = {e: stack.enter_context(nc.semaphore("es_" + e)) for e in ("pe", "act", "dve", "pool")}
        self.ecount = {e: 0 for e in self.esem}
        self.dsem = {q: [stack.enter_context(nc.semaphore("ds_%s%d" % (q, i))) for i in range(self.NDMA)]
                     for q in ("sp", "act", "pool")}
        self.dcount = {q: 0 for q in self.dsem}
        self.waited = {e: {} for e in self.ENGS}
        self.phase = 0
        self.bufs = []

    def buf(self, name=""):
        b = Buf(name)
        self.bufs.append(b)
        return b

    def bufs_n(self, n, name=""):
        return [self.buf(name + str(i)) for i in range(n)]

    def _track(self, op, reads, writes):
        deps = []
        for b in reads:
            if b.lw is not None:
                deps.append(b.lw)
        for b in writes:
            if b.lw is not None:
                deps.append(b.lw)
            deps.extend(b.rd)
        for b in reads:
            b.rd.append(op)
        for b in writes:
            b.lw = op
            b.rd = []
        seen = set()
        for d in deps:
            if d is op or id(d) in seen:
                continue
            seen.add(id(d))
            if (not d.dma) and (not op.dma) and d.eng == op.eng and d.eng in self.NOSYNC:
                continue
            op.deps.append(d)
            d.sig = True

    def add(self, eng, fn, reads=(), writes=()):
        op = Op(eng, fn, False)
        self._track(op, reads, writes)
        self.ops.append(op)
        return op

    def dma(self, q, out, in_, reads=(), writes=(), **kw):
        op = Op(q, (lambda e, out=out, in_=in_, kw=kw: e.dma_start(out=out, in_=in_, **kw)), True)
        self._track(op, reads, writes)
        self.ops.append(op)
        return op

    def emit(self, name=None):
        nc = self.nc
        for op in self.ops:
            if op.dma:
                j = self.dcount[op.eng]
                self.dcount[op.eng] = j + 1
                op.sem = self.dsem[op.eng][j % self.NDMA]
                op.val = 16 * (j // self.NDMA + 1)
                op.prev_same_sem = 16 * (j // self.NDMA)
            elif op.sig:
                self.ecount[op.eng] += 1
                op.sem = self.esem[op.eng]
                op.val = self.ecount[op.eng]
        per = {e: [o for o in self.ops if o.eng == e] for e in self.ENGS}
        waited = self.waited
        dsem = self.dsem
        dcount = dict(self.dcount)
        NDMA = self.NDMA

        def run(e, ename):
            w = waited[ename]

            def wait(sem, val):
                k = sem.num
                if w.get(k, 0) >= val:
                    return
                w[k] = val
                e.wait_ge(sem, val)

            for op in per[ename]:
                for d in op.deps:
                    wait(d.sem, d.val)
                if op.dma:
                    if op.prev_same_sem:
                        wait(op.sem, op.prev_same_sem)
                    op.fn(e).then_inc(op.sem, 16)
                else:
                    ins = op.fn(e)
                    if op.sig:
                        ins.then_inc(op.sem, 1)
            if ename in dsem:
                n = dcount[ename]
                for i in range(NDMA):
                    cnt = (n - i + NDMA - 1) // NDMA if n > i else 0
                    if cnt:
                        wait(dsem[ename][i], 16 * cnt)

        with nc.Block() as block:
            @block.tensor
            def _(e):
                run(e, "pe")

            @block.scalar
            def _(e):
                run(e, "act")

            @block.vector
            def _(e):
                run(e, "dve")

            @block.gpsimd
            def _(e):
                run(e, "pool")

            @block.sync
            def _(e):
                run(e, "sp")
        self.ops = []
        self.phase += 1
        for b in self.bufs:
            b.lw = None
            b.rd = []


def _bc(ap, shape):
    return ap.to_broadcast(list(shape))


class K:
    def __init__(self, dbg=None):
        from contextlib import ExitStack
        self.dbg = dbg
        self.nc = nc = bass.Bass("TRN2", target_bir_lowering=False)
        self.root = ExitStack()
        self.S = Sched(nc, self.root)
        self.din = {}
        self.dq = 0

    def inp(self, name, shape):
        t = self.nc.dram_tensor(name, list(shape), F32, kind="ExternalInput").ap()
        self.din[name] = t
        return t

    def sb(self, stack, name, shape, dt=F32):
        return stack.enter_context(self.nc.sbuf_tensor(name, list(shape), dt))

    def ps(self, stack, name, shape, dt=F32):
        return stack.enter_context(self.nc.psum_tensor(name, list(shape), dt))

    def q(self):
        self.dq ^= 1
        return "sp" if self.dq else "act"

    def act(self, out, in_, func, reads, writes, **kw):
        return self.S.add("act", lambda e: e.activation(out=out, in_=in_, func=func, **kw), reads, writes)

    def mm(self, out, lhsT, rhs, start, stop, reads, writes, **kw):
        return self.S.add("pe", lambda e: e.matmul(out, lhsT, rhs, start=start, stop=stop, **kw), reads, writes)

    def tr(self, out, in_, ident, reads, writes):
        return self.S.add("pe", lambda e: e.transpose(out, in_, ident), reads, writes)

    def dve(self, fn, reads, writes):
        return self.S.add("dve", fn, reads, writes)

    def pool(self, fn, reads, writes):
        return self.S.add("pool", fn, reads, writes)

    def rstd(self, rstd, ss, scale, bufs_r, bufs_w):
        S = self.S
        self.dve(lambda e: e.tensor_scalar(out=rstd, in0=ss, scalar1=scale, scalar2=EPS, op0=ALU.mult, op1=ALU.add),
                 bufs_r, bufs_w)
        self.act(rstd, rstd, AF.Sqrt, bufs_w, bufs_w)
        self.dve(lambda e: e.reciprocal(out=rstd, in_=rstd), bufs_w, bufs_w)

    def load_weight(self, dst, dram2d, nk, n, gain, stage, stage_bufs, dst_buf, gain_buf):
        for kc in range(nk):
            st = stage[kc % 2]
            sbuf = stage_bufs[kc % 2]
            self.S.dma(self.q(), st[:, 0:n], dram2d[kc * 128:(kc + 1) * 128, :], [], [sbuf])
            if gain is None:
                self.dve(lambda e, st=st, kc=kc: e.tensor_copy(out=dst[:, kc, :], in_=st[:, 0:n]), [sbuf], [dst_buf])
            else:
                self.dve(lambda e, st=st, kc=kc: e.tensor_scalar(out=dst[:, kc, :], in0=st[:, 0:n],
                                                                 scalar1=gain[:, kc:kc + 1], scalar2=None,
                                                                 op0=ALU.mult),
                         [sbuf, gain_buf], [dst_buf])

    def sincos(self, ang, sin_o, cos_o, tmp, B):
        MAGIC = 12582912.0
        C1 = 6.28125
        C2 = TWO_PI - 6.28125
        d = self.dve
        d(lambda e: e.tensor_scalar(out=tmp, in0=ang, scalar1=1.0 / TWO_PI, scalar2=MAGIC, op0=ALU.mult, op1=ALU.add), B, B)
        d(lambda e: e.tensor_scalar(out=tmp, in0=tmp, scalar1=MAGIC, scalar2=None, op0=ALU.subtract), B, B)
        d(lambda e: e.scalar_tensor_tensor(out=ang, in0=tmp, scalar=-C1, in1=ang, op0=ALU.mult, op1=ALU.add), B, B)
        d(lambda e: e.scalar_tensor_tensor(out=ang, in0=tmp, scalar=-C2, in1=ang, op0=ALU.mult, op1=ALU.add), B, B)
        d(lambda e: e.tensor_scalar(out=ang, in0=ang, scalar1=math.pi, scalar2=-math.pi, op0=ALU.min, op1=ALU.max), B, B)
        self.act(sin_o, ang, AF.Sin, B, B)
        d(lambda e: e.scalar_tensor_tensor(out=tmp, in0=ang, scalar=-1.0, in1=ang, op0=ALU.mult, op1=ALU.max), B, B)
        self.act(cos_o, tmp, AF.Sin, B, B, scale=-1.0, bias=self.halfpi[:, 0:1])

    def build(self):
        from contextlib import ExitStack
        nc, S = self.nc, self.S
        inp = self.inp
        x = inp("x", [T, D])
        p_in = inp("p", [T, 256])
        ln_mix_g = inp("ln_mix_g", [1, D])
        w_in = inp("w_in", [1, D, 1536])
        a_re = inp("s5_a_re", [1, 32, 64]); a_im = inp("s5_a_im", [1, 32, 64])
        log_dt = inp("s5_log_dt", [1, 32])
        b_re = inp("s5_b_re", [1, 32, 64, 16]); b_im = inp("s5_b_im", [1, 32, 64, 16])
        c_re = inp("s5_c_re", [1, 32, 16, 64]); c_im = inp("s5_c_im", [1, 32, 16, 64])
        s5_d = inp("s5_d", [1, 512])
        glu_w = inp("glu_w", [1, 512, 512]); glu_b = inp("glu_b", [1, 512])
        sgu_ln_g = inp("sgu_ln_g", [1, 512]); sgu_ln_b = inp("sgu_ln_b", [1, 512])
        sgu_ws = inp("sgu_ws", [1, 4, 128, 128]); sgu_b = inp("sgu_b", [1, 4, 128])
        on_ssm = inp("out_norm_ssm", [1, 512]); on_sgu = inp("out_norm_sgu", [1, 512])
        w_out = inp("w_out", [1, D, D])
        ln_ffn_g = inp("ln_ffn_g", [1, D])
        peer_wq = inp("peer_wq", [1, D, 2048])
        peer_keys = inp("peer_keys", [1, 8, 2, 128, 128])
        peer_u = inp("peer_u", [1, 16384, D]); peer_v = inp("peer_v", [1, 16384, D])
        ple_norm_g = inp("ple_norm_g", [1, D])
        ple_w_gate = inp("ple_w_gate", [1, D, D]); ple_w_proj = inp("ple_w_proj", [1, 256, D])
        final_g = inp("final_g", [D])
        out = nc.dram_tensor("out", [T, D], F32, kind="ExternalOutput").ap()
        self.out = out
        dbg = self.dbg
        if dbg:
            self.dbg_out = {}

        def dbg_t(name, shape, dt=F32):
            t = nc.dram_tensor("dbg_" + name, list(shape), dt, kind="ExternalOutput").ap()
            self.dbg_out[name] = t
            return t

        h1_s = nc.dram_tensor("h1_s", [T, D], F32, kind="Internal").ap()
        hnT_s = nc.dram_tensor("hnT_s", [NT, 128, D], BF16, kind="Internal").ap()
        UT_s = nc.dram_tensor("UT_s", [128, 128, D], BF16, kind="Internal").ap()
        V_s = nc.dram_tensor("V_s", [128, 128, D], BF16, kind="Internal").ap()

        root = self.root
        sb, ps = self.sb, self.ps
        identF = sb(root, "identF", [128, 128], F32)
        identB = sb(root, "identB", [128, 128], BF16)
        onesB = sb(root, "onesB", [128, 128], BF16)
        self.halfpi = sb(root, "halfpi", [128, 1], F32)
        cB = S.buf("consts")
        S.add("pool", lambda e: e.memset(identF[:], 1.0), [], [cB])
        S.add("pool", lambda e: e.affine_select(out=identF[:], in_=identF[:], pattern=[[-1, 128]],
                                                compare_op=ALU.is_equal, fill=0.0, base=0, channel_multiplier=1), [cB], [cB])
        S.add("pool", lambda e: e.tensor_copy(out=identB[:], in_=identF[:]), [cB], [cB])
        S.add("pool", lambda e: e.memset(onesB[:], 1.0), [], [cB])
        S.add("pool", lambda e: e.memset(self.halfpi[:], math.pi / 2), [], [cB])
        S.emit()

        mixer = ExitStack()
        xsT_all = sb(mixer, "xsT_all", [128, 4, T], BF16)
        ygT_all = sb(mixer, "ygT_all", [128, 4, T], BF16)
        ss_sgu = sb(mixer, "ss_sgu", [128, NT], F32)
        xsB = S.bufs_n(4, "xsT")
        ygB = S.buf("ygT")
        ssgB = S.buf("ss_sgu")

        a1 = ExitStack()
        win = sb(a1, "win", [128, 8, 1536], BF16)
        stage = [sb(a1, "stage%d" % i, [128, 1536], F32) for i in range(2)]
        stageB = S.bufs_n(2, "stage")
        gmix = sb(a1, "gmix", [128, 8], F32)
        winB = S.buf("win"); gmixB = S.buf("gmix")
        S.dma("sp", gmix[:], ln_mix_g.rearrange("o (kc p) -> p (o kc)", p=128), [], [gmixB], allow_slow_non_contiguous=True)
        self.load_weight(win, w_in[0], 8, 1536, gmix, stage, stageB, winB, gmixB)
        wsmT = sb(a1, "wsmT", [128, 4, 128], BF16)
        Rt = sb(a1, "Rt", [128, 4, 128], F32)
        sg_g = sb(a1, "sg_g", [128, 4], F32)
        sg_b = sb(a1, "sg_b", [128, 4], F32)
        bs1 = sb(a1, "bs1", [1, 512], F32)
        bs1b = sb(a1, "bs1b", [1, 512], BF16)
        sguB = S.buf("sgu_consts")
        ws_t = sb(a1, "ws_t", [128, 4, 128], F32)
        ws_tB = S.buf("ws_t")
        pA = [ps(a1, "pA%d" % i, [128, 512], F32) for i in range(6)]
        pAB = S.bufs_n(6, "pA")
        pT = ps(a1, "pT", [128, 1024], BF16)
        pTB = S.buf("pT")
        S.dma("sp", sg_g[:], sgu_ln_g.rearrange("o (h d) -> d (o h)", d=128), [], [sguB], allow_slow_non_contiguous=True)
        S.dma("sp", sg_b[:], sgu_ln_b.rearrange("o (h d) -> d (o h)", d=128), [], [sguB], allow_slow_non_contiguous=True)
        S.dma("sp", bs1[:], sgu_b.rearrange("o h t -> o (h t)"), [], [sguB])
        for h in range(4):
            S.dma("act", ws_t[:, h, :], sgu_ws[0, h], [], [ws_tB])
        S.add("pool", lambda e: e.affine_select(out=ws_t[:], in_=ws_t[:], pattern=[[0, 4], [-1, 128]],
                                                compare_op=ALU.is_ge, fill=0.0, base=0, channel_multiplier=1),
              [ws_tB], [ws_tB])
        for h in range(4):
            self.tr(pA[0][:, h * 128:(h + 1) * 128], ws_t[:, h, :], identF[:], [ws_tB], [pAB[0]])
        self.dve(lambda e: e.tensor_copy(out=wsmT[:].rearrange("p h t -> p (h t)"), in_=pA[0][:]), [pAB[0]], [sguB])
        self.dve(lambda e: e.tensor_copy(out=bs1b[:], in_=bs1[:]), [sguB], [sguB])
        self.mm(pA[1][:], onesB[:], wsmT[:].rearrange("p h t -> p (h t)"), True, True, [sguB], [pAB[1]])
        self.mm(pA[2][:], onesB[0:1, :], bs1b[:], True, True, [sguB], [pAB[2]])
        self.dve(lambda e: e.tensor_copy(out=Rt[:].rearrange("p h t -> p (h t)"), in_=pA[2][:]), [pAB[2]], [sguB])
        for h in range(4):
            self.dve(lambda e, h=h: e.scalar_tensor_tensor(out=Rt[:, h, :], in0=pA[1][:, h * 128:(h + 1) * 128],
                                                            scalar=sg_b[:, h:h + 1], in1=Rt[:, h, :],
                                                            op0=ALU.mult, op1=ALU.add),
                     [pAB[1], sguB], [sguB])

        xt = [sb(a1, "xt%d" % i, [128, D], F32) for i in range(2)]
        xtB = S.bufs_n(2, "xt")
        junk = sb(a1, "junk", [128, D], BF16)
        junkB = S.buf("junk")
        st1 = [sb(a1, "st1_%d" % i, [128, 2], F32) for i in range(2)]
        st1B = S.bufs_n(2, "st1")
        xnb = [sb(a1, "xnb%d" % i, [128, D], BF16) for i in range(2)]
        xnbB = S.bufs_n(2, "xnb")
        xnT = [sb(a1, "xnT%d" % i, [128, 8, 128], BF16) for i in range(2)]
        xnTB = S.bufs_n(2, "xnT")
        gu = [sb(a1, "gu%d" % i, [128, 4, 128], F32) for i in range(2)]
        guB = S.bufs_n(2, "gu")
        gv = [sb(a1, "gv%d" % i, [128, 512], F32) for i in range(2)]
        gvB = S.bufs_n(2, "gv")
        bst = sb(a1, "bst", [128, 4, 6], F32); mv = sb(a1, "mv", [128, 4, 2], F32); lrs = sb(a1, "lrs", [128, 4], F32)
        lnB = S.buf("lnstats")
        vn = [sb(a1, "vn%d" % i, [128, 512], BF16) for i in range(2)]
        vnB = S.bufs_n(2, "vn")
        mt = sb(a1, "mt", [128, 4, 128], F32); mtB = S.buf("mt")
        ysq = sb(a1, "ysq", [128, 4, 128], BF16); ysqB = S.buf("ysq")

        NTB = 4
        ust = [sb(a1, "ust%d" % i, [128, D], F32) for i in range(NTB)]; ustB = S.bufs_n(NTB, "ust")
        vst = [sb(a1, "vst%d" % i, [128, D], F32) for i in range(NTB)]; vstB = S.bufs_n(NTB, "vst")
        ubf = [sb(a1, "ubf%d" % i, [128, D], BF16) for i in range(2)]; ubfB = S.bufs_n(2, "ubf")
        utb = [sb(a1, "utb%d" % i, [128, D], BF16) for i in range(3)]; utbB = S.bufs_n(3, "utb")
        vbf = [sb(a1, "vbf%d" % i, [128, D], BF16) for i in range(3)]; vbfB = S.bufs_n(3, "vbf")
        pTu = ps(a1, "pTu", [128, D], BF16); pTuB = S.buf("pTu")

        def t_load(tb_):
            if tb_ >= 128:
                return
            k4 = tb_ % NTB
            S.dma("sp", ust[k4][:], peer_u[0, tb_ * 128:(tb_ + 1) * 128, :], [], [ustB[k4]])
            S.dma("sp", vst[k4][:], peer_v[0, tb_ * 128:(tb_ + 1) * 128, :], [], [vstB[k4]])

        def t_block(tb_):
            t_load(tb_ + 2)
            k = tb_ % 2
            k4 = tb_ % NTB
            self.act(ubf[k][:], ust[k4][:], AF.Copy, [ustB[k4]], [ubfB[k]])
            for kc in range(8):
                self.tr(pTu[:, kc * 128:(kc + 1) * 128], ubf[k][:, kc * 128:(kc + 1) * 128], identB[:], [ubfB[k]], [pTuB])
            k3 = tb_ % 3
            self.dve(lambda e, k3=k3: e.tensor_copy(out=utb[k3][:], in_=pTu[:]), [pTuB], [utbB[k3]])
            S.add("pool", lambda e, k3=k3, k4=k4: e.tensor_copy(out=vbf[k3][:], in_=vst[k4][:]), [vstB[k4]], [vbfB[k3]])
            if tb_ >= 1:
                t_store(tb_ - 1)

        def t_store(tb_):
            k3 = tb_ % 3
            S.dma("act", UT_s[tb_], utb[k3][:], [utbB[k3]], [])
            S.dma("act", V_s[tb_], vbf[k3][:], [vbfB[k3]], [])

        t_load(0)
        t_load(1)
        def a1_front(i):
            b = i % 2
            cols = slice(i * 128, (i + 1) * 128)
            for tb_ in range(4 * i, 4 * i + 4):
                t_block(tb_)
            S.dma("sp", xt[b][:], x[i * 128:(i + 1) * 128, :], [], [xtB[b]])
            self.dve(lambda e, b=b: e.scalar_tensor_tensor(out=junk[:], in0=xt[b][:], scalar=1.0, in1=xt[b][:],
                                                           op0=ALU.mult, op1=ALU.mult, accum_out=st1[b][:, 0:1]),
                     [xtB[b]], [junkB, st1B[b]])
            self.rstd(st1[b][:, 1:2], st1[b][:, 0:1], 1.0 / D, [st1B[b]], [st1B[b]])
            self.act(xnb[b][:], xt[b][:], AF.Copy, [xtB[b], st1B[b]], [xnbB[b]], scale=st1[b][:, 1:2])
            for kc in range(8):
                self.tr(pT[:, kc * 128:(kc + 1) * 128], xnb[b][:, kc * 128:(kc + 1) * 128], identB[:], [xnbB[b]], [pTB])
            self.dve(lambda e, b=b: e.tensor_copy(out=xnT[b][:].rearrange("p k t -> p (k t)"), in_=pT[:]), [pTB], [xnTB[b]])

        def a1_back(i):
            b = i % 2
            cols = slice(i * 128, (i + 1) * 128)
            for c in range(4):
                for kc in range(8):
                    self.mm(pA[0][:, c * 128:(c + 1) * 128], win[:, kc, c * 128:(c + 1) * 128], xnT[b][:, kc, :],
                            kc == 0, kc == 7, [winB, xnTB[b]], [pAB[0]])
            self.act(xsT_all[:, :, cols], pA[0][:].rearrange("p (c t) -> p c t", c=4), AF.Copy, [pAB[0]], xsB)
            for c in range(4):
                for kc in range(8):
                    self.mm(pA[1][:, c * 128:(c + 1) * 128], win[:, kc, 512 + c * 128:512 + (c + 1) * 128], xnT[b][:, kc, :],
                            kc == 0, kc == 7, [winB, xnTB[b]], [pAB[1]])
            self.act(gu[b][:].rearrange("p c t -> p (c t)"), pA[1][:], AF.Gelu, [pAB[1]], [guB[b]])
            for kc in range(8):
                self.mm(pA[2][:], xnT[b][:, kc, :], win[:, kc, 1024:1536], kc == 0, kc == 7, [winB, xnTB[b]], [pAB[2]])
            self.act(gv[b][:], pA[2][:], AF.Gelu, [pAB[2]], [gvB[b]])
            for h in range(4):
                self.dve(lambda e, b=b, h=h: e.bn_stats(out=bst[:, h, :], in_=gv[b][:, h * 128:(h + 1) * 128]), [gvB[b]], [lnB])
            for h in range(4):
                self.dve(lambda e, h=h: e.bn_aggr(out=mv[:, h, :], in_=bst[:, h, :]), [lnB], [lnB])
            self.dve(lambda e: e.tensor_scalar(out=lrs[:], in0=mv[:, :, 1], scalar1=EPS, scalar2=None, op0=ALU.add), [lnB], [lnB])
            self.act(lrs[:], lrs[:], AF.Sqrt, [lnB], [lnB])
            self.dve(lambda e: e.reciprocal(out=lrs[:], in_=lrs[:]), [lnB], [lnB])
            for h in range(4):
                self.dve(lambda e, b=b, h=h: e.tensor_scalar(out=vn[b][:, h * 128:(h + 1) * 128], in0=gv[b][:, h * 128:(h + 1) * 128],
                                                             scalar1=mv[:, h, 0:1], scalar2=lrs[:, h:h + 1],
                                                             op0=ALU.subtract, op1=ALU.mult),
                         [gvB[b], lnB], [vnB[b]])
            for h in range(4):
                self.mm(pA[3][:, h * 128:(h + 1) * 128], vn[b][:, h * 128:(h + 1) * 128], wsmT[:, h, :], True, True,
                        [vnB[b], sguB], [pAB[3]])
            for h in range(4):
                self.dve(lambda e, h=h: e.scalar_tensor_tensor(out=mt[:, h, :], in0=pA[3][:, h * 128:(h + 1) * 128],
                                                                scalar=sg_g[:, h:h + 1], in1=Rt[:, h, :],
                                                                op0=ALU.mult, op1=ALU.add),
                         [pAB[3], sguB], [mtB])
            self.dve(lambda e, b=b, cols=cols: e.tensor_tensor(out=ygT_all[:, :, cols], in0=mt[:], in1=gu[b][:], op=ALU.mult),
                     [mtB, guB[b]], [ygB])
            self.act(ysq[:], ygT_all[:, :, cols], AF.Square, [ygB], [ysqB])
            for c in range(4):
                self.mm(pA[4][:, 0:1], ysq[:, c, :], onesB[:, 0:1], c == 0, c == 3, [ysqB], [pAB[4]])
            self.dve(lambda e, i=i: e.tensor_copy(out=ss_sgu[:, i:i + 1], in_=pA[4][:, 0:1]), [pAB[4]], [ssgB])
        a1_front(0)
        for i in range(NT):
            if i + 1 < NT:
                a1_front(i + 1)
            a1_back(i)
        t_store(127)
        if dbg == "a1":
            d1 = dbg_t("xsT", [128, 4, T], BF16); d2 = dbg_t("ygT", [128, 4, T], BF16); d3 = dbg_t("ss_sgu", [128, NT])
            S.dma("sp", d1, xsT_all[:], xsB, []); S.dma("sp", d2, ygT_all[:], [ygB], []); S.dma("sp", d3, ss_sgu[:], [ssgB], [])
        S.emit()
        a1.close()
        if dbg == "a1":
            mixer.close()
            return

        s5p = ExitStack()
        T0 = sb(s5p, "T0", [128, 32, 128], BF16)
        Wv = sb(s5p, "Wv", [128, 32, 2, 64], BF16)
        Wy = sb(s5p, "Wy", [128, 32, 2, 128], BF16)
        cosT = sb(s5p, "cosT", [128, 16, 256], F32)
        sinT = sb(s5p, "sinT", [128, 16, 256], F32)
        r8 = sb(s5p, "r8", [128, 16], F32)
        dtile = sb(s5p, "dtile", [128, 32], F32)
        s5cB = S.buf("s5consts")
        selB = S.buf("sel")
        pp = ExitStack()
        are = sb(pp, "are", [128, 16]); aim = sb(pp, "aim", [128, 16]); dtb = sb(pp, "dtb", [128, 16])
        lrdt = sb(pp, "lrdt", [128, 16]); ang1 = sb(pp, "ang1", [128, 16])
        braw = [sb(pp, "braw%d" % i, [128, 16, 16]) for i in range(2)]
        craw = [sb(pp, "craw%d" % i, [128, 16, 16]) for i in range(2)]
        tauI = sb(pp, "tauI", [128, 17], I32); tau = sb(pp, "tau", [128, 17])
        kI = sb(pp, "kI", [128, 256], I32); kF = sb(pp, "kF", [128, 256])
        E17 = sb(pp, "E17", [128, 16, 17]); A17 = sb(pp, "A17", [128, 16, 17]); tmp17 = sb(pp, "tmp17", [128, 16, 17])
        sn17 = sb(pp, "sn17", [128, 16, 17]); cs17 = sb(pp, "cs17", [128, 16, 17])
        pw = [sb(pp, "pw%d" % i, [128, 16, 17]) for i in range(2)]
        sm = [sb(pp, "sm%d" % i, [128, 16]) for i in range(8)]
        Bb = [sb(pp, "Bb%d" % i, [128, 16, 16]) for i in range(2)]
        tb = [sb(pp, "tb%d" % i, [128, 16, 16]) for i in range(2)]
        Xr = sb(pp, "Xr", [128, 8, 8, 16]); Xi = sb(pp, "Xi", [128, 8, 8, 16])
        Yr = sb(pp, "Yr", [128, 8, 8, 16]); Yi = sb(pp, "Yi", [128, 8, 8, 16])
        t1 = sb(pp, "t1", [128, 8, 8, 16]); t2 = sb(pp, "t2", [128, 8, 8, 16])
        mask = sb(pp, "mask", [128, 128])
        Zb = [sb(pp, "Zb%d" % i, [128, 8, 128], BF16) for i in range(2)]
        pSb = ps(pp, "pSb", [128, 2, 1024], BF16); pSbB = S.bufs_n(2, "pSb")
        phr = sb(pp, "phr", [128, 16]); pht = sb(pp, "pht", [128, 16])
        pS = [ps(pp, "pS%d" % i, [128, 512], F32) for i in range(2)]
        pSB = S.bufs_n(2, "pS")
        PB = S.buf("s5prep")
        XB = S.buf("X"); YB = S.buf("Y")
        NC = dict(allow_slow_non_contiguous=True)
        for par in range(2):
            rows = slice(par * 64, (par + 1) * 64)
            S.dma(self.q(), are[rows, :], a_re[0].rearrange("(gp par) p -> par p gp", par=2)[par], [], [PB], **NC)
            S.dma(self.q(), aim[rows, :], a_im[0].rearrange("(gp par) p -> par p gp", par=2)[par], [], [PB], **NC)
            S.dma(self.q(), dtb[rows, :], bass.AP(log_dt.tensor, par, [[0, 64], [2, 16]]), [], [PB], **NC)
            S.dma(self.q(), braw[0][rows], b_re[0].rearrange("(gp par) p c -> par p gp c", par=2)[par], [], [PB])
            S.dma(self.q(), braw[1][rows], b_im[0].rearrange("(gp par) p c -> par p gp c", par=2)[par], [], [PB])
            for gp in range(16):
                S.dma(self.q(), craw[0][rows, gp, :], c_re[0, 2 * gp + par].rearrange("c p -> p c"), [], [PB], **NC)
                S.dma(self.q(), craw[1][rows, gp, :], c_im[0, 2 * gp + par].rearrange("c p -> p c"), [], [PB], **NC)
        for t in range(8):
            S.dma(self.q(), dtile[16 * t:16 * (t + 1), :], s5_d[0].rearrange("(g c) -> c g", c=16), [], [s5cB], **NC)
        S.add("pool", lambda e: e.iota(tauI[:, 0:8], pattern=[[-1, 8]], base=0, channel_multiplier=0), [], [PB])
        S.add("pool", lambda e: e.iota(tauI[:, 8:17], pattern=[[1, 9]], base=0, channel_multiplier=0), [PB], [PB])
        S.add("pool", lambda e: e.iota(kI[:], pattern=[[1, 256]], base=0, channel_multiplier=0), [PB], [PB])
        S.add("pool", lambda e: e.memset(mask[:], 1.0), [PB], [PB])
        S.add("pool", lambda e: e.affine_select(out=mask[:], in_=mask[:], pattern=[[16, 8], [0, 16]], compare_op=ALU.is_ge,
                                                fill=0.0, base=15, channel_multiplier=-1), [PB], [PB])
        if dbg == "s5p0":
            S.dma("sp", dbg_t("craw", [128, 16, 16]), craw[0][:], [PB], [])
            S.dma("sp", dbg_t("dtb", [128, 16]), dtb[:], [PB], [])
            S.dma("sp", dbg_t("mask", [128, 128]), mask[:], [PB], [])
            S.dma("sp", dbg_t("dtile", [128, 32]), dtile[:], [s5cB], [])
            S.emit(); pp.close(); s5p.close(); mixer.close()
            return
        P1 = [PB]
        d = lambda fn: self.dve(fn, P1, P1)
        TT = lambda o, a, b, op: d(lambda e: e.tensor_tensor(out=o, in0=a, in1=b, op=op))
        d(lambda e: e.tensor_copy(out=tau[:], in_=tauI[:]))
        d(lambda e: e.tensor_copy(out=kF[:], in_=kI[:]))
        self.act(dtb[:], dtb[:], AF.Exp, P1, P1)
        TT(lrdt[:], are[:], dtb[:], ALU.mult)
        TT(ang1[:], aim[:], dtb[:], ALU.mult)
        s17 = [128, 16, 17]
        taub = _bc(tau[:].unsqueeze(1), s17)
        TT(E17[:], _bc(lrdt[:].unsqueeze(2), s17), taub, ALU.mult)
        self.act(E17[:], E17[:], AF.Exp, P1, P1)
        TT(A17[:], _bc(ang1[:].unsqueeze(2), s17), taub, ALU.mult)
        self.sincos(A17[:], sn17[:], cs17[:], tmp17[:], P1)
        TT(pw[0][:], E17[:], cs17[:], ALU.mult)
        TT(pw[1][:], E17[:], sn17[:], ALU.mult)
        d(lambda e: e.tensor_copy(out=r8[:], in_=E17[:, :, 16]))
        if dbg == "s5p1":
            S.dma("sp", dbg_t("pw0", [128, 16, 17]), pw[0][:], [PB], [])
            S.dma("sp", dbg_t("pw1", [128, 16, 17]), pw[1][:], [PB], [])
            S.emit(); pp.close(); s5p.close(); mixer.close()
            return
        abr, abi = pw[0][:, :, 9], pw[1][:, :, 9]
        nr, den, cre, cim, u1, u2 = [m[:] for m in sm[:6]]
        d(lambda e: e.tensor_scalar(out=nr, in0=abr, scalar1=-1.0, scalar2=None, op0=ALU.add))
        TT(den, are[:], are[:], ALU.mult); TT(u1, aim[:], aim[:], ALU.mult); TT(den, den, u1, ALU.add)
        d(lambda e: e.reciprocal(out=den, in_=den))
        TT(u1, nr, are[:], ALU.mult); TT(u2, abi, aim[:], ALU.mult); TT(cre, u1, u2, ALU.add); TT(cre, cre, den, ALU.mult)
        TT(u1, abi, are[:], ALU.mult); TT(u2, nr, aim[:], ALU.mult); TT(cim, u1, u2, ALU.subtract); TT(cim, cim, den, ALU.mult)
        s16 = [128, 16, 16]
        creb, cimb = _bc(cre.unsqueeze(2), s16), _bc(cim.unsqueeze(2), s16)
        TT(tb[0][:], creb, braw[0][:], ALU.mult); TT(tb[1][:], cimb, braw[1][:], ALU.mult); TT(Bb[0][:], tb[0][:], tb[1][:], ALU.subtract)
        TT(tb[0][:], creb, braw[1][:], ALU.mult); TT(tb[1][:], cimb, braw[0][:], ALU.mult); TT(Bb[1][:], tb[0][:], tb[1][:], ALU.add)
        s4 = [128, 8, 8, 16]

        def cmul(o_re, o_im, ar, ai, br, bi, neg_im=False, rw=P1):
            dd = lambda fn: self.dve(fn, rw, rw)
            dd(lambda e: e.tensor_tensor(out=t1[:], in0=ar, in1=br, op=ALU.mult))
            dd(lambda e: e.tensor_tensor(out=t2[:], in0=ai, in1=bi, op=ALU.mult))
            dd(lambda e: e.tensor_tensor(out=o_re, in0=t1[:], in1=t2[:], op=ALU.subtract))
            dd(lambda e: e.tensor_tensor(out=t1[:], in0=ar, in1=bi, op=ALU.mult))
            dd(lambda e: e.tensor_tensor(out=t2[:], in0=ai, in1=br, op=ALU.mult))
            if neg_im:
                dd(lambda e: e.scalar_tensor_tensor(out=o_im, in0=t1[:], scalar=-1.0, in1=t2[:], op0=ALU.mult, op1=ALU.subtract))
            else:
                dd(lambda e: e.tensor_tensor(out=o_im, in0=t1[:], in1=t2[:], op=ALU.add))

        RW = [PB, XB, YB, s5cB]
        self.dve(lambda e: e.memset(Wy[:], 0.0), RW, RW)
        for hf in range(2):
            gs = slice(8 * hf, 8 * hf + 8)
            Nre = _bc(pw[0][:, gs, 0:8].unsqueeze(3), s4); Nim = _bc(pw[1][:, gs, 0:8].unsqueeze(3), s4)
            Pre = _bc(pw[0][:, gs, 8:16].unsqueeze(3), s4); Pim = _bc(pw[1][:, gs, 8:16].unsqueeze(3), s4)
            P1re = _bc(pw[0][:, gs, 9:17].unsqueeze(3), s4); P1im = _bc(pw[1][:, gs, 9:17].unsqueeze(3), s4)
            Bre = _bc(Bb[0][:, gs].unsqueeze(2), s4); Bim = _bc(Bb[1][:, gs].unsqueeze(2), s4)
            Cre = _bc(craw[0][:, gs].unsqueeze(2), s4); Cim = _bc(craw[1][:, gs].unsqueeze(2), s4)
            cmul(Xr[:], Xi[:], Nre, Nim, Bre, Bim, rw=RW)
            cmul(Yr[:], Yi[:], Pre, Pim, Cre, Cim, neg_im=True, rw=RW)
            if dbg == "s5p2":
                S.dma("sp", dbg_t("Xr", [128, 8, 8, 16]), Xr[:], RW, [])
                S.dma("sp", dbg_t("Yi", [128, 8, 8, 16]), Yi[:], RW, [])
                S.emit(); pp.close(); s5p.close(); mixer.close()
                return
            for par in range(2):
                rows = slice(par * 64, (par + 1) * 64)
                for r in range(2):
                    for gi in range(4):
                        gp = 4 * r + gi
                        self.mm(pS[par][:, gi * 128:(gi + 1) * 128], Xr[rows, gp].rearrange("p s c -> p (s c)"),
                                Yr[rows, gp].rearrange("p s c -> p (s c)"), True, False, RW, [pSB[par]])
                        self.mm(pS[par][:, gi * 128:(gi + 1) * 128], Xi[rows, gp].rearrange("p s c -> p (s c)"),
                                Yi[rows, gp].rearrange("p s c -> p (s c)"), False, True, RW, [pSB[par]])
                    gsl2 = slice(16 * hf + 8 * r + par, 16 * hf + 8 * r + 8, 2)
                    self.dve(lambda e, par=par, gsl2=gsl2: e.tensor_tensor(out=T0[:, gsl2, :], in0=pS[par][:].rearrange("p (g m) -> p g m", g=4),
                                                                          in1=_bc(mask[:].unsqueeze(1), [128, 4, 128]), op=ALU.mult),
                             [pSB[par], PB], [s5cB])
            if dbg == "s5p3":
                S.dma("sp", dbg_t("T0", [128, 32, 128], BF16), T0[:], RW, [])
                S.emit(); pp.close(); s5p.close(); mixer.close()
                return
            s3 = [128, 8, 128]
            P7re = _bc(pw[0][:, gs, 15:16], s3); P7im = _bc(pw[1][:, gs, 15:16], s3)
            t1v, t2v = t1[:].rearrange("p g s c -> p g (s c)"), t2[:].rearrange("p g s c -> p g (s c)")
            Xrv, Xiv = Xr[:].rearrange("p g s c -> p g (s c)"), Xi[:].rearrange("p g s c -> p g (s c)")
            Zr, Zi = Zb[0][:], Zb[1][:]
            dz = lambda fn: self.dve(fn, RW, RW)
            dz(lambda e, t1v=t1v, P7re=P7re, Xrv=Xrv: e.tensor_tensor(out=t1v, in0=P7re, in1=Xrv, op=ALU.mult))
            dz(lambda e, t2v=t2v, P7im=P7im, Xiv=Xiv: e.tensor_tensor(out=t2v, in0=P7im, in1=Xiv, op=ALU.mult))
            dz(lambda e, Zr=Zr, t1v=t1v, t2v=t2v: e.tensor_tensor(out=Zr, in0=t1v, in1=t2v, op=ALU.subtract))
            dz(lambda e, t1v=t1v, P7re=P7re, Xiv=Xiv: e.tensor_tensor(out=t1v, in0=P7re, in1=Xiv, op=ALU.mult))
            dz(lambda e, t2v=t2v, P7im=P7im, Xrv=Xrv: e.tensor_tensor(out=t2v, in0=P7im, in1=Xrv, op=ALU.mult))
            dz(lambda e, Zi=Zi, t1v=t1v, t2v=t2v: e.tensor_tensor(out=Zi, in0=t1v, in1=t2v, op=ALU.add))
            for par in range(2):
                rows = slice(par * 64, (par + 1) * 64)
                for r in range(2):
                    for gi in range(4):
                        gp = 4 * r + gi
                        self.tr(pSb[:, par, gi * 128:gi * 128 + 64], Zb[0][rows, gp, :], identB[rows, rows], RW, [pSbB[par]])
                        self.tr(pSb[:, par, gi * 128 + 64:(gi + 1) * 128], Zb[1][rows, gp, :], identB[rows, rows], RW, [pSbB[par]])
                    gsl2 = slice(16 * hf + 8 * r + par, 16 * hf + 8 * r + 8, 2)
                    self.dve(lambda e, par=par, gsl2=gsl2: e.tensor_copy(out=Wv[:, gsl2], in_=pSb[:, par, 0:512].rearrange("p (g r m) -> p g r m", g=4, r=2)),
                             [pSbB[par]], [s5cB])
            P1re = _bc(pw[0][:, gs, 9:17].unsqueeze(3), s4); P1im = _bc(pw[1][:, gs, 9:17].unsqueeze(3), s4)
            cmul(Xr[:], Xi[:], P1re, P1im, Cre, Cim, neg_im=True, rw=RW)
            for par in range(2):
                rows = slice(par * 64, (par + 1) * 64)
                gsl2 = slice(16 * hf + par, 16 * hf + 16, 2)
                for ri, src in enumerate((Xr, Xi)):
                    self.dve(lambda e, rows=rows, gsl2=gsl2, ri=ri, src=src: e.tensor_copy(
                        out=Wy[rows, gsl2, ri, :], in_=src[rows].rearrange("p g s c -> p g (s c)")), RW, RW)
        d(lambda e: e.tensor_scalar(out=phr[:], in0=ang1[:], scalar1=8.0, scalar2=None, op0=ALU.mult))
        MAGIC = 12582912.0
        d(lambda e: e.tensor_scalar(out=pht[:], in0=phr[:], scalar1=1.0 / TWO_PI, scalar2=MAGIC, op0=ALU.mult, op1=ALU.add))
        d(lambda e: e.tensor_scalar(out=pht[:], in0=pht[:], scalar1=MAGIC, scalar2=None, op0=ALU.subtract))
        d(lambda e: e.scalar_tensor_tensor(out=phr[:], in0=pht[:], scalar=-6.28125, in1=phr[:], op0=ALU.mult, op1=ALU.add))
        d(lambda e: e.scalar_tensor_tensor(out=phr[:], in0=pht[:], scalar=-(TWO_PI - 6.28125), in1=phr[:], op0=ALU.mult, op1=ALU.add))
        sh = [128, 4, 256]
        angb = t1[:].rearrange("p g s c -> p (g s c)").rearrange("p (a k) -> p a k", k=256)
        tmpb = t2[:].rearrange("p g s c -> p (g s c)").rearrange("p (a k) -> p a k", k=256)
        for hf in range(4):
            gsl = slice(4 * hf, 4 * hf + 4)
            self.dve(lambda e, gsl=gsl: e.tensor_tensor(out=angb, in0=_bc(phr[:, gsl].unsqueeze(2), sh),
                                                        in1=_bc(kF[:].unsqueeze(1), sh), op=ALU.mult), RW, RW)
            self.sincos(angb, sinT[:, gsl, :], cosT[:, gsl, :], tmpb, RW)
        if dbg == "s5p":
            for nm, tt, dt_ in (("T0", T0, BF16), ("Wv", Wv, BF16), ("Wy", Wy, BF16), ("cosT", cosT, F32), ("sinT", sinT, F32)):
                S.dma("sp", dbg_t(nm, list(tt[:].shape), dt_), tt[:], [s5cB, PB], [])
            S.dma("sp", dbg_t("pw0", [128, 16, 17]), pw[0][:], [PB], [])
            S.dma("sp", dbg_t("pw1", [128, 16, 17]), pw[1][:], [PB], [])
        S.emit()
        pp.close()
        if dbg == "s5p":
            s5p.close(); mixer.close()
            return

        sm_ = ExitStack()
        Sel = sb(sm_, "Sel", [128, 8, 8, 128], BF16)
        SelO = sb(sm_, "SelO", [128, 8, 8, 128], BF16)
        S.add("pool", lambda e: e.memset(Sel[:], 0.0), [], [selB])
        S.add("pool", lambda e: e.memset(SelO[:], 0.0), [], [selB])
        idv = identB[:].rearrange("p (g c) -> p g c", c=16)
        for s_ in range(8):
            S.add("pool", lambda e, s_=s_: e.tensor_copy(out=Sel[:, :, s_, 16 * s_:16 * (s_ + 1)], in_=idv), [selB], [selB])
            S.add("pool", lambda e, s_=s_: e.tensor_copy(out=SelO[:, s_, :, 16 * s_:16 * (s_ + 1)], in_=idv), [selB], [selB])
        ublk = [[sb(sm_, "ublk%d%d" % (i, j), [128, 512], BF16) for j in range(2)] for i in range(2)]
        ublkB = [S.bufs_n(2, "ublk%d" % i) for i in range(2)]
        yblk = sb(sm_, "yblk", [128, 8, 512], BF16); yblkB = S.bufs_n(8, "yblk")
        Gm = [sb(sm_, "Gm%d" % i, [128, 2, 256]) for i in range(2)]
        Sc = [sb(sm_, "Sc%d" % i, [128, 2, 256]) for i in range(2)]
        tq = [sb(sm_, "tq%d" % i, [128, 2, 256]) for i in range(2)]
        Hp = [sb(sm_, "Hp%d" % i, [128, 2, 512], BF16) for i in range(2)]
        HpB = S.bufs_n(2, "Hp")
        wB = S.buf("s5work")
        pU = [ps(sm_, "pU%d" % i, [128, 512]) for i in range(2)]; pUB = S.bufs_n(2, "pU")
        pV = ps(sm_, "pV", [128, 2, 512]); pVB = S.buf("pV")
        pY = [ps(sm_, "pY%d" % i, [128, 512]) for i in range(2)]; pYB = S.bufs_n(2, "pY")
        pO = [ps(sm_, "pO%d" % i, [128, 512]) for i in range(2)]; pOB = S.bufs_n(2, "pO")
        self.dve(lambda e: e.memset(Sc[0][:], 0.0), [wB], [wB])
        self.dve(lambda e: e.memset(Sc[1][:], 0.0), [wB], [wB])
        def xv_of(c):
            return xsT_all[:, c, :].rearrange("p (b j s) -> p b j s", b=2, s=8)

        def st_R(gp):
            c = gp // 4; db = gp % 2; xv = xv_of(c)
            for par in range(2):
                g = 2 * gp + par
                gl = g % 8
                for s_ in range(8):
                    self.mm(pU[par][:], Sel[:, gl, s_, :], xv[:, :, :, s_], s_ == 0, s_ == 7, [selB, xsB[c]], [pUB[par]])
                self.act(ublk[db][par][:], pU[par][:], AF.Copy, [pUB[par]], [ublkB[db][par]])

        def st_V(gp):
            db = gp % 2
            for par in range(2):
                g = 2 * gp + par
                for ri in range(2):
                    self.mm(pV[par * 64:(par + 1) * 64, ri, :], Wv[:, g, ri, :], ublk[db][par][:], True, True,
                            [s5cB, ublkB[db][par]], [pVB])

        def dv(fn, r=()):
            self.dve(fn, [wB, s5cB] + list(r), [wB])

        def st_M(gp):
            cm = _bc(cosT[:, gp, 1:256].unsqueeze(1), [128, 2, 255]); sn = _bc(sinT[:, gp, 1:256].unsqueeze(1), [128, 2, 255])
            Vre = pV[:, 0, :].rearrange("p (b j) -> p b j", b=2)[:, :, 0:255]
            Vim = pV[:, 1, :].rearrange("p (b j) -> p b j", b=2)[:, :, 0:255]
            dv(lambda e: e.tensor_tensor(out=tq[0][:, :, 0:255], in0=Vre, in1=cm, op=ALU.mult), [pVB])
            dv(lambda e: e.tensor_tensor(out=tq[1][:, :, 0:255], in0=Vim, in1=sn, op=ALU.mult), [pVB])
            dv(lambda e: e.tensor_tensor(out=Gm[0][:, :, 0:255], in0=tq[0][:, :, 0:255], in1=tq[1][:, :, 0:255], op=ALU.add))
            dv(lambda e: e.tensor_tensor(out=tq[0][:, :, 0:255], in0=Vim, in1=cm, op=ALU.mult), [pVB])
            dv(lambda e: e.tensor_tensor(out=tq[1][:, :, 0:255], in0=Vre, in1=sn, op=ALU.mult), [pVB])
            dv(lambda e: e.tensor_tensor(out=Gm[1][:, :, 0:255], in0=tq[0][:, :, 0:255], in1=tq[1][:, :, 0:255], op=ALU.subtract))

        def st_SD(gp):
            db = gp % 2
            r8b = _bc(r8[:, gp:gp + 1], [128, 255])
            for ri in range(2):
                for b_ in range(2):
                    dv(lambda e, ri=ri, b_=b_: e.tensor_tensor_scan(out=Sc[ri][:, b_, 1:256], data0=r8b, data1=Gm[ri][:, b_, 0:255],
                                                                     initial=0.0, op0=ALU.mult, op1=ALU.add))
            cd = _bc(cosT[:, gp, :].unsqueeze(1), [128, 2, 256]); sd = _bc(sinT[:, gp, :].unsqueeze(1), [128, 2, 256])
            Hv = Hp[db][:].rearrange("p r (b j) -> p r b j", b=2)
            dv(lambda e: e.tensor_tensor(out=tq[0][:], in0=Sc[0][:], in1=cd, op=ALU.mult))
            dv(lambda e: e.tensor_tensor(out=tq[1][:], in0=Sc[1][:], in1=sd, op=ALU.mult))
            self.dve(lambda e: e.tensor_tensor(out=Hv[:, 0], in0=tq[0][:], in1=tq[1][:], op=ALU.subtract), [wB], [wB, HpB[db]])
            dv(lambda e: e.tensor_tensor(out=tq[0][:], in0=Sc[1][:], in1=cd, op=ALU.mult))
            dv(lambda e: e.tensor_tensor(out=tq[1][:], in0=Sc[0][:], in1=sd, op=ALU.mult))
            self.dve(lambda e: e.tensor_tensor(out=Hv[:, 1], in0=tq[0][:], in1=tq[1][:], op=ALU.add), [wB], [wB, HpB[db]])

        def st_Y(gp):
            db = gp % 2
            for par in range(2):
                g = 2 * gp + par
                gl = g % 8
                self.mm(pY[par][:], T0[:, g, :], ublk[db][par][:], True, False, [s5cB, ublkB[db][par]], [pYB[par]])
                self.mm(pY[par][:], Wy[:, g, 0, :], Hp[db][:, 0, :], False, False, [s5cB, HpB[db]], [pYB[par]])
                self.mm(pY[par][:], Wy[:, g, 1, :], Hp[db][:, 1, :], False, True, [s5cB, HpB[db]], [pYB[par]])
                self.dve(lambda e, par=par, g=g, gl=gl: e.scalar_tensor_tensor(
                    out=yblk[:, gl, :], in0=ublk[db][par][:], scalar=dtile[:, g:g + 1], in1=pY[par][:], op0=ALU.mult, op1=ALU.add),
                    [ublkB[db][par], pYB[par], s5cB], [yblkB[gl]])

        def st_O(c):
            xv = xv_of(c)
            for t_ in range(8):
                po = pO[t_ % 2]; poB = pOB[t_ % 2]
                for gl in range(8):
                    self.mm(po[:], SelO[:, gl, t_, :], yblk[:, gl, :], gl == 0, gl == 7, [selB, yblkB[gl]], [poB])
                self.act(xv[:, :, :, t_], po[:].rearrange("p (b j) -> p b j", b=2), AF.Gelu, [poB], [xsB[c]])

        st_R(0)
        st_V(0)
        for gp in range(16):
            st_M(gp)
            if gp + 1 < 16:
                st_R(gp + 1)
                st_V(gp + 1)
            st_SD(gp)
            st_Y(gp)
            if gp % 4 == 3:
                st_O(gp // 4)
        if dbg == "s5":
            S.dma("sp", dbg_t("gysT", [128, 4, T], BF16), xsT_all[:], xsB, [])
        S.emit()
        sm_.close(); s5p.close()
        if dbg == "s5":
            mixer.close()
            return

        a2 = ExitStack()
        wout = sb(a2, "wout", [128, 8, 1024], BF16); woutB = S.buf("wout")
        glub = sb(a2, "glub", [128, 4, 512], BF16); glubB = S.buf("glub")
        stage2 = [sb(a2, "stage2_%d" % i, [128, 1024], F32) for i in range(2)]; stage2B = S.bufs_n(2, "stage2")
        gout = sb(a2, "gout", [128, 8], F32); goutB = S.buf("gout")
        gb4 = sb(a2, "gb4", [128, 4], F32)
        gffn = sb(a2, "gffn", [128, D], F32); gffnB = S.buf("gffn")
        S.dma("sp", gout[:, 0:4], on_ssm.rearrange("o (kc p) -> p (o kc)", p=128), [], [goutB], **NC)
        S.dma("sp", gout[:, 4:8], on_sgu.rearrange("o (kc p) -> p (o kc)", p=128), [], [goutB], **NC)
        S.dma("sp", gb4[:], glu_b.rearrange("o (kc p) -> p (o kc)", p=128), [], [goutB], **NC)
        S.dma("act", gffn[:], bass.AP(ln_ffn_g.tensor, 0, [[0, 128], [1, D]]), [], [gffnB])
        self.load_weight(wout, w_out[0], 8, 1024, gout, stage2, stage2B, woutB, goutB)
        self.load_weight(glub, glu_w[0], 4, 512, None, stage2, stage2B, glubB, None)
        pG = ps(a2, "pG", [128, 512]); pGB = S.buf("pG")
        pSS = ps(a2, "pSS", [128, 512]); pSSB = S.buf("pSS")
        pA2 = [ps(a2, "pA2_%d" % i, [128, 1024]) for i in range(2)]; pA2B = S.bufs_n(2, "pA2")
        pT2 = ps(a2, "pT2", [128, 1024], BF16); pT2B = S.buf("pT2")
        sg = sb(a2, "sg", [128, 4, 128], F32); sgB = S.buf("sg")
        ysg = [sb(a2, "ysg%d" % i, [128, 4, 128], BF16) for i in range(2)]; ysgB = S.bufs_n(2, "ysg")
        ysq2 = sb(a2, "ysq2", [128, 4, 128], BF16); ysq2B = S.buf("ysq2")
        rs = [sb(a2, "rs%d" % i, [128, 4], F32) for i in range(2)]; rsB = S.bufs_n(2, "rs")
        xt2 = [sb(a2, "xt2_%d" % i, [128, D], F32) for i in range(2)]; xt2B = S.bufs_n(2, "xt2")
        h1t = [sb(a2, "h1t%d" % i, [128, D], F32) for i in range(3)]; h1B = S.bufs_n(3, "h1t")
        junk2 = sb(a2, "junk2", [128, D], BF16); junk2B = S.buf("junk2")
        hnb = [sb(a2, "hnb%d" % i, [128, D], BF16) for i in range(2)]; hnbB = S.bufs_n(2, "hnb")
        hnT = [sb(a2, "hnT%d" % i, [128, D], BF16) for i in range(3)]; hnTB = S.bufs_n(3, "hnT")
        def a2_front(i):
            b = i % 2
            cols = slice(i * 128, (i + 1) * 128)
            S.dma("sp", xt2[b][:], x[i * 128:(i + 1) * 128, :], [], [xt2B[b]])
            for c in range(4):
                for kc in range(4):
                    self.mm(pG[:, c * 128:(c + 1) * 128], glub[:, kc, c * 128:(c + 1) * 128], xsT_all[:, kc, cols],
                            kc == 0, kc == 3, [glubB, xsB[kc]], [pGB])
            for c in range(4):
                self.act(sg[:, c, :], pG[:, c * 128:(c + 1) * 128], AF.Sigmoid, [pGB, goutB], [sgB], bias=gb4[:, c:c + 1])
            self.dve(lambda e, b=b, cols=cols: e.tensor_tensor(out=ysg[b][:], in0=sg[:], in1=xsT_all[:, :, cols], op=ALU.mult),
                     [sgB] + xsB, [ysgB[b]])
            self.act(ysq2[:], ysg[b][:], AF.Square, [ysgB[b]], [ysq2B])
            for c in range(4):
                self.mm(pSS[:, 0:1], ysq2[:, c, :], onesB[:, 0:1], c == 0, c == 3, [ysq2B], [pSSB])
            self.dve(lambda e, b=b: e.tensor_copy(out=rs[b][:, 0:1], in_=pSS[:, 0:1]), [pSSB], [rsB[b]])
            self.dve(lambda e, b=b, i=i: e.tensor_copy(out=rs[b][:, 1:2], in_=ss_sgu[:, i:i + 1]), [ssgB], [rsB[b]])
            self.rstd(rs[b][:, 0:2], rs[b][:, 0:2], 1.0 / 512, [rsB[b]], [rsB[b]])

        def a2_back(i):
            b = i % 2
            c3 = i % 3
            cols = slice(i * 128, (i + 1) * 128)
            for half in range(2):
                hs = slice(half * 512, (half + 1) * 512)
                for kc in range(4):
                    self.mm(pA2[0][:, hs], ysg[b][:, kc, :], wout[:, kc, hs], kc == 0, kc == 3, [ysgB[b], woutB], [pA2B[0]])
            for half in range(2):
                hs = slice(half * 512, (half + 1) * 512)
                for kc in range(4):
                    self.mm(pA2[1][:, hs], ygT_all[:, kc, cols], wout[:, 4 + kc, hs], kc == 0, kc == 3, [ygB, woutB], [pA2B[1]])
            self.dve(lambda e, b=b: e.scalar_tensor_tensor(out=h1t[c3][:], in0=pA2[0][:], scalar=rs[b][:, 0:1], in1=xt2[b][:],
                                                           op0=ALU.mult, op1=ALU.add), [pA2B[0], rsB[b], xt2B[b]], [h1B[c3]])
            self.dve(lambda e, b=b: e.scalar_tensor_tensor(out=h1t[c3][:], in0=pA2[1][:], scalar=rs[b][:, 1:2], in1=h1t[c3][:],
                                                           op0=ALU.mult, op1=ALU.add), [pA2B[1], rsB[b], h1B[c3]], [h1B[c3]])
            self.dve(lambda e, b=b: e.scalar_tensor_tensor(out=junk2[:], in0=h1t[c3][:], scalar=1.0, in1=h1t[c3][:],
                                                           op0=ALU.mult, op1=ALU.mult, accum_out=rs[b][:, 2:3]),
                     [h1B[c3]], [junk2B, rsB[b]])
            self.rstd(rs[b][:, 3:4], rs[b][:, 2:3], 1.0 / D, [rsB[b]], [rsB[b]])
            self.dve(lambda e, b=b: e.scalar_tensor_tensor(out=hnb[b][:], in0=h1t[c3][:], scalar=rs[b][:, 3:4], in1=gffn[:],
                                                           op0=ALU.mult, op1=ALU.mult), [h1B[c3], rsB[b], gffnB], [hnbB[b]])

        def a2_back2(i):
            b = i % 2
            c3 = i % 3
            for kc in range(8):
                self.tr(pT2[:, kc * 128:(kc + 1) * 128], hnb[b][:, kc * 128:(kc + 1) * 128], identB[:], [hnbB[b]], [pT2B])
            self.act(hnT[c3][:], pT2[:], AF.Copy, [pT2B], [hnTB[c3]])
            if i >= 1:
                a2_store(i - 1)

        def a2_store(i):
            c3 = i % 3
            S.dma("act", h1_s[i * 128:(i + 1) * 128, :], h1t[c3][:], [h1B[c3]], [])
            S.dma("act", hnT_s[i], hnT[c3][:], [hnTB[c3]], [])
        a2_front(0)
        for i in range(NT):
            if i + 1 < NT:
                a2_front(i + 1)
            a2_back(i)
            if i >= 1:
                a2_back2(i - 1)
        a2_back2(NT - 1)
        a2_store(NT - 1)
        S.emit()
        a2.close(); mixer.close()
        if dbg == "a2":
            d1 = dbg_t("h1", [T, D])
            with nc.sbuf_tensor("dbgbuf", [128, D], F32) as dbuf:
                dB = S.buf("dbuf")
                for i in range(NT):
                    S.dma("sp", dbuf[:], h1_s[i * 128:(i + 1) * 128, :], [], [dB])
                    S.dma("sp", d1[i * 128:(i + 1) * 128, :], dbuf[:], [dB], [])
                S.emit()
            return

        rt_s = nc.dram_tensor("rt_s", [3, 128, T], F32, kind="Internal").ap()
        rp = ExitStack()
        wq = sb(rp, "wq", [128, 8, 2048], BF16); wqB = S.buf("wq")
        stq = [sb(rp, "stq%d" % i, [128, 2048], F32) for i in range(2)]; stqB = S.bufs_n(2, "stq")
        keysT = sb(rp, "keysT", [128, 16, 128], BF16); keysB = S.buf("keysT")
        kst = [sb(rp, "kst%d" % i, [128, 128], F32) for i in range(2)]; kstB = S.bufs_n(2, "kst")
        iota16 = sb(rp, "iota16", [128, 16], F32); iotaI = sb(rp, "iotaI", [128, 16], I32); ioB = S.buf("iota16")
        S.add("pool", lambda e: e.iota(iotaI[:], pattern=[[1, 16]], base=0, channel_multiplier=0), [], [ioB])
        S.add("pool", lambda e: e.tensor_copy(out=iota16[:], in_=iotaI[:]), [ioB], [ioB])
        self.load_weight(wq, peer_wq[0], 8, 2048, None, stq, stqB, wqB, None)
        pR = [ps(rp, "pR%d" % i, [128, 2048]) for i in range(2)]; pRB = S.bufs_n(2, "pR")
        for hk in range(16):
            S.dma(self.q(), kst[hk % 2][:], peer_keys[0, hk // 2, hk % 2], [], [kstB[hk % 2]])
            self.tr(pR[0][:, hk * 128:(hk + 1) * 128], kst[hk % 2][:], identF[:], [kstB[hk % 2]], [pRB[0]])
        self.dve(lambda e: e.tensor_copy(out=keysT[:].rearrange("p a n -> p (a n)"), in_=pR[0][:]), [pRB[0]], [keysB])
        hnp = [sb(rp, "hnp%d" % i, [128, 2, D], BF16) for i in range(2)]; hnpB = S.bufs_n(2, "hnp")
        qT = sb(rp, "qT", [128, 16, 256], BF16); qTB = S.buf("qT")
        def two(name, shape, dt=F32):
            return [sb(rp, "%s_%d" % (name, i), shape, dt) for i in range(2)]
        sc4 = [two("sc%d" % q_, [128, 16, 128]) for q_ in range(2)]; scB4 = [S.bufs_n(2, "sc%d_" % q_) for q_ in range(2)]
        scw2 = two("scw", [128, 16, 128])
        m82 = two("m8", [128, 16, 16]); ix2 = two("ix", [128, 16, 16], U32); ixf2 = two("ixf", [128, 16, 16])
        cand2 = two("cand", [128, 8, 256]); candw2 = two("candw", [128, 8, 256])
        c82 = two("c8", [128, 8, 16]); cix2 = two("cix", [128, 8, 16], U32)
        cia2 = two("cia", [128, 8, 16], U32); cib2 = two("cib", [128, 8, 16], U32)
        iaf2 = two("iaf", [128, 8, 16]); ibf2 = two("ibf", [128, 8, 16])
        oh2 = two("oh", [128, 8, 16, 16])
        res32 = two("res3", [128, 3, 8, 16])
        esum2 = two("esum", [128, 8])
        rtT = [sb(rp, "rtT%d" % i, [128, 3, 128], F32) for i in range(2)]; rtTB = S.bufs_n(2, "rtT")
        kB2 = S.bufs_n(2, "topk"); candB2 = S.bufs_n(2, "cand")
        tkB2 = [[S.bufs_n(5, "tk%d_%d_" % (j_, hk)) for hk in range(16)] for j_ in range(2)]
        tcB2 = [[S.bufs_n(5, "tc%d_%d_" % (j_, h)) for h in range(8)] for j_ in range(2)]
        s44 = [128, 8, 16, 16]

        def sub_tile(ps_, j):
            sc, scB, scw, m8, ix, ixf = sc4[ps_ % 2][j], scB4[ps_ % 2][j], scw2[j], m82[j], ix2[j], ixf2[j]
            cand, candw, c8, cix, cia, cib = cand2[j], candw2[j], c82[j], cix2[j], cia2[j], cib2[j]
            iaf, ibf, oh, res3, esum = iaf2[j], ibf2[j], oh2[j], res32[j], esum2[j]
            kB, candB, hB, cB_ = kB2[j], candB2[j], tkB2[j], tcB2[j]
            tile_i = 2 * ps_ + j
            for hk in range(16):
                self.dve(lambda e, hk=hk: e.max(out=m8[:, hk, 0:8], in_=sc[:, hk, :]), [scB], [hB[hk][0]])
                if hk % 4 == 3:
                    yield
            for hk in range(16):
                self.dve(lambda e, hk=hk: e.match_replace(out=scw[:, hk, :], in_to_replace=m8[:, hk, 0:8], in_values=sc[:, hk, :], imm_value=-1e30),
                         [scB, hB[hk][0]], [hB[hk][1]])
                if hk % 4 == 3:
                    yield
            for hk in range(16):
                self.dve(lambda e, hk=hk: e.max(out=m8[:, hk, 8:16], in_=scw[:, hk, :]), [hB[hk][1]], [hB[hk][2]])
                if hk % 4 == 3:
                    yield
            for hk in range(16):
                self.dve(lambda e, hk=hk: e.max_index(out=ix[:, hk, 0:8], in_max=m8[:, hk, 0:8], in_values=sc[:, hk, :]),
                         [scB, hB[hk][0]], [hB[hk][3]])
                if hk % 4 == 3:
                    yield
            for hk in range(16):
                self.dve(lambda e, hk=hk: e.max_index(out=ix[:, hk, 8:16], in_max=m8[:, hk, 8:16], in_values=sc[:, hk, :]),
                         [scB, hB[hk][2]], [hB[hk][4]])
                if hk % 4 == 3:
                    yield
            allh = [bb for hk in range(16) for bb in hB[hk]]
            self.dve(lambda e: e.tensor_copy(out=ixf[:], in_=ix[:]), allh, [kB])
            yield
            m8v = m8[:].rearrange("p (h k) n -> p h k n", k=2)
            ixv = ixf[:].rearrange("p (h k) n -> p h k n", k=2)
            self.dve(lambda e: e.tensor_tensor(out=cand[:].rearrange("p h (a b) -> p h a b", b=16),
                                               in0=_bc(m8v[:, :, 0, :].unsqueeze(3), s44), in1=_bc(m8v[:, :, 1, :].unsqueeze(2), s44), op=ALU.add),
                     allh, [candB])
            yield
            for h in range(8):
                self.dve(lambda e, h=h: e.max(out=c8[:, h, 0:8], in_=cand[:, h, :]), [candB], [cB_[h][0]])
                if h % 4 == 3:
                    yield
            for h in range(8):
                self.dve(lambda e, h=h: e.match_replace(out=candw[:, h, :], in_to_replace=c8[:, h, 0:8], in_values=cand[:, h, :], imm_value=-1e30),
                         [candB, cB_[h][0]], [cB_[h][1]])
                if h % 4 == 3:
                    yield
            for h in range(8):
                self.dve(lambda e, h=h: e.max(out=c8[:, h, 8:16], in_=candw[:, h, :]), [cB_[h][1]], [cB_[h][2]])
                if h % 4 == 3:
                    yield
            for h in range(8):
                self.dve(lambda e, h=h: e.max_index(out=cix[:, h, 0:8], in_max=c8[:, h, 0:8], in_values=cand[:, h, :]),
                         [candB, cB_[h][0]], [cB_[h][3]])
                if h % 4 == 3:
                    yield
            for h in range(8):
                self.dve(lambda e, h=h: e.max_index(out=cix[:, h, 8:16], in_max=c8[:, h, 8:16], in_values=cand[:, h, :]),
                         [candB, cB_[h][2]], [cB_[h][4]])
                if h % 4 == 3:
                    yield
            allc = [bb for h in range(8) for bb in cB_[h]]
            KB = [kB, scB] + allc

            def dk(fn):
                self.dve(fn, KB + [ioB], KB)
            g3 = res3[:, 2]
            dk(lambda e: e.tensor_tensor(out=g3, in0=c8[:], in1=_bc(c8[:, :, 0:1], [128, 8, 16]), op=ALU.subtract))
            yield
            self.act(g3, g3, AF.Exp, KB, KB)
            yield
            dk(lambda e: e.tensor_reduce(out=esum[:], in_=g3, axis=AX.X, op=ALU.add))
            yield
            dk(lambda e: e.reciprocal(out=esum[:], in_=esum[:]))
            yield
            dk(lambda e: e.tensor_tensor(out=g3, in0=g3, in1=_bc(esum[:].unsqueeze(2), [128, 8, 16]), op=ALU.mult))
            yield
            dk(lambda e: e.tensor_single_scalar(out=cia[:], in_=cix[:], scalar=4, op=ALU.logical_shift_right))
            yield
            dk(lambda e: e.tensor_single_scalar(out=cib[:], in_=cix[:], scalar=15, op=ALU.bitwise_and))
            yield
            dk(lambda e: e.tensor_copy(out=iaf[:], in_=cia[:]))
            yield
            dk(lambda e: e.tensor_copy(out=ibf[:], in_=cib[:]))
            yield
            io4 = _bc(iota16[:].unsqueeze(1).unsqueeze(1), s44)
            for w_, (sel_f, kk) in enumerate(((iaf, 0), (ibf, 1))):
                dk(lambda e, sel_f=sel_f: e.tensor_tensor(out=oh[:], in0=_bc(sel_f[:].unsqueeze(3), s44), in1=io4, op=ALU.is_equal))
                yield
                dk(lambda e, kk=kk: e.tensor_tensor(out=oh[:], in0=oh[:], in1=_bc(ixv[:, :, kk, :].unsqueeze(2), s44), op=ALU.mult))
                yield
                dk(lambda e, w_=w_: e.tensor_reduce(out=res3[:, w_], in_=oh[:], axis=AX.X, op=ALU.add))
                yield
            rb = tile_i % 2
            for w_ in range(3):
                self.tr(pR[j][:, w_ * 128:(w_ + 1) * 128], res3[:, w_].rearrange("p h k -> p (h k)"), identF[:], KB, [pRB[j]])
            self.act(rtT[rb][:].rearrange("p w t -> p (w t)"), pR[j][:, 0:384], AF.Copy, [pRB[j]], [rtTB[rb]])
            S.dma("sp", rt_s[:, :, tile_i * 128:(tile_i + 1) * 128].rearrange("w p t -> p w t"), rtT[rb][:], [rtTB[rb]], [])
            yield

        def r_front(ps_):
            b2 = ps_ % 2
            for j in range(2):
                S.dma("sp", hnp[b2][:, j, :], hnT_s[2 * ps_ + j], [], [hnpB[b2]])
            for j in range(2):
                for hk in range(16):
                    for kc in range(8):
                        self.mm(pR[j][:, hk * 128:(hk + 1) * 128], wq[:, kc, hk * 128:(hk + 1) * 128],
                                hnp[b2][:, j, kc * 128:(kc + 1) * 128], kc == 0, kc == 7, [wqB, hnpB[b2]], [pRB[j]])
            for j in range(2):
                self.act(qT[:, :, j * 128:(j + 1) * 128], pR[j][:].rearrange("p (a t) -> p a t", a=16), AF.Copy, [pRB[j]], [qTB])
            for j in range(2):
                for hk in range(16):
                    self.mm(pR[j][:, hk * 128:(hk + 1) * 128], qT[:, hk, j * 128:(j + 1) * 128], keysT[:, hk, :], True, True,
                            [qTB, keysB], [pRB[j]])
                self.act(sc4[b2][j][:].rearrange("p a n -> p (a n)"), pR[j][:], AF.Copy, [pRB[j]], [scB4[b2][j]])

        r_front(0)
        for ps_ in range(T // 256):
            if ps_ + 1 < T // 256:
                r_front(ps_ + 1)
            gens = [sub_tile(ps_, 0), sub_tile(ps_, 1)]
            live = [True, True]
            while any(live):
                for gi_ in range(2):
                    if live[gi_]:
                        try:
                            next(gens[gi_])
                        except StopIteration:
                            live[gi_] = False
        if dbg == "r":
            dd = dbg_t("rt", [3, 128, T])
            with nc.sbuf_tensor("dbgbuf2", [128, 3, T], F32) as dbuf:
                dB = S.buf("dbuf2")
                S.emit()
                S.dma("sp", dbuf[:], rt_s.rearrange("w p t -> p w t"), [], [dB])
                S.dma("sp", dd.rearrange("w p t -> p w t"), dbuf[:], [dB], [])
                S.emit()
            rp.close()
            return
        S.emit()
        rp.close()

        ep = ExitStack()
        wg = sb(ep, "wg", [128, 8, 1024], BF16); wgB = S.buf("wg")
        wpj = sb(ep, "wpj", [128, 2, 1024], BF16); wpjB = S.buf("wpj")
        ot_ = sb(ep, "ot", [128, D], F32); ot = [ot_, ot_]; otB_ = S.buf("ot"); otB = [otB_, otB_]
        stE = ot; stEB = otB
        gpl4 = sb(ep, "gpl4", [128, 8], F32); gplB = S.buf("gpl4")
        gfin = sb(ep, "gfin", [128, D], F32); gfinB = S.buf("gfin")
        iotaB = sb(ep, "iotaB", [128, 128], BF16); iotaI2 = sb(ep, "iotaI2", [128, 128], I32); io2B = S.buf("iotaB")
        S.add("pool", lambda e: e.iota(iotaI2[:], pattern=[[1, 128]], base=0, channel_multiplier=0), [], [io2B])
        S.add("pool", lambda e: e.tensor_copy(out=iotaB[:], in_=iotaI2[:]), [io2B], [io2B])
        S.dma("sp", gpl4[:], ple_norm_g.rearrange("o (kc p) -> p (o kc)", p=128), [], [gplB], **NC)
        S.dma("act", gfin[:], bass.AP(final_g.tensor, 0, [[0, 128], [1, D]]), [], [gfinB])
        self.load_weight(wg, ple_w_gate[0], 8, 1024, gpl4, stE, stEB, wgB, gplB)
        self.load_weight(wpj, ple_w_proj[0], 2, 1024, None, stE, stEB, wpjB, None)
        GTs = [sb(ep, "GT%d" % i, [128, 256, 128], BF16) for i in range(2)]; GTBs = S.bufs_n(2, "GT")
        rtp = [sb(ep, "rtp%d" % i, [128, 3, 256], F32) for i in range(2)]; rtpB = S.bufs_n(2, "rtp")
        hne_ = sb(ep, "hne", [128, 2, D], BF16); hne = [hne_, hne_]; hneB_ = S.buf("hne"); hneB = [hneB_, hneB_]
        NPQ = 8
        Pq = [sb(ep, "Pq%d" % i, [128, 128], BF16) for i in range(NPQ)]; PqB = S.bufs_n(NPQ, "Pq")
        Qq = [sb(ep, "Qq%d" % i, [128, 128], BF16) for i in range(NPQ)]; QqB = S.bufs_n(NPQ, "Qq")
        NST = 4
        ut = [sb(ep, "ut%d" % i, [128, D], BF16) for i in range(NST)]; utB = S.bufs_n(NST, "ut")
        vt = [sb(ep, "vt%d" % i, [128, D], BF16) for i in range(NST)]; vtB = S.bufs_n(NST, "vt")
        gs = [sb(ep, "gs%d" % i, [128, 256], BF16) for i in range(2)]; gsB = S.bufs_n(2, "gs")
        aT = [sb(ep, "aT%d" % i, [128, 256], BF16) for i in range(3)]; aTB = S.bufs_n(3, "aT")
        yacc = [ps(ep, "yacc%d" % i, [128, 1024]) for i in range(2)]; yaccB = S.bufs_n(2, "yacc")
        pWS = ps(ep, "pWS", [128, 1024]); pWSB = S.bufs_n(2, "pWS")
        pWG = ps(ep, "pWG", [128, 512]); pWGB = S.buf("pWG")
        pWC = ps(ep, "pWC", [128, 512]); pWCB = S.buf("pWC")
        pWCb = pWC[:].bitcast(BF16)
        pWD, pWDB, pWDb = pWC, pWCB, pWCb
        h1e = [sb(ep, "h1e%d" % i, [128, D], F32) for i in range(2)]; h1eB = S.bufs_n(2, "h1e")
        pte_ = sb(ep, "pte", [128, 256], F32); pte = [pte_, pte_]; pteB_ = S.buf("pte"); pteB = [pteB_, pteB_]
        rse = sb(ep, "rse", [128, 4], F32); rseB = S.buf("rse")
        h2n = sb(ep, "h2n", [128, D], BF16); h2nB = S.buf("h2n")
        h2nT = sb(ep, "h2nT", [128, D], BF16); h2nTB = S.buf("h2nT")
        pbf = sb(ep, "pbf", [128, 256], BF16); pbfB = S.buf("pbf")
        pTs = sb(ep, "pTs", [128, 256], BF16); pTsB = S.buf("pTs")
        gate = ot_; gateB = otB_
        junk3 = gate; junk3B = gateB
        def pass_loads(pq):
            bq = pq % 2
            S.dma("sp", rtp[bq][:], rt_s[:, :, pq * 256:(pq + 1) * 256].rearrange("w p t -> p w t"), [], [rtpB[bq]])

        def hne_load(pq):
            for j in range(2):
                S.dma("act", hne[0][:, j, :], hnT_s[2 * pq + j], [], [hneB[0]])

        def g_pq(pq, t4):
            bq = pq % 2
            for tt in range(4):
                t = 4 * t4 + tt
                r_ = t % NPQ
                self.dve(lambda e, r_=r_, t=t, bq=bq: e.tensor_scalar(out=Pq[r_][:], in0=iotaB[:], scalar1=rtp[bq][:, 0, t:t + 1],
                                                                     scalar2=rtp[bq][:, 2, t:t + 1], op0=ALU.is_equal, op1=ALU.mult),
                         [rtpB[bq], io2B], [PqB[r_]])
                self.dve(lambda e, r_=r_, t=t, bq=bq: e.tensor_scalar(out=Qq[r_][:], in0=iotaB[:], scalar1=rtp[bq][:, 1, t:t + 1],
                                                                     scalar2=None, op0=ALU.is_equal),
                         [rtpB[bq], io2B], [QqB[r_]])

        def g_mm(pq, t4):
            bq = pq % 2
            for tt in range(4):
                t = 4 * t4 + tt
                r_ = t % NPQ
                self.mm(pWG[:, tt * 128:(tt + 1) * 128], Qq[r_][:], Pq[r_][:], True, True,
                        [PqB[r_], QqB[r_]], [pWGB])
            self.act(GTs[bq][:, 4 * t4:4 * t4 + 4, :].rearrange("p t i -> p (t i)"), pWG[:], AF.Copy, [pWGB], [GTBs[bq]])

        def g_tokens(pq, t4):
            g_pq(pq, t4)
            g_mm(pq, t4)

        def epi(pq, sub):
            H = h1e[sub]; HB = h1eB[sub]
            ti = 2 * pq + sub
            rows_ = slice(ti * 128, (ti + 1) * 128)
            S.dma("act", pte[sub][:], p_in[rows_, :], [], [pteB[sub]])
            self.dve(lambda e: e.scalar_tensor_tensor(out=junk3[:], in0=H[:], scalar=1.0, in1=H[:], op0=ALU.mult, op1=ALU.mult,
                                                      accum_out=rse[:, 0:1]), [HB], [junk3B, rseB])
            yield
            self.rstd(rse[:, 1:2], rse[:, 0:1], 1.0 / D, [rseB], [rseB])
            yield
            self.act(h2n[:], H[:], AF.Copy, [HB, rseB], [h2nB], scale=rse[:, 1:2])
            yield
            for kc in range(8):
                self.tr(pWCb[:, kc * 128:(kc + 1) * 128], h2n[:, kc * 128:(kc + 1) * 128], identB[:], [h2nB], [pWCB])
            yield
            self.act(h2nT[:], pWCb[:, 0:1024], AF.Copy, [pWCB], [h2nTB])
            self.act(pbf[:], pte[sub][:], AF.Copy, [pteB[sub]], [pbfB])
            yield
            for kc in range(2):
                self.tr(pWDb[:, kc * 128:(kc + 1) * 128], pbf[:, kc * 128:(kc + 1) * 128], identB[:], [pbfB], [pWDB])
            yield
            self.dve(lambda e: e.tensor_copy(out=pTs[:], in_=pWDb[:, 0:256]), [pWDB], [pTsB])
            yield
            for half in range(2):
                hs = slice(half * 512, (half + 1) * 512)
                for kc in range(8):
                    self.mm(pWD[:], h2nT[:, kc * 128:(kc + 1) * 128], wg[:, kc, hs], kc == 0, kc == 7, [h2nTB, wgB], [pWDB])
                yield
                self.act(gate[:, hs], pWD[:], AF.Sigmoid, [pWDB], [gateB])
                yield
                for kc in range(2):
                    self.mm(pWC[:], pTs[:, kc * 128:(kc + 1) * 128], wpj[:, kc, hs], kc == 0, kc == 1, [pTsB, wpjB], [pWCB])
                yield
                self.dve(lambda e, hs=hs: e.tensor_tensor(out=gate[:, hs], in0=pWC[:], in1=gate[:, hs], op=ALU.mult), [pWCB, gateB], [gateB])
                yield
                self.dve(lambda e, hs=hs: e.tensor_tensor(out=H[:, hs], in0=H[:, hs], in1=gate[:, hs], op=ALU.add), [gateB, HB], [HB])
                yield
            self.dve(lambda e: e.scalar_tensor_tensor(out=junk3[:], in0=H[:], scalar=1.0, in1=H[:], op0=ALU.mult, op1=ALU.mult,
                                                      accum_out=rse[:, 2:3]), [HB], [junk3B, rseB])
            yield
            self.rstd(rse[:, 3:4], rse[:, 2:3], 1.0 / D, [rseB], [rseB])
            yield
            self.dve(lambda e: e.scalar_tensor_tensor(out=ot[sub][:], in0=H[:], scalar=rse[:, 3:4], in1=gfin[:],
                                                      op0=ALU.mult, op1=ALU.mult), [HB, rseB, gfinB], [otB[sub]])
            yield
            yield
            S.dma("sp", out[rows_, :], ot[sub][:], [otB[sub]], [])
            yield

        pend = []
        pass_loads(0)
        hne_load(0)
        for t4 in range(64):
            g_tokens(0, t4)
        for ps_ in range(T // 256):
            b2 = ps_ % 2
            GT = GTs[b2]; GTB = GTBs[b2]
            if ps_ + 1 < T // 256:
                pass_loads(ps_ + 1)
            def s_stage(blk):
                k = blk % NST
                S.dma("sp", ut[k][:], UT_s[blk], [], [utB[k]])
                S.dma("act", vt[k][:], V_s[blk], [], [vtB[k]])
                sb_ = blk % 2
                psx = pWS[:, sb_ * 512:sb_ * 512 + 256]
                for kc in range(8):
                    self.mm(psx.rearrange("p (j t) -> p j t", j=2), ut[k][:, kc * 128:(kc + 1) * 128], hne[b2][:, :, kc * 128:(kc + 1) * 128],
                            kc == 0, kc == 7, [utB[k], hneB[b2]], [pWSB[sb_]])
                self.act(gs[sb_][:], psx, AF.Gelu, [pWSB[sb_]], [gsB[sb_]])
                a3 = blk % 3
                self.dve(lambda e, sb_=sb_, a3=a3, blk=blk, GT=GT: e.tensor_tensor(out=aT[a3][:], in0=gs[sb_][:], in1=GT[:, :, blk], op=ALU.mult),
                         [gsB[sb_], GTB], [aTB[a3]])

            def v_stage(blk):
                k = blk % NST
                sb_ = blk % 2
                for sub in range(2):
                    for half in range(2):
                        self.mm(yacc[sub][:, half * 512:(half + 1) * 512], aT[blk % 3][:, sub * 128:(sub + 1) * 128],
                                vt[k][:, half * 512:(half + 1) * 512], blk == 0, blk == 127, [aTB[blk % 3], vtB[k]], [yaccB[sub]])

            nxt = ps_ + 1 < T // 256
            s_stage(0)
            if nxt:
                g_pq(ps_ + 1, 0)
            for blk in range(129):
                if blk >= 2 and blk % 2 == 0 and pend:
                    try:
                        next(pend[0])
                    except StopIteration:
                        pend.pop(0)
                if blk == 100:
                    for g_ in pend:
                        for _ in g_:
                            pass
                    pend = []
                    for sub in range(2):
                        ti_ = 2 * ps_ + sub
                        S.dma("sp", h1e[sub][:], h1_s[ti_ * 128:(ti_ + 1) * 128, :], [], [h1eB[sub]])
                if nxt and blk % 2 == 0 and blk // 2 + 1 < 64:
                    g_pq(ps_ + 1, blk // 2 + 1)
                if blk + 1 < 128:
                    s_stage(blk + 1)
                if blk == 126 and nxt:
                    hne_load(ps_ + 1)
                if blk >= 1:
                    v_stage(blk - 1)
                if nxt and blk % 2 == 1 and blk < 128:
                    g_mm(ps_ + 1, blk // 2)
            for sub in range(2):
                self.dve(lambda e, sub=sub: e.tensor_tensor(out=h1e[sub][:], in0=yacc[sub][:], in1=h1e[sub][:], op=ALU.add),
                         [yaccB[sub], h1eB[sub]], [h1eB[sub]])
            pend = [epi(ps_, 0), epi(ps_, 1)]
        for g_ in pend:
            for _ in g_:
                pass
        S.emit()
        ep.close()


_W_NAMES = ["ln_mix_g", "w_in", "s5_a_re", "s5_a_im", "s5_log_dt", "s5_b_re", "s5_b_im", "s5_c_re", "s5_c_im", "s5_d",
            "glu_w", "glu_b", "sgu_ln_g", "sgu_ln_b", "sgu_ws", "sgu_b", "out_norm_ssm", "out_norm_sgu", "w_out",
            "ln_ffn_g", "peer_wq", "peer_keys", "peer_u", "peer_v", "ple_norm_g", "ple_w_gate", "ple_w_proj", "final_g"]


def _run(inputs, dbg=None, cores=NCORES):
    k = K(dbg)
    k.build()
    w = {n: np.ascontiguousarray(np.asarray(inputs[n], dtype=np.float32)) for n in _W_NAMES}
    x = np.asarray(inputs["x"], dtype=np.float32).reshape(16 * SEQ, D)
    p = np.asarray(inputs["p"], dtype=np.float32).reshape(16 * SEQ, 256)
    in_maps = []
    for c in range(cores):
        m = {n: w[n] for n in k.din if n in w}
        m["x"] = np.ascontiguousarray(x[c * T:(c + 1) * T])
        m["p"] = np.ascontiguousarray(p[c * T:(c + 1) * T])
        in_maps.append({n: m[n] for n in k.din})
    res = run_bass_kernel_spmd(k.nc, in_maps, core_ids=list(range(cores)))
    return res


def kernel(**inputs):
    res = _run(inputs)
    outs = [np.asarray(r["out"], dtype=np.float32).reshape(NB, SEQ, D) for r in res.results]
    return np.concatenate(outs, axis=0)
```

```python
import math
import numpy as np
import concourse.bass as bass
import concourse.mybir as mybir
from concourse.bass_utils import run_bass_kernel_spmd

F32 = mybir.dt.float32
BF16 = mybir.dt.bfloat16
U32 = mybir.dt.uint32
I32 = mybir.dt.int32
AF = mybir.ActivationFunctionType
ALU = mybir.AluOpType
AX = mybir.AxisListType

NCORES = 8
D = 1024
SEQ = 2048
NB = 2
T = NB * SEQ
NT = T // 128
EPS = 1e-6
TWO_PI = 2.0 * math.pi


class Buf:
    __slots__ = ("name", "lw", "rd")

    def __init__(self, name=""):
        self.name = name
        self.lw = None
        self.rd = []


class Op:
    __slots__ = ("eng", "fn", "deps", "dma", "sem", "val", "sig", "sigidx", "prev_same_sem")

    def __init__(self, eng, fn, dma):
        self.eng = eng
        self.fn = fn
        self.deps = []
        self.dma = dma
        self.sem = None
        self.val = 0
        self.sig = False
        self.sigidx = 0
        self.prev_same_sem = 0


class Sched:
    ENGS = ("pe", "act", "dve", "pool", "sp")
    NDMA = 12
    NOSYNC = ("pe",)

    def __init__(self, nc, stack):
        self.nc = nc
        self.ops = []
        self.esem = {e: stack.enter_context(nc.semaphore("es_" + e)) for e in ("pe", "act", "dve", "pool")}
        self.ecount = {e: 0 for e in self.esem}
        self.dsem = {q: [stack.enter_context(nc.semaphore("ds_%s%d" % (q, i))) for i in range(self.NDMA)]
                     for q in ("sp", "act", "pool")}
        self.dcount = {q: 0 for q in self.dsem}
        self.waited = {e: {} for e in self.ENGS}
        self.phase = 0
        self.bufs = []

    def buf(self, name=""):
        b = Buf(name)
        self.bufs.append(b)
        return b

    def bufs_n(self, n, name=""):
        return [self.buf(name + str(i)) for i in range(n)]

    def _track(self, op, reads, writes):
        deps = []
        for b in reads:
            if b.lw is not None:
                deps.append(b.lw)
        for b in writes:
            if b.lw is not None:
                deps.append(b.lw)
            deps.extend(b.rd)
        for b in reads:
            b.rd.append(op)
        for b in writes:
            b.lw = op
            b.rd = []
        seen = set()
        for d in deps:
            if d is op or id(d) in seen:
                continue
            seen.add(id(d))
            if (not d.dma) and (not op.dma) and d.eng == op.eng and d.eng in self.NOSYNC:
                continue
            op.deps.append(d)
            d.sig = True

    def add(self, eng, fn, reads=(), writes=()):
        op = Op(eng, fn, False)
        self._track(op, reads, writes)
        self.ops.append(op)
        return op

    def dma(self, q, out, in_, reads=(), writes=(), **kw):
        op = Op(q, (lambda e, out=out, in_=in_, kw=kw: e.dma_start(out=out, in_=in_, **kw)), True)
        self._track(op, reads, writes)
        self.ops.append(op)
        return op

    def emit(self, name=None):
        nc = self.nc
        for op in self.ops:
            if op.dma:
                j = self.dcount[op.eng]
                self.dcount[op.eng] = j + 1
                op.sem = self.dsem[op.eng][j % self.NDMA]
                op.val = 16 * (j // self.NDMA + 1)
                op.prev_same_sem = 16 * (j // self.NDMA)
            elif op.sig:
                self.ecount[op.eng] += 1
                op.sem = self.esem[op.eng]
                op.val = self.ecount[op.eng]
        per = {e: [o for o in self.ops if o.eng == e] for e in self.ENGS}
        waited = self.waited
        dsem = self.dsem
        dcount = dict(self.dcount)
        NDMA = self.NDMA

        def run(e, ename):
            w = waited[ename]

            def wait(sem, val):
                k = sem.num
                if w.get(k, 0) >= val:
                    return
                w[k] = val
                e.wait_ge(sem, val)

            for op in per[ename]:
                for d in op.deps:
                    wait(d.sem, d.val)
                if op.dma:
                    if op.prev_same_sem:
                        wait(op.sem, op.prev_same_sem)
                    op.fn(e).then_inc(op.sem, 16)
                else:
                    ins = op.fn(e)
                    if op.sig:
                        ins.then_inc(op.sem, 1)
            if ename in dsem:
                n = dcount[ename]
                for i in range(NDMA):
                    cnt = (n - i + NDMA - 1) // NDMA if n > i else 0
                    if cnt:
                        wait(dsem[ename][i], 16 * cnt)

        with nc.Block() as block:
            @block.tensor
            def _(e):
                run(e, "pe")

            @block.scalar
            def _(e):
                run(e, "act")

            @block.vector
            def _(e):
                run(e, "dve")

            @block.gpsimd
            def _(e):
                run(e, "pool")

            @block.sync
            def _(e):
                run(e, "sp")
        self.ops = []
        self.phase += 1
        for b in self.bufs:
            b.lw = None
            b.rd = []


def _bc(ap, shape):
    return ap.to_broadcast(list(shape))


class K:
    def __init__(self, dbg=None):
        from contextlib import ExitStack
        self.dbg = dbg
        self.nc = nc = bass.Bass("TRN2", target_bir_lowering=False)
        self.root = ExitStack()
        self.S = Sched(nc, self.root)
        self.din = {}
        self.dq = 0

    def inp(self, name, shape):
        t = self.nc.dram_tensor(name, list(shape), F32, kind="ExternalInput").ap()
        self.din[name] = t
        return t

    def sb(self, stack, name, shape, dt=F32):
        return stack.enter_context(self.nc.sbuf_tensor(name, list(shape), dt))

    def ps(self, stack, name, shape, dt=F32):
        return stack.enter_context(self.nc.psum_tensor(name, list(shape), dt))

    def q(self):
        self.dq ^= 1
        return "sp" if self.dq else "act"

    def act(self, out, in_, func, reads, writes, **kw):
        return self.S.add("act", lambda e: e.activation(out=out, in_=in_, func=func, **kw), reads, writes)

    def mm(self, out, lhsT, rhs, start, stop, reads, writes, **kw):
        return self.S.add("pe", lambda e: e.matmul(out, lhsT, rhs, start=start, stop=stop, **kw), reads, writes)

    def tr(self, out, in_, ident, reads, writes):
        return self.S.add("pe", lambda e: e.transpose(out, in_, ident), reads, writes)

    def dve(self, fn, reads, writes):
        return self.S.add("dve", fn, reads, writes)

    def pool(self, fn, reads, writes):
        return self.S.add("pool", fn, reads, writes)

    def rstd(self, rstd, ss, scale, bufs_r, bufs_w):
        S = self.S
        self.dve(lambda e: e.tensor_scalar(out=rstd, in0=ss, scalar1=scale, scalar2=EPS, op0=ALU.mult, op1=ALU.add),
                 bufs_r, bufs_w)
        self.act(rstd, rstd, AF.Sqrt, bufs_w, bufs_w)
        self.dve(lambda e: e.reciprocal(out=rstd, in_=rstd), bufs_w, bufs_w)

    def load_weight(self, dst, dram2d, nk, n, gain, stage, stage_bufs, dst_buf, gain_buf):
        for kc in range(nk):
            st = stage[kc % 2]
            sbuf = stage_bufs[kc % 2]
            self.S.dma(self.q(), st[:, 0:n], dram2d[kc * 128:(kc + 1) * 128, :], [], [sbuf])
            if gain is None:
                self.dve(lambda e, st=st, kc=kc: e.tensor_copy(out=dst[:, kc, :], in_=st[:, 0:n]), [sbuf], [dst_buf])
            else:
                self.dve(lambda e, st=st, kc=kc: e.tensor_scalar(out=dst[:, kc, :], in0=st[:, 0:n],
                                                                 scalar1=gain[:, kc:kc + 1], scalar2=None,
                                                                 op0=ALU.mult),
                         [sbuf, gain_buf], [dst_buf])

    def sincos(self, ang, sin_o, cos_o, tmp, B):
        MAGIC = 12582912.0
        C1 = 6.28125
        C2 = TWO_PI - 6.28125
        d = self.dve
        d(lambda e: e.tensor_scalar(out=tmp, in0=ang, scalar1=1.0 / TWO_PI, scalar2=MAGIC, op0=ALU.mult, op1=ALU.add), B, B)
        d(lambda e: e.tensor_scalar(out=tmp, in0=tmp, scalar1=MAGIC, scalar2=None, op0=ALU.subtract), B, B)
        d(lambda e: e.scalar_tensor_tensor(out=ang, in0=tmp, scalar=-C1, in1=ang, op0=ALU.mult, op1=ALU.add), B, B)
        d(lambda e: e.scalar_tensor_tensor(out=ang, in0=tmp, scalar=-C2, in1=ang, op0=ALU.mult, op1=ALU.add), B, B)
        d(lambda e: e.tensor_scalar(out=ang, in0=ang, scalar1=math.pi, scalar2=-math.pi, op0=ALU.min, op1=ALU.max), B, B)
        self.act(sin_o, ang, AF.Sin, B, B)
        d(lambda e: e.scalar_tensor_tensor(out=tmp, in0=ang, scalar=-1.0, in1=ang, op0=ALU.mult, op1=ALU.max), B, B)
        self.act(cos_o, tmp, AF.Sin, B, B, scale=-1.0, bias=self.halfpi[:, 0:1])

    def build(self):
        from contextlib import ExitStack
        nc, S = self.nc, self.S
        inp = self.inp
        x = inp("x", [T, D])
        p_in = inp("p", [T, 256])
        ln_mix_g = inp("ln_mix_g", [1, D])
        w_in = inp("w_in", [1, D, 1536])
        a_re = inp("s5_a_re", [1, 32, 64]); a_im = inp("s5_a_im", [1, 32, 64])
        log_dt = inp("s5_log_dt", [1, 32])
        b_re = inp("s5_b_re", [1, 32, 64, 16]); b_im = inp("s5_b_im", [1, 32, 64, 16])
        c_re = inp("s5_c_re", [1, 32, 16, 64]); c_im = inp("s5_c_im", [1, 32, 16, 64])
        s5_d = inp("s5_d", [1, 512])
        glu_w = inp("glu_w", [1, 512, 512]); glu_b = inp("glu_b", [1, 512])
        sgu_ln_g = inp("sgu_ln_g", [1, 512]); sgu_ln_b = inp("sgu_ln_b", [1, 512])
        sgu_ws = inp("sgu_ws", [1, 4, 128, 128]); sgu_b = inp("sgu_b", [1, 4, 128])
        on_ssm = inp("out_norm_ssm", [1, 512]); on_sgu = inp("out_norm_sgu", [1, 512])
        w_out = inp("w_out", [1, D, D])
        ln_ffn_g = inp("ln_ffn_g", [1, D])
        peer_wq = inp("peer_wq", [1, D, 2048])
        peer_keys = inp("peer_keys", [1, 8, 2, 128, 128])
        peer_u = inp("peer_u", [1, 16384, D]); peer_v = inp("peer_v", [1, 16384, D])
        ple_norm_g = inp("ple_norm_g", [1, D])
        ple_w_gate = inp("ple_w_gate", [1, D, D]); ple_w_proj = inp("ple_w_proj", [1, 256, D])
        final_g = inp("final_g", [D])
        out = nc.dram_tensor("out", [T, D], F32, kind="ExternalOutput").ap()
        self.out = out
        dbg = self.dbg
        if dbg:
            self.dbg_out = {}

        def dbg_t(name, shape, dt=F32):
            t = nc.dram_tensor("dbg_" + name, list(shape), dt, kind="ExternalOutput").ap()
            self.dbg_out[name] = t
            return t

        h1_s = nc.dram_tensor("h1_s", [T, D], F32, kind="Internal").ap()
        hnT_s = nc.dram_tensor("hnT_s", [NT, 128, D], BF16, kind="Internal").ap()
        UT_s = nc.dram_tensor("UT_s", [128, 128, D], BF16, kind="Internal").ap()
        V_s = nc.dram_tensor("V_s", [128, 128, D], BF16, kind="Internal").ap()

        root = self.root
        sb, ps = self.sb, self.ps
        identF = sb(root, "identF", [128, 128], F32)
        identB = sb(root, "identB", [128, 128], BF16)
        onesB = sb(root, "onesB", [128, 128], BF16)
        self.halfpi = sb(root, "halfpi", [128, 1], F32)
        cB = S.buf("consts")
        S.add("pool", lambda e: e.memset(identF[:], 1.0), [], [cB])
        S.add("pool", lambda e: e.affine_select(out=identF[:], in_=identF[:], pattern=[[-1, 128]],
                                                compare_op=ALU.is_equal, fill=0.0, base=0, channel_multiplier=1), [cB], [cB])
        S.add("pool", lambda e: e.tensor_copy(out=identB[:], in_=identF[:]), [cB], [cB])
        S.add("pool", lambda e: e.memset(onesB[:], 1.0), [], [cB])
        S.add("pool", lambda e: e.memset(self.halfpi[:], math.pi / 2), [], [cB])
        S.emit()

        mixer = ExitStack()
        xsT_all = sb(mixer, "xsT_all", [128, 4, T], BF16)
        ygT_all = sb(mixer, "ygT_all", [128, 4, T], BF16)
        ss_sgu = sb(mixer, "ss_sgu", [128, NT], F32)
        xsB = S.bufs_n(4, "xsT")
        ygB = S.buf("ygT")
        ssgB = S.buf("ss_sgu")

        a1 = ExitStack()
        win = sb(a1, "win", [128, 8, 1536], BF16)
        stage = [sb(a1, "stage%d" % i, [128, 1536], F32) for i in range(2)]
        stageB = S.bufs_n(2, "stage")
        gmix = sb(a1, "gmix", [128, 8], F32)
        winB = S.buf("win"); gmixB = S.buf("gmix")
        S.dma("sp", gmix[:], ln_mix_g.rearrange("o (kc p) -> p (o kc)", p=128), [], [gmixB], allow_slow_non_contiguous=True)
        self.load_weight(win, w_in[0], 8, 1536, gmix, stage, stageB, winB, gmixB)
        wsmT = sb(a1, "wsmT", [128, 4, 128], BF16)
        Rt = sb(a1, "Rt", [128, 4, 128], F32)
        sg_g = sb(a1, "sg_g", [128, 4], F32)
        sg_b = sb(a1, "sg_b", [128, 4], F32)
        bs1 = sb(a1, "bs1", [1, 512], F32)
        bs1b = sb(a1, "bs1b", [1, 512], BF16)
        sguB = S.buf("sgu_consts")
        ws_t = sb(a1, "ws_t", [128, 4, 128], F32)
        ws_tB = S.buf("ws_t")
        pA = [ps(a1, "pA%d" % i, [128, 512], F32) for i in range(6)]
        pAB = S.bufs_n(6, "pA")
        pT = ps(a1, "pT", [128, 1024], BF16)
        pTB = S.buf("pT")
        S.dma("sp", sg_g[:], sgu_ln_g.rearrange("o (h d) -> d (o h)", d=128), [], [sguB], allow_slow_non_contiguous=True)
        S.dma("sp", sg_b[:], sgu_ln_b.rearrange("o (h d) -> d (o h)", d=128), [], [sguB], allow_slow_non_contiguous=True)
        S.dma("sp", bs1[:], sgu_b.rearrange("o h t -> o (h t)"), [], [sguB])
        for h in range(4):
            S.dma("act", ws_t[:, h, :], sgu_ws[0, h], [], [ws_tB])
        S.add("pool", lambda e: e.affine_select(out=ws_t[:], in_=ws_t[:], pattern=[[0, 4], [-1, 128]],
                                                compare_op=ALU.is_ge, fill=0.0, base=0, channel_multiplier=1),
              [ws_tB], [ws_tB])
        for h in range(4):
            self.tr(pA[0][:, h * 128:(h + 1) * 128], ws_t[:, h, :], identF[:], [ws_tB], [pAB[0]])
        self.dve(lambda e: e.tensor_copy(out=wsmT[:].rearrange("p h t -> p (h t)"), in_=pA[0][:]), [pAB[0]], [sguB])
        self.dve(lambda e: e.tensor_copy(out=bs1b[:], in_=bs1[:]), [sguB], [sguB])
        self.mm(pA[1][:], onesB[:], wsmT[:].rearrange("p h t -> p (h t)"), True, True, [sguB], [pAB[1]])
        self.mm(pA[2][:], onesB[0:1, :], bs1b[:], True, True, [sguB], [pAB[2]])
        self.dve(lambda e: e.tensor_copy(out=Rt[:].rearrange("p h t -> p (h t)"), in_=pA[2][:]), [pAB[2]], [sguB])
        for h in range(4):
            self.dve(lambda e, h=h: e.scalar_tensor_tensor(out=Rt[:, h, :], in0=pA[1][:, h * 128:(h + 1) * 128],
                                                            scalar=sg_b[:, h:h + 1], in1=Rt[:, h, :],
                                                            op0=ALU.mult, op1=ALU.add),
                     [pAB[1], sguB], [sguB])

        xt = [sb(a1, "xt%d" % i, [128, D], F32) for i in range(2)]
        xtB = S.bufs_n(2, "xt")
        junk = sb(a1, "junk", [128, D], BF16)
        junkB = S.buf("junk")
        st1 = [sb(a1, "st1_%d" % i, [128, 2], F32) for i in range(2)]
        st1B = S.bufs_n(2, "st1")
        xnb = [sb(a1, "xnb%d" % i, [128, D], BF16) for i in range(2)]
        xnbB = S.bufs_n(2, "xnb")
        xnT = [sb(a1, "xnT%d" % i, [128, 8, 128], BF16) for i in range(2)]
        xnTB = S.bufs_n(2, "xnT")
        gu = [sb(a1, "gu%d" % i, [128, 4, 128], F32) for i in range(2)]
        guB = S.bufs_n(2, "gu")
        gv = [sb(a1, "gv%d" % i, [128, 512], F32) for i in range(2)]
        gvB = S.bufs_n(2, "gv")
        bst = sb(a1, "bst", [128, 4, 6], F32); mv = sb(a1, "mv", [128, 4, 2], F32); lrs = sb(a1, "lrs", [128, 4], F32)
        lnB = S.buf("lnstats")
        vn = [sb(a1, "vn%d" % i, [128, 512], BF16) for i in range(2)]
        vnB = S.bufs_n(2, "vn")
        mt = sb(a1, "mt", [128, 4, 128], F32); mtB = S.buf("mt")
        ysq = sb(a1, "ysq", [128, 4, 128], BF16); ysqB = S.buf("ysq")

        NTB = 4
        ust = [sb(a1, "ust%d" % i, [128, D], F32) for i in range(NTB)]; ustB = S.bufs_n(NTB, "ust")
        vst = [sb(a1, "vst%d" % i, [128, D], F32) for i in range(NTB)]; vstB = S.bufs_n(NTB, "vst")
        ubf = [sb(a1, "ubf%d" % i, [128, D], BF16) for i in range(2)]; ubfB = S.bufs_n(2, "ubf")
        utb = [sb(a1, "utb%d" % i, [128, D], BF16) for i in range(3)]; utbB = S.bufs_n(3, "utb")
        vbf = [sb(a1, "vbf%d" % i, [128, D], BF16) for i in range(3)]; vbfB = S.bufs_n(3, "vbf")
        pTu = ps(a1, "pTu", [128, D], BF16); pTuB = S.buf("pTu")

        def t_load(tb_):
            if tb_ >= 128:
                return
            k4 = tb_ % NTB
            S.dma("sp", ust[k4][:], peer_u[0, tb_ * 128:(tb_ + 1) * 128, :], [], [ustB[k4]])
            S.dma("sp", vst[k4][:], peer_v[0, tb_ * 128:(tb_ + 1) * 128, :], [], [vstB[k4]])

        def t_block(tb_):
            t_load(tb_ + 2)
            k = tb_ % 2
            k4 = tb_ % NTB
            self.act(ubf[k][:], ust[k4][:], AF.Copy, [ustB[k4]], [ubfB[k]])
            for kc in range(8):
                self.tr(pTu[:, kc * 128:(kc + 1) * 128], ubf[k][:, kc * 128:(kc + 1) * 128], identB[:], [ubfB[k]], [pTuB])
            k3 = tb_ % 3
            self.dve(lambda e, k3=k3: e.tensor_copy(out=utb[k3][:], in_=pTu[:]), [pTuB], [utbB[k3]])
            S.add("pool", lambda e, k3=k3, k4=k4: e.tensor_copy(out=vbf[k3][:], in_=vst[k4][:]), [vstB[k4]], [vbfB[k3]])
            if tb_ >= 1:
                t_store(tb_ - 1)

        def t_store(tb_):
            k3 = tb_ % 3
            S.dma("act", UT_s[tb_], utb[k3][:], [utbB[k3]], [])
            S.dma("act", V_s[tb_], vbf[k3][:], [vbfB[k3]], [])

        t_load(0)
        t_load(1)
        def a1_front(i):
            b = i % 2
            cols = slice(i * 128, (i + 1) * 128)
            for tb_ in range(4 * i, 4 * i + 4):
                t_block(tb_)
            S.dma("sp", xt[b][:], x[i * 128:(i + 1) * 128, :], [], [xtB[b]])
            self.dve(lambda e, b=b: e.scalar_tensor_tensor(out=junk[:], in0=xt[b][:], scalar=1.0, in1=xt[b][:],
                                                           op0=ALU.mult, op1=ALU.mult, accum_out=st1[b][:, 0:1]),
                     [xtB[b]], [junkB, st1B[b]])
            self.rstd(st1[b][:, 1:2], st1[b][:, 0:1], 1.0 / D, [st1B[b]], [st1B[b]])
            self.act(xnb[b][:], xt[b][:], AF.Copy, [xtB[b], st1B[b]], [xnbB[b]], scale=st1[b][:, 1:2])
            for kc in range(8):
                self.tr(pT[:, kc * 128:(kc + 1) * 128], xnb[b][:, kc * 128:(kc + 1) * 128], identB[:], [xnbB[b]], [pTB])
            self.dve(lambda e, b=b: e.tensor_copy(out=xnT[b][:].rearrange("p k t -> p (k t)"), in_=pT[:]), [pTB], [xnTB[b]])

        def a1_back(i):
            b = i % 2
            cols = slice(i * 128, (i + 1) * 128)
            for c in range(4):
                for kc in range(8):
                    self.mm(pA[0][:, c * 128:(c + 1) * 128], win[:, kc, c * 128:(c + 1) * 128], xnT[b][:, kc, :],
                            kc == 0, kc == 7, [winB, xnTB[b]], [pAB[0]])
            self.act(xsT_all[:, :, cols], pA[0][:].rearrange("p (c t) -> p c t", c=4), AF.Copy, [pAB[0]], xsB)
            for c in range(4):
                for kc in range(8):
                    self.mm(pA[1][:, c * 128:(c + 1) * 128], win[:, kc, 512 + c * 128:512 + (c + 1) * 128], xnT[b][:, kc, :],
                            kc == 0, kc == 7, [winB, xnTB[b]], [pAB[1]])
            self.act(gu[b][:].rearrange("p c t -> p (c t)"), pA[1][:], AF.Gelu, [pAB[1]], [guB[b]])
            for kc in range(8):
                self.mm(pA[2][:], xnT[b][:, kc, :], win[:, kc, 1024:1536], kc == 0, kc == 7, [winB, xnTB[b]], [pAB[2]])
            self.act(gv[b][:], pA[2][:], AF.Gelu, [pAB[2]], [gvB[b]])
            for h in range(4):
                self.dve(lambda e, b=b, h=h: e.bn_stats(out=bst[:, h, :], in_=gv[b][:, h * 128:(h + 1) * 128]), [gvB[b]], [lnB])
            for h in range(4):
                self.dve(lambda e, h=h: e.bn_aggr(out=mv[:, h, :], in_=bst[:, h, :]), [lnB], [lnB])
            self.dve(lambda e: e.tensor_scalar(out=lrs[:], in0=mv[:, :, 1], scalar1=EPS, scalar2=None, op0=ALU.add), [lnB], [lnB])
            self.act(lrs[:], lrs[:], AF.Sqrt, [lnB], [lnB])
            self.dve(lambda e: e.reciprocal(out=lrs[:], in_=lrs[:]), [lnB], [lnB])
            for h in range(4):
                self.dve(lambda e, b=b, h=h: e.tensor_scalar(out=vn[b][:, h * 128:(h + 1) * 128], in0=gv[b][:, h * 128:(h + 1) * 128],
                                                             scalar1=mv[:, h, 0:1], scalar2=lrs[:, h:h + 1],
                                                             op0=ALU.subtract, op1=ALU.mult),
                         [gvB[b], lnB], [vnB[b]])

        def a1_back2(i):
            b = i % 2
            cols = slice(i * 128, (i + 1) * 128)
            for h in range(4):
                self.mm(pA[3][:, h * 128:(h + 1) * 128], vn[b][:, h * 128:(h + 1) * 128], wsmT[:, h, :], True, True,
                        [vnB[b], sguB], [pAB[3]])
            for h in range(4):
                self.dve(lambda e, h=h: e.scalar_tensor_tensor(out=mt[:, h, :], in0=pA[3][:, h * 128:(h + 1) * 128],
                                                                scalar=sg_g[:, h:h + 1], in1=Rt[:, h, :],
                                                                op0=ALU.mult, op1=ALU.add),
                         [pAB[3], sguB], [mtB])
            self.dve(lambda e, b=b, cols=cols: e.tensor_tensor(out=ygT_all[:, :, cols], in0=mt[:], in1=gu[b][:], op=ALU.mult),
                     [mtB, guB[b]], [ygB])
            self.act(ysq[:], ygT_all[:, :, cols], AF.Square, [ygB], [ysqB])
            for c in range(4):
                self.mm(pA[4][:, 0:1], ysq[:, c, :], onesB[:, 0:1], c == 0, c == 3, [ysqB], [pAB[4]])
            self.dve(lambda e, i=i: e.tensor_copy(out=ss_sgu[:, i:i + 1], in_=pA[4][:, 0:1]), [pAB[4]], [ssgB])
        a1_front(0)
        for i in range(NT):
            if i + 1 < NT:
                a1_front(i + 1)
            a1_back(i)
            if i >= 1:
                a1_back2(i - 1)
        a1_back2(NT - 1)
        t_store(127)
        if dbg == "a1":
            d1 = dbg_t("xsT", [128, 4, T], BF16); d2 = dbg_t("ygT", [128, 4, T], BF16); d3 = dbg_t("ss_sgu", [128, NT])
            S.dma("sp", d1, xsT_all[:], xsB, []); S.dma("sp", d2, ygT_all[:], [ygB], []); S.dma("sp", d3, ss_sgu[:], [ssgB], [])
        S.emit()
        a1.close()
        if dbg == "a1":
            mixer.close()
            return

        s5p = ExitStack()
        T0 = sb(s5p, "T0", [128, 32, 128], BF16)
        Wv = sb(s5p, "Wv", [128, 32, 2, 64], BF16)
        Wy = sb(s5p, "Wy", [128, 32, 2, 128], BF16)
        cosT = sb(s5p, "cosT", [128, 16, 256], F32)
        sinT = sb(s5p, "sinT", [128, 16, 256], F32)
        r8 = sb(s5p, "r8", [128, 16], F32)
        dtile = sb(s5p, "dtile", [128, 32], F32)
        s5cB = S.buf("s5consts")
        selB = S.buf("sel")
        pp = ExitStack()
        are = sb(pp, "are", [128, 16]); aim = sb(pp, "aim", [128, 16]); dtb = sb(pp, "dtb", [128, 16])
        lrdt = sb(pp, "lrdt", [128, 16]); ang1 = sb(pp, "ang1", [128, 16])
        braw = [sb(pp, "braw%d" % i, [128, 16, 16]) for i in range(2)]
        craw = [sb(pp, "craw%d" % i, [128, 16, 16]) for i in range(2)]
        tauI = sb(pp, "tauI", [128, 17], I32); tau = sb(pp, "tau", [128, 17])
        kI = sb(pp, "kI", [128, 256], I32); kF = sb(pp, "kF", [128, 256])
        E17 = sb(pp, "E17", [128, 16, 17]); A17 = sb(pp, "A17", [128, 16, 17]); tmp17 = sb(pp, "tmp17", [128, 16, 17])
        sn17 = sb(pp, "sn17", [128, 16, 17]); cs17 = sb(pp, "cs17", [128, 16, 17])
        pw = [sb(pp, "pw%d" % i, [128, 16, 17]) for i in range(2)]
        sm = [sb(pp, "sm%d" % i, [128, 16]) for i in range(8)]
        Bb = [sb(pp, "Bb%d" % i, [128, 16, 16]) for i in range(2)]
        tb = [sb(pp, "tb%d" % i, [128, 16, 16]) for i in range(2)]
        Xr = sb(pp, "Xr", [128, 8, 8, 16]); Xi = sb(pp, "Xi", [128, 8, 8, 16])
        Yr = sb(pp, "Yr", [128, 8, 8, 16]); Yi = sb(pp, "Yi", [128, 8, 8, 16])
        t1 = sb(pp, "t1", [128, 8, 8, 16]); t2 = sb(pp, "t2", [128, 8, 8, 16])
        mask = sb(pp, "mask", [128, 128])
        Zb = [sb(pp, "Zb%d" % i, [128, 8, 128], BF16) for i in range(2)]
        pSb = ps(pp, "pSb", [128, 2, 1024], BF16); pSbB = S.bufs_n(2, "pSb")
        phr = sb(pp, "phr", [128, 16]); pht = sb(pp, "pht", [128, 16])
        pS = [ps(pp, "pS%d" % i, [128, 512], F32) for i in range(2)]
        pSB = S.bufs_n(2, "pS")
        PB = S.buf("s5prep")
        XB = S.buf("X"); YB = S.buf("Y")
        NC = dict(allow_slow_non_contiguous=True)
        for par in range(2):
            rows = slice(par * 64, (par + 1) * 64)
            S.dma(self.q(), are[rows, :], a_re[0].rearrange("(gp par) p -> par p gp", par=2)[par], [], [PB], **NC)
            S.dma(self.q(), aim[rows, :], a_im[0].rearrange("(gp par) p -> par p gp", par=2)[par], [], [PB], **NC)
            S.dma(self.q(), dtb[rows, :], bass.AP(log_dt.tensor, par, [[0, 64], [2, 16]]), [], [PB], **NC)
            S.dma(self.q(), braw[0][rows], b_re[0].rearrange("(gp par) p c -> par p gp c", par=2)[par], [], [PB])
            S.dma(self.q(), braw[1][rows], b_im[0].rearrange("(gp par) p c -> par p gp c", par=2)[par], [], [PB])
            for gp in range(16):
                S.dma(self.q(), craw[0][rows, gp, :], c_re[0, 2 * gp + par].rearrange("c p -> p c"), [], [PB], **NC)
                S.dma(self.q(), craw[1][rows, gp, :], c_im[0, 2 * gp + par].rearrange("c p -> p c"), [], [PB], **NC)
        for t in range(8):
            S.dma(self.q(), dtile[16 * t:16 * (t + 1), :], s5_d[0].rearrange("(g c) -> c g", c=16), [], [s5cB], **NC)
        S.add("pool", lambda e: e.iota(tauI[:, 0:8], pattern=[[-1, 8]], base=0, channel_multiplier=0), [], [PB])
        S.add("pool", lambda e: e.iota(tauI[:, 8:17], pattern=[[1, 9]], base=0, channel_multiplier=0), [PB], [PB])
        S.add("pool", lambda e: e.iota(kI[:], pattern=[[1, 256]], base=0, channel_multiplier=0), [PB], [PB])
        S.add("pool", lambda e: e.memset(mask[:], 1.0), [PB], [PB])
        S.add("pool", lambda e: e.affine_select(out=mask[:], in_=mask[:], pattern=[[16, 8], [0, 16]], compare_op=ALU.is_ge,
                                                fill=0.0, base=15, channel_multiplier=-1), [PB], [PB])
        if dbg == "s5p0":
            S.dma("sp", dbg_t("craw", [128, 16, 16]), craw[0][:], [PB], [])
            S.dma("sp", dbg_t("dtb", [128, 16]), dtb[:], [PB], [])
            S.dma("sp", dbg_t("mask", [128, 128]), mask[:], [PB], [])
            S.dma("sp", dbg_t("dtile", [128, 32]), dtile[:], [s5cB], [])
            S.emit(); pp.close(); s5p.close(); mixer.close()
            return
        P1 = [PB]
        d = lambda fn: self.dve(fn, P1, P1)
        TT = lambda o, a, b, op: d(lambda e: e.tensor_tensor(out=o, in0=a, in1=b, op=op))
        d(lambda e: e.tensor_copy(out=tau[:], in_=tauI[:]))
        d(lambda e: e.tensor_copy(out=kF[:], in_=kI[:]))
        self.act(dtb[:], dtb[:], AF.Exp, P1, P1)
        TT(lrdt[:], are[:], dtb[:], ALU.mult)
        TT(ang1[:], aim[:], dtb[:], ALU.mult)
        s17 = [128, 16, 17]
        taub = _bc(tau[:].unsqueeze(1), s17)
        TT(E17[:], _bc(lrdt[:].unsqueeze(2), s17), taub, ALU.mult)
        self.act(E17[:], E17[:], AF.Exp, P1, P1)
        TT(A17[:], _bc(ang1[:].unsqueeze(2), s17), taub, ALU.mult)
        self.sincos(A17[:], sn17[:], cs17[:], tmp17[:], P1)
        TT(pw[0][:], E17[:], cs17[:], ALU.mult)
        TT(pw[1][:], E17[:], sn17[:], ALU.mult)
        d(lambda e: e.tensor_copy(out=r8[:], in_=E17[:, :, 16]))
        if dbg == "s5p1":
            S.dma("sp", dbg_t("pw0", [128, 16, 17]), pw[0][:], [PB], [])
            S.dma("sp", dbg_t("pw1", [128, 16, 17]), pw[1][:], [PB], [])
            S.emit(); pp.close(); s5p.close(); mixer.close()
            return
        abr, abi = pw[0][:, :, 9], pw[1][:, :, 9]
        nr, den, cre, cim, u1, u2 = [m[:] for m in sm[:6]]
        d(lambda e: e.tensor_scalar(out=nr, in0=abr, scalar1=-1.0, scalar2=None, op0=ALU.add))
        TT(den, are[:], are[:], ALU.mult); TT(u1, aim[:], aim[:], ALU.mult); TT(den, den, u1, ALU.add)
        d(lambda e: e.reciprocal(out=den, in_=den))
        TT(u1, nr, are[:], ALU.mult); TT(u2, abi, aim[:], ALU.mult); TT(cre, u1, u2, ALU.add); TT(cre, cre, den, ALU.mult)
        TT(u1, abi, are[:], ALU.mult); TT(u2, nr, aim[:], ALU.mult); TT(cim, u1, u2, ALU.subtract); TT(cim, cim, den, ALU.mult)
        s16 = [128, 16, 16]
        creb, cimb = _bc(cre.unsqueeze(2), s16), _bc(cim.unsqueeze(2), s16)
        TT(tb[0][:], creb, braw[0][:], ALU.mult); TT(tb[1][:], cimb, braw[1][:], ALU.mult); TT(Bb[0][:], tb[0][:], tb[1][:], ALU.subtract)
        TT(tb[0][:], creb, braw[1][:], ALU.mult); TT(tb[1][:], cimb, braw[0][:], ALU.mult); TT(Bb[1][:], tb[0][:], tb[1][:], ALU.add)
        s4 = [128, 8, 8, 16]

        def cmul(o_re, o_im, ar, ai, br, bi, neg_im=False, rw=P1):
            dd = lambda fn: self.dve(fn, rw, rw)
            dd(lambda e: e.tensor_tensor(out=t1[:], in0=ar, in1=br, op=ALU.mult))
            dd(lambda e: e.tensor_tensor(out=t2[:], in0=ai, in1=bi, op=ALU.mult))
            dd(lambda e: e.tensor_tensor(out=o_re, in0=t1[:], in1=t2[:], op=ALU.subtract))
            dd(lambda e: e.tensor_tensor(out=t1[:], in0=ar, in1=bi, op=ALU.mult))
            dd(lambda e: e.tensor_tensor(out=t2[:], in0=ai, in1=br, op=ALU.mult))
            if neg_im:
                dd(lambda e: e.scalar_tensor_tensor(out=o_im, in0=t1[:], scalar=-1.0, in1=t2[:], op0=ALU.mult, op1=ALU.subtract))
            else:
                dd(lambda e: e.tensor_tensor(out=o_im, in0=t1[:], in1=t2[:], op=ALU.add))

        RW = [PB, XB, YB, s5cB]
        self.dve(lambda e: e.memset(Wy[:], 0.0), RW, RW)
        for hf in range(2):
            gs = slice(8 * hf, 8 * hf + 8)
            Nre = _bc(pw[0][:, gs, 0:8].unsqueeze(3), s4); Nim = _bc(pw[1][:, gs, 0:8].unsqueeze(3), s4)
            Pre = _bc(pw[0][:, gs, 8:16].unsqueeze(3), s4); Pim = _bc(pw[1][:, gs, 8:16].unsqueeze(3), s4)
            P1re = _bc(pw[0][:, gs, 9:17].unsqueeze(3), s4); P1im = _bc(pw[1][:, gs, 9:17].unsqueeze(3), s4)
            Bre = _bc(Bb[0][:, gs].unsqueeze(2), s4); Bim = _bc(Bb[1][:, gs].unsqueeze(2), s4)
            Cre = _bc(craw[0][:, gs].unsqueeze(2), s4); Cim = _bc(craw[1][:, gs].unsqueeze(2), s4)
            cmul(Xr[:], Xi[:], Nre, Nim, Bre, Bim, rw=RW)
            cmul(Yr[:], Yi[:], Pre, Pim, Cre, Cim, neg_im=True, rw=RW)
            if dbg == "s5p2":
                S.dma("sp", dbg_t("Xr", [128, 8, 8, 16]), Xr[:], RW, [])
                S.dma("sp", dbg_t("Yi", [128, 8, 8, 16]), Yi[:], RW, [])
                S.emit(); pp.close(); s5p.close(); mixer.close()
                return
            for par in range(2):
                rows = slice(par * 64, (par + 1) * 64)
                for r in range(2):
                    for gi in range(4):
                        gp = 4 * r + gi
                        self.mm(pS[par][:, gi * 128:(gi + 1) * 128], Xr[rows, gp].rearrange("p s c -> p (s c)"),
                                Yr[rows, gp].rearrange("p s c -> p (s c)"), True, False, RW, [pSB[par]])
                        self.mm(pS[par][:, gi * 128:(gi + 1) * 128], Xi[rows, gp].rearrange("p s c -> p (s c)"),
                                Yi[rows, gp].rearrange("p s c -> p (s c)"), False, True, RW, [pSB[par]])
                    gsl2 = slice(16 * hf + 8 * r + par, 16 * hf + 8 * r + 8, 2)
                    self.dve(lambda e, par=par, gsl2=gsl2: e.tensor_tensor(out=T0[:, gsl2, :], in0=pS[par][:].rearrange("p (g m) -> p g m", g=4),
                                                                          in1=_bc(mask[:].unsqueeze(1), [128, 4, 128]), op=ALU.mult),
                             [pSB[par], PB], [s5cB])
            if dbg == "s5p3":
                S.dma("sp", dbg_t("T0", [128, 32, 128], BF16), T0[:], RW, [])
                S.emit(); pp.close(); s5p.close(); mixer.close()
                return
            s3 = [128, 8, 128]
            P7re = _bc(pw[0][:, gs, 15:16], s3); P7im = _bc(pw[1][:, gs, 15:16], s3)
            t1v, t2v = t1[:].rearrange("p g s c -> p g (s c)"), t2[:].rearrange("p g s c -> p g (s c)")
            Xrv, Xiv = Xr[:].rearrange("p g s c -> p g (s c)"), Xi[:].rearrange("p g s c -> p g (s c)")
            Zr, Zi = Zb[0][:], Zb[1][:]
            dz = lambda fn: self.dve(fn, RW, RW)
            dz(lambda e, t1v=t1v, P7re=P7re, Xrv=Xrv: e.tensor_tensor(out=t1v, in0=P7re, in1=Xrv, op=ALU.mult))
            dz(lambda e, t2v=t2v, P7im=P7im, Xiv=Xiv: e.tensor_tensor(out=t2v, in0=P7im, in1=Xiv, op=ALU.mult))
            dz(lambda e, Zr=Zr, t1v=t1v, t2v=t2v: e.tensor_tensor(out=Zr, in0=t1v, in1=t2v, op=ALU.subtract))
            dz(lambda e, t1v=t1v, P7re=P7re, Xiv=Xiv: e.tensor_tensor(out=t1v, in0=P7re, in1=Xiv, op=ALU.mult))
            dz(lambda e, t2v=t2v, P7im=P7im, Xrv=Xrv: e.tensor_tensor(out=t2v, in0=P7im, in1=Xrv, op=ALU.mult))
            dz(lambda e, Zi=Zi, t1v=t1v, t2v=t2v: e.tensor_tensor(out=Zi, in0=t1v, in1=t2v, op=ALU.add))
            for par in range(2):
                rows = slice(par * 64, (par + 1) * 64)
                for r in range(2):
                    for gi in range(4):
                        gp = 4 * r + gi
                        self.tr(pSb[:, par, gi * 128:gi * 128 + 64], Zb[0][rows, gp, :], identB[rows, rows], RW, [pSbB[par]])
                        self.tr(pSb[:, par, gi * 128 + 64:(gi + 1) * 128], Zb[1][rows, gp, :], identB[rows, rows], RW, [pSbB[par]])
                    gsl2 = slice(16 * hf + 8 * r + par, 16 * hf + 8 * r + 8, 2)
                    self.dve(lambda e, par=par, gsl2=gsl2: e.tensor_copy(out=Wv[:, gsl2], in_=pSb[:, par, 0:512].rearrange("p (g r m) -> p g r m", g=4, r=2)),
                             [pSbB[par]], [s5cB])
            P1re = _bc(pw[0][:, gs, 9:17].unsqueeze(3), s4); P1im = _bc(pw[1][:, gs, 9:17].unsqueeze(3), s4)
            cmul(Xr[:], Xi[:], P1re, P1im, Cre, Cim, neg_im=True, rw=RW)
            for par in range(2):
                rows = slice(par * 64, (par + 1) * 64)
                gsl2 = slice(16 * hf + par, 16 * hf + 16, 2)
                for ri, src in enumerate((Xr, Xi)):
                    self.dve(lambda e, rows=rows, gsl2=gsl2, ri=ri, src=src: e.tensor_copy(
                        out=Wy[rows, gsl2, ri, :], in_=src[rows].rearrange("p g s c -> p g (s c)")), RW, RW)
        d(lambda e: e.tensor_scalar(out=phr[:], in0=ang1[:], scalar1=8.0, scalar2=None, op0=ALU.mult))
        MAGIC = 12582912.0
        d(lambda e: e.tensor_scalar(out=pht[:], in0=phr[:], scalar1=1.0 / TWO_PI, scalar2=MAGIC, op0=ALU.mult, op1=ALU.add))
        d(lambda e: e.tensor_scalar(out=pht[:], in0=pht[:], scalar1=MAGIC, scalar2=None, op0=ALU.subtract))
        d(lambda e: e.scalar_tensor_tensor(out=phr[:], in0=pht[:], scalar=-6.28125, in1=phr[:], op0=ALU.mult, op1=ALU.add))
        d(lambda e: e.scalar_tensor_tensor(out=phr[:], in0=pht[:], scalar=-(TWO_PI - 6.28125), in1=phr[:], op0=ALU.mult, op1=ALU.add))
        sh = [128, 4, 256]
        angb = t1[:].rearrange("p g s c -> p (g s c)").rearrange("p (a k) -> p a k", k=256)
        tmpb = t2[:].rearrange("p g s c -> p (g s c)").rearrange("p (a k) -> p a k", k=256)
        for hf in range(4):
            gsl = slice(4 * hf, 4 * hf + 4)
            self.dve(lambda e, gsl=gsl: e.tensor_tensor(out=angb, in0=_bc(phr[:, gsl].unsqueeze(2), sh),
                                                        in1=_bc(kF[:].unsqueeze(1), sh), op=ALU.mult), RW, RW)
            self.sincos(angb, sinT[:, gsl, :], cosT[:, gsl, :], tmpb, RW)
        if dbg == "s5p":
            for nm, tt, dt_ in (("T0", T0, BF16), ("Wv", Wv, BF16), ("Wy", Wy, BF16), ("cosT", cosT, F32), ("sinT", sinT, F32)):
                S.dma("sp", dbg_t(nm, list(tt[:].shape), dt_), tt[:], [s5cB, PB], [])
            S.dma("sp", dbg_t("pw0", [128, 16, 17]), pw[0][:], [PB], [])
            S.dma("sp", dbg_t("pw1", [128, 16, 17]), pw[1][:], [PB], [])
        S.emit()
        pp.close()
        if dbg == "s5p":
            s5p.close(); mixer.close()
            return

        sm_ = ExitStack()
        Sel = sb(sm_, "Sel", [128, 8, 8, 128], BF16)
        SelO = sb(sm_, "SelO", [128, 8, 8, 128], BF16)
        S.add("pool", lambda e: e.memset(Sel[:], 0.0), [], [selB])
        S.add("pool", lambda e: e.memset(SelO[:], 0.0), [], [selB])
        idv = identB[:].rearrange("p (g c) -> p g c", c=16)
        for s_ in range(8):
            S.add("pool", lambda e, s_=s_: e.tensor_copy(out=Sel[:, :, s_, 16 * s_:16 * (s_ + 1)], in_=idv), [selB], [selB])
            S.add("pool", lambda e, s_=s_: e.tensor_copy(out=SelO[:, s_, :, 16 * s_:16 * (s_ + 1)], in_=idv), [selB], [selB])
        ublk = [[sb(sm_, "ublk%d%d" % (i, j), [128, 512], BF16) for j in range(2)] for i in range(2)]
        ublkB = [S.bufs_n(2, "ublk%d" % i) for i in range(2)]
        yblk = sb(sm_, "yblk", [128, 8, 512], BF16); yblkB = S.bufs_n(8, "yblk")
        Gm = [sb(sm_, "Gm%d" % i, [128, 2, 256]) for i in range(2)]
        Sc = [sb(sm_, "Sc%d" % i, [128, 2, 256]) for i in range(2)]
        tq = [sb(sm_, "tq%d" % i, [128, 2, 256]) for i in range(2)]
        Hp = [sb(sm_, "Hp%d" % i, [128, 2, 512], BF16) for i in range(2)]
        HpB = S.bufs_n(2, "Hp")
        wB = S.buf("s5work")
        pU = [ps(sm_, "pU%d" % i, [128, 512]) for i in range(2)]; pUB = S.bufs_n(2, "pU")
        pV = ps(sm_, "pV", [128, 2, 512]); pVB = S.buf("pV")
        pY = [ps(sm_, "pY%d" % i, [128, 512]) for i in range(2)]; pYB = S.bufs_n(2, "pY")
        pO = [ps(sm_, "pO%d" % i, [128, 512]) for i in range(2)]; pOB = S.bufs_n(2, "pO")
        self.dve(lambda e: e.memset(Sc[0][:], 0.0), [wB], [wB])
        self.dve(lambda e: e.memset(Sc[1][:], 0.0), [wB], [wB])
        def xv_of(c):
            return xsT_all[:, c, :].rearrange("p (b j s) -> p b j s", b=2, s=8)

        def st_R(gp):
            c = gp // 4; db = gp % 2; xv = xv_of(c)
            for par in range(2):
                g = 2 * gp + par
                gl = g % 8
                for s_ in range(8):
                    self.mm(pU[par][:], Sel[:, gl, s_, :], xv[:, :, :, s_], s_ == 0, s_ == 7, [selB, xsB[c]], [pUB[par]])
                self.act(ublk[db][par][:], pU[par][:], AF.Copy, [pUB[par]], [ublkB[db][par]])

        def st_V(gp):
            db = gp % 2
            for par in range(2):
                g = 2 * gp + par
                for ri in range(2):
                    self.mm(pV[par * 64:(par + 1) * 64, ri, :], Wv[:, g, ri, :], ublk[db][par][:], True, True,
                            [s5cB, ublkB[db][par]], [pVB])

        def dv(fn, r=()):
            self.dve(fn, [wB, s5cB] + list(r), [wB])

        def st_M(gp):
            cm = _bc(cosT[:, gp, 1:256].unsqueeze(1), [128, 2, 255]); sn = _bc(sinT[:, gp, 1:256].unsqueeze(1), [128, 2, 255])
            Vre = pV[:, 0, :].rearrange("p (b j) -> p b j", b=2)[:, :, 0:255]
            Vim = pV[:, 1, :].rearrange("p (b j) -> p b j", b=2)[:, :, 0:255]
            dv(lambda e: e.tensor_tensor(out=tq[0][:, :, 0:255], in0=Vre, in1=cm, op=ALU.mult), [pVB])
            dv(lambda e: e.tensor_tensor(out=tq[1][:, :, 0:255], in0=Vim, in1=sn, op=ALU.mult), [pVB])
            dv(lambda e: e.tensor_tensor(out=Gm[0][:, :, 0:255], in0=tq[0][:, :, 0:255], in1=tq[1][:, :, 0:255], op=ALU.add))
            dv(lambda e: e.tensor_tensor(out=tq[0][:, :, 0:255], in0=Vim, in1=cm, op=ALU.mult), [pVB])
            dv(lambda e: e.tensor_tensor(out=tq[1][:, :, 0:255], in0=Vre, in1=sn, op=ALU.mult), [pVB])
            dv(lambda e: e.tensor_tensor(out=Gm[1][:, :, 0:255], in0=tq[0][:, :, 0:255], in1=tq[1][:, :, 0:255], op=ALU.subtract))

        def st_SD(gp):
            db = gp % 2
            r8b = _bc(r8[:, gp:gp + 1], [128, 255])
            for ri in range(2):
                for b_ in range(2):
                    dv(lambda e, ri=ri, b_=b_: e.tensor_tensor_scan(out=Sc[ri][:, b_, 1:256], data0=r8b, data1=Gm[ri][:, b_, 0:255],
                                                                     initial=0.0, op0=ALU.mult, op1=ALU.add))
            cd = _bc(cosT[:, gp, :].unsqueeze(1), [128, 2, 256]); sd = _bc(sinT[:, gp, :].unsqueeze(1), [128, 2, 256])
            Hv = Hp[db][:].rearrange("p r (b j) -> p r b j", b=2)
            dv(lambda e: e.tensor_tensor(out=tq[0][:], in0=Sc[0][:], in1=cd, op=ALU.mult))
            dv(lambda e: e.tensor_tensor(out=tq[1][:], in0=Sc[1][:], in1=sd, op=ALU.mult))
            self.dve(lambda e: e.tensor_tensor(out=Hv[:, 0], in0=tq[0][:], in1=tq[1][:], op=ALU.subtract), [wB], [wB, HpB[db]])
            dv(lambda e: e.tensor_tensor(out=tq[0][:], in0=Sc[1][:], in1=cd, op=ALU.mult))
            dv(lambda e: e.tensor_tensor(out=tq[1][:], in0=Sc[0][:], in1=sd, op=ALU.mult))
            self.dve(lambda e: e.tensor_tensor(out=Hv[:, 1], in0=tq[0][:], in1=tq[1][:], op=ALU.add), [wB], [wB, HpB[db]])

        def st_Y(gp):
            db = gp % 2
            for par in range(2):
                g = 2 * gp + par
                gl = g % 8
                self.mm(pY[par][:], T0[:, g, :], ublk[db][par][:], True, False, [s5cB, ublkB[db][par]], [pYB[par]])
                self.mm(pY[par][:], Wy[:, g, 0, :], Hp[db][:, 0, :], False, False, [s5cB, HpB[db]], [pYB[par]])
                self.mm(pY[par][:], Wy[:, g, 1, :], Hp[db][:, 1, :], False, True, [s5cB, HpB[db]], [pYB[par]])
                self.dve(lambda e, par=par, g=g, gl=gl: e.scalar_tensor_tensor(
                    out=yblk[:, gl, :], in0=ublk[db][par][:], scalar=dtile[:, g:g + 1], in1=pY[par][:], op0=ALU.mult, op1=ALU.add),
                    [ublkB[db][par], pYB[par], s5cB], [yblkB[gl]])

        def st_O(c):
            xv = xv_of(c)
            for t_ in range(8):
                po = pO[t_ % 2]; poB = pOB[t_ % 2]
                for gl in range(8):
                    self.mm(po[:], SelO[:, gl, t_, :], yblk[:, gl, :], gl == 0, gl == 7, [selB, yblkB[gl]], [poB])
                self.act(xv[:, :, :, t_], po[:].rearrange("p (b j) -> p b j", b=2), AF.Gelu, [poB], [xsB[c]])

        st_R(0)
        st_V(0)
        for gp in range(16):
            st_M(gp)
            if gp + 1 < 16:
                st_R(gp + 1)
                st_V(gp + 1)
            st_SD(gp)
            st_Y(gp)
            if gp % 4 == 3:
                st_O(gp // 4)
        if dbg == "s5":
            S.dma("sp", dbg_t("gysT", [128, 4, T], BF16), xsT_all[:], xsB, [])
        S.emit()
        sm_.close(); s5p.close()
        if dbg == "s5":
            mixer.close()
            return

        a2 = ExitStack()
        wout = sb(a2, "wout", [128, 8, 1024], BF16); woutB = S.buf("wout")
        glub = sb(a2, "glub", [128, 4, 512], BF16); glubB = S.buf("glub")
        stage2 = [sb(a2, "stage2_%d" % i, [128, 1024], F32) for i in range(2)]; stage2B = S.bufs_n(2, "stage2")
        gout = sb(a2, "gout", [128, 8], F32); goutB = S.buf("gout")
        gb4 = sb(a2, "gb4", [128, 4], F32)
        gffn = sb(a2, "gffn", [128, D], F32); gffnB = S.buf("gffn")
        S.dma("sp", gout[:, 0:4], on_ssm.rearrange("o (kc p) -> p (o kc)", p=128), [], [goutB], **NC)
        S.dma("sp", gout[:, 4:8], on_sgu.rearrange("o (kc p) -> p (o kc)", p=128), [], [goutB], **NC)
        S.dma("sp", gb4[:], glu_b.rearrange("o (kc p) -> p (o kc)", p=128), [], [goutB], **NC)
        S.dma("act", gffn[:], bass.AP(ln_ffn_g.tensor, 0, [[0, 128], [1, D]]), [], [gffnB])
        self.load_weight(wout, w_out[0], 8, 1024, gout, stage2, stage2B, woutB, goutB)
        self.load_weight(glub, glu_w[0], 4, 512, None, stage2, stage2B, glubB, None)
        pG = ps(a2, "pG", [128, 512]); pGB = S.buf("pG")
        pSS = ps(a2, "pSS", [128, 512]); pSSB = S.buf("pSS")
        pA2 = [ps(a2, "pA2_%d" % i, [128, 1024]) for i in range(2)]; pA2B = S.bufs_n(2, "pA2")
        pT2 = ps(a2, "pT2", [128, 1024], BF16); pT2B = S.buf("pT2")
        sg = sb(a2, "sg", [128, 4, 128], F32); sgB = S.buf("sg")
        ysg = [sb(a2, "ysg%d" % i, [128, 4, 128], BF16) for i in range(2)]; ysgB = S.bufs_n(2, "ysg")
        ysq2 = sb(a2, "ysq2", [128, 4, 128], BF16); ysq2B = S.buf("ysq2")
        rs = [sb(a2, "rs%d" % i, [128, 4], F32) for i in range(2)]; rsB = S.bufs_n(2, "rs")
        xt2 = [sb(a2, "xt2_%d" % i, [128, D], F32) for i in range(2)]; xt2B = S.bufs_n(2, "xt2")
        h1t = [sb(a2, "h1t%d" % i, [128, D], F32) for i in range(3)]; h1B = S.bufs_n(3, "h1t")
        junk2 = sb(a2, "junk2", [128, D], BF16); junk2B = S.buf("junk2")
        hnb = [sb(a2, "hnb%d" % i, [128, D], BF16) for i in range(2)]; hnbB = S.bufs_n(2, "hnb")
        hnT = [sb(a2, "hnT%d" % i, [128, D], BF16) for i in range(3)]; hnTB = S.bufs_n(3, "hnT")
        def a2_front(i):
            b = i % 2
            cols = slice(i * 128, (i + 1) * 128)
            S.dma("sp", xt2[b][:], x[i * 128:(i + 1) * 128, :], [], [xt2B[b]])
            for c in range(4):
                for kc in range(4):
                    self.mm(pG[:, c * 128:(c + 1) * 128], glub[:, kc, c * 128:(c + 1) * 128], xsT_all[:, kc, cols],
                            kc == 0, kc == 3, [glubB, xsB[kc]], [pGB])
            for c in range(4):
                self.act(sg[:, c, :], pG[:, c * 128:(c + 1) * 128], AF.Sigmoid, [pGB, goutB], [sgB], bias=gb4[:, c:c + 1])
            self.dve(lambda e, b=b, cols=cols: e.tensor_tensor(out=ysg[b][:], in0=sg[:], in1=xsT_all[:, :, cols], op=ALU.mult),
                     [sgB] + xsB, [ysgB[b]])
            self.act(ysq2[:], ysg[b][:], AF.Square, [ysgB[b]], [ysq2B])
            for c in range(4):
                self.mm(pSS[:, 0:1], ysq2[:, c, :], onesB[:, 0:1], c == 0, c == 3, [ysq2B], [pSSB])
            self.dve(lambda e, b=b: e.tensor_copy(out=rs[b][:, 0:1], in_=pSS[:, 0:1]), [pSSB], [rsB[b]])
            self.dve(lambda e, b=b, i=i: e.tensor_copy(out=rs[b][:, 1:2], in_=ss_sgu[:, i:i + 1]), [ssgB], [rsB[b]])
            self.rstd(rs[b][:, 0:2], rs[b][:, 0:2], 1.0 / 512, [rsB[b]], [rsB[b]])

        def a2_back(i):
            b = i % 2
            c3 = i % 3
            cols = slice(i * 128, (i + 1) * 128)
            for half in range(2):
                hs = slice(half * 512, (half + 1) * 512)
                for kc in range(4):
                    self.mm(pA2[0][:, hs], ysg[b][:, kc, :], wout[:, kc, hs], kc == 0, kc == 3, [ysgB[b], woutB], [pA2B[0]])
            for half in range(2):
                hs = slice(half * 512, (half + 1) * 512)
                for kc in range(4):
                    self.mm(pA2[1][:, hs], ygT_all[:, kc, cols], wout[:, 4 + kc, hs], kc == 0, kc == 3, [ygB, woutB], [pA2B[1]])
            self.dve(lambda e, b=b: e.scalar_tensor_tensor(out=h1t[c3][:], in0=pA2[0][:], scalar=rs[b][:, 0:1], in1=xt2[b][:],
                                                           op0=ALU.mult, op1=ALU.add), [pA2B[0], rsB[b], xt2B[b]], [h1B[c3]])
            self.dve(lambda e, b=b: e.scalar_tensor_tensor(out=h1t[c3][:], in0=pA2[1][:], scalar=rs[b][:, 1:2], in1=h1t[c3][:],
                                                           op0=ALU.mult, op1=ALU.add), [pA2B[1], rsB[b], h1B[c3]], [h1B[c3]])
            self.dve(lambda e, b=b: e.scalar_tensor_tensor(out=junk2[:], in0=h1t[c3][:], scalar=1.0, in1=h1t[c3][:],
                                                           op0=ALU.mult, op1=ALU.mult, accum_out=rs[b][:, 2:3]),
                     [h1B[c3]], [junk2B, rsB[b]])
            self.rstd(rs[b][:, 3:4], rs[b][:, 2:3], 1.0 / D, [rsB[b]], [rsB[b]])
            self.dve(lambda e, b=b: e.scalar_tensor_tensor(out=hnb[b][:], in0=h1t[c3][:], scalar=rs[b][:, 3:4], in1=gffn[:],
                                                           op0=ALU.mult, op1=ALU.mult), [h1B[c3], rsB[b], gffnB], [hnbB[b]])

        def a2_back2(i):
            b = i % 2
            c3 = i % 3
            for kc in range(8):
                self.tr(pT2[:, kc * 128:(kc + 1) * 128], hnb[b][:, kc * 128:(kc + 1) * 128], identB[:], [hnbB[b]], [pT2B])
            self.act(hnT[c3][:], pT2[:], AF.Copy, [pT2B], [hnTB[c3]])
            if i >= 1:
                a2_store(i - 1)

        def a2_store(i):
            c3 = i % 3
            S.dma("act", h1_s[i * 128:(i + 1) * 128, :], h1t[c3][:], [h1B[c3]], [])
            S.dma("act", hnT_s[i], hnT[c3][:], [hnTB[c3]], [])
        a2_front(0)
        for i in range(NT):
            if i + 1 < NT:
                a2_front(i + 1)
            a2_back(i)
            if i >= 1:
                a2_back2(i - 1)
        a2_back2(NT - 1)
        a2_store(NT - 1)
        S.emit()
        a2.close(); mixer.close()
        if dbg == "a2":
            d1 = dbg_t("h1", [T, D])
            with nc.sbuf_tensor("dbgbuf", [128, D], F32) as dbuf:
                dB = S.buf("dbuf")
                for i in range(NT):
                    S.dma("sp", dbuf[:], h1_s[i * 128:(i + 1) * 128, :], [], [dB])
                    S.dma("sp", d1[i * 128:(i + 1) * 128, :], dbuf[:], [dB], [])
                S.emit()
            return

        rt_s = nc.dram_tensor("rt_s", [3, 128, T], F32, kind="Internal").ap()
        rp = ExitStack()
        wq = sb(rp, "wq", [128, 8, 2048], BF16); wqB = S.buf("wq")
        stq = [sb(rp, "stq%d" % i, [128, 2048], F32) for i in range(2)]; stqB = S.bufs_n(2, "stq")
        keysT = sb(rp, "keysT", [128, 16, 128], BF16); keysB = S.buf("keysT")
        kst = [sb(rp, "kst%d" % i, [128, 128], F32) for i in range(2)]; kstB = S.bufs_n(2, "kst")
        iota16 = sb(rp, "iota16", [128, 16], F32); iotaI = sb(rp, "iotaI", [128, 16], I32); ioB = S.buf("iota16")
        S.add("pool", lambda e: e.iota(iotaI[:], pattern=[[1, 16]], base=0, channel_multiplier=0), [], [ioB])
        S.add("pool", lambda e: e.tensor_copy(out=iota16[:], in_=iotaI[:]), [ioB], [ioB])
        self.load_weight(wq, peer_wq[0], 8, 2048, None, stq, stqB, wqB, None)
        pR = [ps(rp, "pR%d" % i, [128, 2048]) for i in range(2)]; pRB = S.bufs_n(2, "pR")
        for hk in range(16):
            S.dma(self.q(), kst[hk % 2][:], peer_keys[0, hk // 2, hk % 2], [], [kstB[hk % 2]])
            self.tr(pR[0][:, hk * 128:(hk + 1) * 128], kst[hk % 2][:], identF[:], [kstB[hk % 2]], [pRB[0]])
        self.dve(lambda e: e.tensor_copy(out=keysT[:].rearrange("p a n -> p (a n)"), in_=pR[0][:]), [pRB[0]], [keysB])
        hnp = [sb(rp, "hnp%d" % i, [128, 2, D], BF16) for i in range(2)]; hnpB = S.bufs_n(2, "hnp")
        qT = sb(rp, "qT", [128, 16, 256], BF16); qTB = S.buf("qT")
        def two(name, shape, dt=F32):
            return [sb(rp, "%s_%d" % (name, i), shape, dt) for i in range(2)]
        sc4 = [two("sc%d" % q_, [128, 16, 128]) for q_ in range(2)]; scB4 = [S.bufs_n(2, "sc%d_" % q_) for q_ in range(2)]
        scw2 = two("scw", [128, 16, 128])
        m82 = two("m8", [128, 16, 16]); ix2 = two("ix", [128, 16, 16], U32); ixf2 = two("ixf", [128, 16, 16])
        cand2 = two("cand", [128, 8, 256]); candw2 = two("candw", [128, 8, 256])
        c82 = two("c8", [128, 8, 16]); cix2 = two("cix", [128, 8, 16], U32)
        cia2 = two("cia", [128, 8, 16], U32); cib2 = two("cib", [128, 8, 16], U32)
        iaf2 = two("iaf", [128, 8, 16]); ibf2 = two("ibf", [128, 8, 16])
        oh2 = two("oh", [128, 8, 16, 16])
        res32 = two("res3", [128, 3, 8, 16])
        esum2 = two("esum", [128, 8])
        rtT = [sb(rp, "rtT%d" % i, [128, 3, 128], F32) for i in range(2)]; rtTB = S.bufs_n(2, "rtT")
        kB2 = S.bufs_n(2, "topk"); candB2 = S.bufs_n(2, "cand")
        tkB2 = [[S.bufs_n(5, "tk%d_%d_" % (j_, hk)) for hk in range(16)] for j_ in range(2)]
        tcB2 = [[S.bufs_n(5, "tc%d_%d_" % (j_, h)) for h in range(8)] for j_ in range(2)]
        s44 = [128, 8, 16, 16]

        def sub_tile(ps_, j):
            sc, scB, scw, m8, ix, ixf = sc4[ps_ % 2][j], scB4[ps_ % 2][j], scw2[j], m82[j], ix2[j], ixf2[j]
            cand, candw, c8, cix, cia, cib = cand2[j], candw2[j], c82[j], cix2[j], cia2[j], cib2[j]
            iaf, ibf, oh, res3, esum = iaf2[j], ibf2[j], oh2[j], res32[j], esum2[j]
            kB, candB, hB, cB_ = kB2[j], candB2[j], tkB2[j], tcB2[j]
            tile_i = 2 * ps_ + j
            for hk in range(16):
                self.dve(lambda e, hk=hk: e.max(out=m8[:, hk, 0:8], in_=sc[:, hk, :]), [scB], [hB[hk][0]])
                if hk % 4 == 3:
                    yield
            for hk in range(16):
                self.dve(lambda e, hk=hk: e.match_replace(out=scw[:, hk, :], in_to_replace=m8[:, hk, 0:8], in_values=sc[:, hk, :], imm_value=-1e30),
                         [scB, hB[hk][0]], [hB[hk][1]])
                if hk % 4 == 3:
                    yield
            for hk in range(16):
                self.dve(lambda e, hk=hk: e.max(out=m8[:, hk, 8:16], in_=scw[:, hk, :]), [hB[hk][1]], [hB[hk][2]])
                if hk % 4 == 3:
                    yield
            for hk in range(16):
                self.dve(lambda e, hk=hk: e.max_index(out=ix[:, hk, 0:8], in_max=m8[:, hk, 0:8], in_values=sc[:, hk, :]),
                         [scB, hB[hk][0]], [hB[hk][3]])
                if hk % 4 == 3:
                    yield
            for hk in range(16):
                self.dve(lambda e, hk=hk: e.max_index(out=ix[:, hk, 8:16], in_max=m8[:, hk, 8:16], in_values=sc[:, hk, :]),
                         [scB, hB[hk][2]], [hB[hk][4]])
                if hk % 4 == 3:
                    yield
            allh = [bb for hk in range(16) for bb in hB[hk]]
            self.dve(lambda e: e.tensor_copy(out=ixf[:], in_=ix[:]), allh, [kB])
            yield
            m8v = m8[:].rearrange("p (h k) n -> p h k n", k=2)
            ixv = ixf[:].rearrange("p (h k) n -> p h k n", k=2)
            self.dve(lambda e: e.tensor_tensor(out=cand[:].rearrange("p h (a b) -> p h a b", b=16),
                                               in0=_bc(m8v[:, :, 0, :].unsqueeze(3), s44), in1=_bc(m8v[:, :, 1, :].unsqueeze(2), s44), op=ALU.add),
                     allh, [candB])
            yield
            for h in range(8):
                self.dve(lambda e, h=h: e.max(out=c8[:, h, 0:8], in_=cand[:, h, :]), [candB], [cB_[h][0]])
                if h % 4 == 3:
                    yield
            for h in range(8):
                self.dve(lambda e, h=h: e.match_replace(out=candw[:, h, :], in_to_replace=c8[:, h, 0:8], in_values=cand[:, h, :], imm_value=-1e30),
                         [candB, cB_[h][0]], [cB_[h][1]])
                if h % 4 == 3:
                    yield
            for h in range(8):
                self.dve(lambda e, h=h: e.max(out=c8[:, h, 8:16], in_=candw[:, h, :]), [cB_[h][1]], [cB_[h][2]])
                if h % 4 == 3:
                    yield
            for h in range(8):
                self.dve(lambda e, h=h: e.max_index(out=cix[:, h, 0:8], in_max=c8[:, h, 0:8], in_values=cand[:, h, :]),
                         [candB, cB_[h][0]], [cB_[h][3]])
                if h % 4 == 3:
                    yield
            for h in range(8):
                self.dve(lambda e, h=h: e.max_index(out=cix[:, h, 8:16], in_max=c8[:, h, 8:16], in_values=cand[:, h, :]),
                         [candB, cB_[h][2]], [cB_[h][4]])
                if h % 4 == 3:
                    yield
            allc = [bb for h in range(8) for bb in cB_[h]]
            KB = [kB, scB] + allc

            def dk(fn):
                self.dve(fn, KB + [ioB], KB)
            g3 = res3[:, 2]
            dk(lambda e: e.tensor_tensor(out=g3, in0=c8[:], in1=_bc(c8[:, :, 0:1], [128, 8, 16]), op=ALU.subtract))
            yield
            self.act(g3, g3, AF.Exp, KB, KB)
            yield
            dk(lambda e: e.tensor_reduce(out=esum[:], in_=g3, axis=AX.X, op=ALU.add))
            yield
            dk(lambda e: e.reciprocal(out=esum[:], in_=esum[:]))
            yield
            dk(lambda e: e.tensor_tensor(out=g3, in0=g3, in1=_bc(esum[:].unsqueeze(2), [128, 8, 16]), op=ALU.mult))
            yield
            dk(lambda e: e.tensor_single_scalar(out=cia[:], in_=cix[:], scalar=4, op=ALU.logical_shift_right))
            yield
            dk(lambda e: e.tensor_single_scalar(out=cib[:], in_=cix[:], scalar=15, op=ALU.bitwise_and))
            yield
            dk(lambda e: e.tensor_copy(out=iaf[:], in_=cia[:]))
            yield
            dk(lambda e: e.tensor_copy(out=ibf[:], in_=cib[:]))
            yield
            io4 = _bc(iota16[:].unsqueeze(1).unsqueeze(1), s44)
            for w_, (sel_f, kk) in enumerate(((iaf, 0), (ibf, 1))):
                dk(lambda e, sel_f=sel_f: e.tensor_tensor(out=oh[:], in0=_bc(sel_f[:].unsqueeze(3), s44), in1=io4, op=ALU.is_equal))
                yield
                dk(lambda e, kk=kk: e.tensor_tensor(out=oh[:], in0=oh[:], in1=_bc(ixv[:, :, kk, :].unsqueeze(2), s44), op=ALU.mult))
                yield
                dk(lambda e, w_=w_: e.tensor_reduce(out=res3[:, w_], in_=oh[:], axis=AX.X, op=ALU.add))
                yield
            rb = tile_i % 2
            for w_ in range(3):
                self.tr(pR[j][:, w_ * 128:(w_ + 1) * 128], res3[:, w_].rearrange("p h k -> p (h k)"), identF[:], KB, [pRB[j]])
            self.act(rtT[rb][:].rearrange("p w t -> p (w t)"), pR[j][:, 0:384], AF.Copy, [pRB[j]], [rtTB[rb]])
            S.dma("sp", rt_s[:, :, tile_i * 128:(tile_i + 1) * 128].rearrange("w p t -> p w t"), rtT[rb][:], [rtTB[rb]], [])
            yield

        def r_front(ps_):
            b2 = ps_ % 2
            for j in range(2):
                S.dma("sp", hnp[b2][:, j, :], hnT_s[2 * ps_ + j], [], [hnpB[b2]])
            for j in range(2):
                for hk in range(16):
                    for kc in range(8):
                        self.mm(pR[j][:, hk * 128:(hk + 1) * 128], wq[:, kc, hk * 128:(hk + 1) * 128],
                                hnp[b2][:, j, kc * 128:(kc + 1) * 128], kc == 0, kc == 7, [wqB, hnpB[b2]], [pRB[j]])
            for j in range(2):
                self.act(qT[:, :, j * 128:(j + 1) * 128], pR[j][:].rearrange("p (a t) -> p a t", a=16), AF.Copy, [pRB[j]], [qTB])
            for j in range(2):
                for hk in range(16):
                    self.mm(pR[j][:, hk * 128:(hk + 1) * 128], qT[:, hk, j * 128:(j + 1) * 128], keysT[:, hk, :], True, True,
                            [qTB, keysB], [pRB[j]])
                self.act(sc4[b2][j][:].rearrange("p a n -> p (a n)"), pR[j][:], AF.Copy, [pRB[j]], [scB4[b2][j]])

        r_front(0)
        for ps_ in range(T // 256):
            if ps_ + 1 < T // 256:
                r_front(ps_ + 1)
            gens = [sub_tile(ps_, 0), sub_tile(ps_, 1)]
            live = [True, True]
            while any(live):
                for gi_ in range(2):
                    if live[gi_]:
                        try:
                            next(gens[gi_])
                        except StopIteration:
                            live[gi_] = False
        if dbg == "r":
            dd = dbg_t("rt", [3, 128, T])
            with nc.sbuf_tensor("dbgbuf2", [128, 3, T], F32) as dbuf:
                dB = S.buf("dbuf2")
                S.emit()
                S.dma("sp", dbuf[:], rt_s.rearrange("w p t -> p w t"), [], [dB])
                S.dma("sp", dd.rearrange("w p t -> p w t"), dbuf[:], [dB], [])
                S.emit()
            rp.close()
            return
        S.emit()
        rp.close()

        ep = ExitStack()
        wg = sb(ep, "wg", [128, 8, 1024], BF16); wgB = S.buf("wg")
        wpj = sb(ep, "wpj", [128, 2, 1024], BF16); wpjB = S.buf("wpj")
        ot_ = sb(ep, "ot", [128, D], F32); ot = [ot_, ot_]; otB_ = S.buf("ot"); otB = [otB_, otB_]
        stE = ot; stEB = otB
        gpl4 = sb(ep, "gpl4", [128, 8], F32); gplB = S.buf("gpl4")
        gfin = sb(ep, "gfin", [128, D], F32); gfinB = S.buf("gfin")
        iotaB = sb(ep, "iotaB", [128, 128], BF16); iotaI2 = sb(ep, "iotaI2", [128, 128], I32); io2B = S.buf("iotaB")
        S.add("pool", lambda e: e.iota(iotaI2[:], pattern=[[1, 128]], base=0, channel_multiplier=0), [], [io2B])
        S.add("pool", lambda e: e.tensor_copy(out=iotaB[:], in_=iotaI2[:]), [io2B], [io2B])
        S.dma("sp", gpl4[:], ple_norm_g.rearrange("o (kc p) -> p (o kc)", p=128), [], [gplB], **NC)
        S.dma("act", gfin[:], bass.AP(final_g.tensor, 0, [[0, 128], [1, D]]), [], [gfinB])
        self.load_weight(wg, ple_w_gate[0], 8, 1024, gpl4, stE, stEB, wgB, gplB)
        self.load_weight(wpj, ple_w_proj[0], 2, 1024, None, stE, stEB, wpjB, None)
        GTs = [sb(ep, "GT%d" % i, [128, 256, 128], BF16) for i in range(2)]; GTBs = S.bufs_n(2, "GT")
        rtp = [sb(ep, "rtp%d" % i, [128, 3, 256], F32) for i in range(2)]; rtpB = S.bufs_n(2, "rtp")
        hne_ = sb(ep, "hne", [128, 2, D], BF16); hne = [hne_, hne_]; hneB_ = S.buf("hne"); hneB = [hneB_, hneB_]
        NPQ = 8
        Pq = [sb(ep, "Pq%d" % i, [128, 128], BF16) for i in range(NPQ)]; PqB = S.bufs_n(NPQ, "Pq")
        Qq = [sb(ep, "Qq%d" % i, [128, 128], BF16) for i in range(NPQ)]; QqB = S.bufs_n(NPQ, "Qq")
        NST = 4
        ut = [sb(ep, "ut%d" % i, [128, D], BF16) for i in range(NST)]; utB = S.bufs_n(NST, "ut")
        vt = [sb(ep, "vt%d" % i, [128, D], BF16) for i in range(NST)]; vtB = S.bufs_n(NST, "vt")
        gs = [sb(ep, "gs%d" % i, [128, 256], BF16) for i in range(2)]; gsB = S.bufs_n(2, "gs")
        aT = [sb(ep, "aT%d" % i, [128, 256], BF16) for i in range(3)]; aTB = S.bufs_n(3, "aT")
        yacc = [ps(ep, "yacc%d" % i, [128, 1024]) for i in range(2)]; yaccB = S.bufs_n(2, "yacc")
        pWS = ps(ep, "pWS", [128, 1024]); pWSB = S.bufs_n(2, "pWS")
        pWG = ps(ep, "pWG", [128, 512]); pWGB = S.buf("pWG")
        pWC = ps(ep, "pWC", [128, 512]); pWCB = S.buf("pWC")
        pWCb = pWC[:].bitcast(BF16)
        pWD, pWDB, pWDb = pWC, pWCB, pWCb
        h1e = [sb(ep, "h1e%d" % i, [128, D], F32) for i in range(2)]; h1eB = S.bufs_n(2, "h1e")
        pte_ = sb(ep, "pte", [128, 256], F32); pte = [pte_, pte_]; pteB_ = S.buf("pte"); pteB = [pteB_, pteB_]
        rse = sb(ep, "rse", [128, 4], F32); rseB = S.buf("rse")
        h2n = sb(ep, "h2n", [128, D], BF16); h2nB = S.buf("h2n")
        h2nT = sb(ep, "h2nT", [128, D], BF16); h2nTB = S.buf("h2nT")
        pbf = sb(ep, "pbf", [128, 256], BF16); pbfB = S.buf("pbf")
        pTs = sb(ep, "pTs", [128, 256], BF16); pTsB = S.buf("pTs")
        gate = ot_; gateB = otB_
        junk3 = gate; junk3B = gateB
        def pass_loads(pq):
            bq = pq % 2
            S.dma("sp", rtp[bq][:], rt_s[:, :, pq * 256:(pq + 1) * 256].rearrange("w p t -> p w t"), [], [rtpB[bq]])

        def hne_load(pq):
            for j in range(2):
                S.dma("act", hne[0][:, j, :], hnT_s[2 * pq + j], [], [hneB[0]])

        def g_pq(pq, t4):
            bq = pq % 2
            for tt in range(4):
                t = 4 * t4 + tt
                r_ = t % NPQ
                self.dve(lambda e, r_=r_, t=t, bq=bq: e.tensor_scalar(out=Pq[r_][:], in0=iotaB[:], scalar1=rtp[bq][:, 0, t:t + 1],
                                                                     scalar2=rtp[bq][:, 2, t:t + 1], op0=ALU.is_equal, op1=ALU.mult),
                         [rtpB[bq], io2B], [PqB[r_]])
                self.dve(lambda e, r_=r_, t=t, bq=bq: e.tensor_scalar(out=Qq[r_][:], in0=iotaB[:], scalar1=rtp[bq][:, 1, t:t + 1],
                                                                     scalar2=None, op0=ALU.is_equal),
                         [rtpB[bq], io2B], [QqB[r_]])

        def g_mm(pq, t4):
            bq = pq % 2
            for tt in range(4):
                t = 4 * t4 + tt
                r_ = t % NPQ
                self.mm(pWG[:, tt * 128:(tt + 1) * 128], Qq[r_][:], Pq[r_][:], True, True,
                        [PqB[r_], QqB[r_]], [pWGB])
            self.act(GTs[bq][:, 4 * t4:4 * t4 + 4, :].rearrange("p t i -> p (t i)"), pWG[:], AF.Copy, [pWGB], [GTBs[bq]])

        def g_tokens(pq, t4):
            g_pq(pq, t4)
            g_mm(pq, t4)

        def epi(pq, sub):
            H = h1e[sub]; HB = h1eB[sub]
            ti = 2 * pq + sub
            rows_ = slice(ti * 128, (ti + 1) * 128)
            S.dma("act", pte[sub][:], p_in[rows_, :], [], [pteB[sub]])
            self.dve(lambda e: e.scalar_tensor_tensor(out=junk3[:], in0=H[:], scalar=1.0, in1=H[:], op0=ALU.mult, op1=ALU.mult,
                                                      accum_out=rse[:, 0:1]), [HB], [junk3B, rseB])
            yield
            self.rstd(rse[:, 1:2], rse[:, 0:1], 1.0 / D, [rseB], [rseB])
            yield
            self.act(h2n[:], H[:], AF.Copy, [HB, rseB], [h2nB], scale=rse[:, 1:2])
            yield
            for kc in range(8):
                self.tr(pWCb[:, kc * 128:(kc + 1) * 128], h2n[:, kc * 128:(kc + 1) * 128], identB[:], [h2nB], [pWCB])
            yield
            self.act(h2nT[:], pWCb[:, 0:1024], AF.Copy, [pWCB], [h2nTB])
            self.act(pbf[:], pte[sub][:], AF.Copy, [pteB[sub]], [pbfB])
            yield
            for kc in range(2):
                self.tr(pWDb[:, kc * 128:(kc + 1) * 128], pbf[:, kc * 128:(kc + 1) * 128], identB[:], [pbfB], [pWDB])
            yield
            self.dve(lambda e: e.tensor_copy(out=pTs[:], in_=pWDb[:, 0:256]), [pWDB], [pTsB])
            yield
            for half in range(2):
                hs = slice(half * 512, (half + 1) * 512)
                for kc in range(8):
                    self.mm(pWD[:], h2nT[:, kc * 128:(kc + 1) * 128], wg[:, kc, hs], kc == 0, kc == 7, [h2nTB, wgB], [pWDB])
                yield
                self.act(gate[:, hs], pWD[:], AF.Sigmoid, [pWDB], [gateB])
                yield
                for kc in range(2):
                    self.mm(pWC[:], pTs[:, kc * 128:(kc + 1) * 128], wpj[:, kc, hs], kc == 0, kc == 1, [pTsB, wpjB], [pWCB])
                yield
                self.dve(lambda e, hs=hs: e.tensor_tensor(out=gate[:, hs], in0=pWC[:], in1=gate[:, hs], op=ALU.mult), [pWCB, gateB], [gateB])
                yield
                self.dve(lambda e, hs=hs: e.tensor_tensor(out=H[:, hs], in0=H[:, hs], in1=gate[:, hs], op=ALU.add), [gateB, HB], [HB])
                yield
            self.dve(lambda e: e.scalar_tensor_tensor(out=junk3[:], in0=H[:], scalar=1.0, in1=H[:], op0=ALU.mult, op1=ALU.mult,
                                                      accum_out=rse[:, 2:3]), [HB], [junk3B, rseB])
            yield
            self.rstd(rse[:, 3:4], rse[:, 2:3], 1.0 / D, [rseB], [rseB])
            yield
            self.dve(lambda e: e.scalar_tensor_tensor(out=ot[sub][:], in0=H[:], scalar=rse[:, 3:4], in1=gfin[:],
                                                      op0=ALU.mult, op1=ALU.mult), [HB, rseB, gfinB], [otB[sub]])
            yield
            yield
            S.dma("sp", out[rows_, :], ot[sub][:], [otB[sub]], [])
            yield

        pend = []
        pass_loads(0)
        hne_load(0)
        for t4 in range(64):
            g_tokens(0, t4)
        for ps_ in range(T // 256):
            b2 = ps_ % 2
            GT = GTs[b2]; GTB = GTBs[b2]
            if ps_ + 1 < T // 256:
                pass_loads(ps_ + 1)
            def s_stage(blk):
                k = blk % NST
                S.dma("sp", ut[k][:], UT_s[blk], [], [utB[k]])
                S.dma("act", vt[k][:], V_s[blk], [], [vtB[k]])
                sb_ = blk % 2
                psx = pWS[:, sb_ * 512:sb_ * 512 + 256]
                for kc in range(8):
                    self.mm(psx.rearrange("p (j t) -> p j t", j=2), ut[k][:, kc * 128:(kc + 1) * 128], hne[b2][:, :, kc * 128:(kc + 1) * 128],
                            kc == 0, kc == 7, [utB[k], hneB[b2]], [pWSB[sb_]])
                self.act(gs[sb_][:], psx, AF.Gelu, [pWSB[sb_]], [gsB[sb_]])
                a3 = blk % 3
                self.dve(lambda e, sb_=sb_, a3=a3, blk=blk, GT=GT: e.tensor_tensor(out=aT[a3][:], in0=gs[sb_][:], in1=GT[:, :, blk], op=ALU.mult),
                         [gsB[sb_], GTB], [aTB[a3]])

            def v_stage(blk):
                k = blk % NST
                sb_ = blk % 2
                for sub in range(2):
                    for half in range(2):
                        self.mm(yacc[sub][:, half * 512:(half + 1) * 512], aT[blk % 3][:, sub * 128:(sub + 1) * 128],
                                vt[k][:, half * 512:(half + 1) * 512], blk == 0, blk == 127, [aTB[blk % 3], vtB[k]], [yaccB[sub]])

            nxt = ps_ + 1 < T // 256
            s_stage(0)
            if nxt:
                g_pq(ps_ + 1, 0)
            for blk in range(129):
                if blk >= 2 and blk % 2 == 0 and pend:
                    try:
                        next(pend[0])
                    except StopIteration:
                        pend.pop(0)
                if blk == 100:
                    for g_ in pend:
                        for _ in g_:
                            pass
                    pend = []
                    for sub in range(2):
                        ti_ = 2 * ps_ + sub
                        S.dma("sp", h1e[sub][:], h1_s[ti_ * 128:(ti_ + 1) * 128, :], [], [h1eB[sub]])
                if nxt and blk % 2 == 0 and blk // 2 + 1 < 64:
                    g_pq(ps_ + 1, blk // 2 + 1)
                if blk + 1 < 128:
                    s_stage(blk + 1)
                if blk == 126 and nxt:
                    hne_load(ps_ + 1)
                if blk >= 1:
                    v_stage(blk - 1)
                if nxt and blk % 2 == 1 and blk < 128:
                    g_mm(ps_ + 1, blk // 2)
            for sub in range(2):
                self.dve(lambda e, sub=sub: e.tensor_tensor(out=h1e[sub][:], in0=yacc[sub][:], in1=h1e[sub][:], op=ALU.add),
                         [yaccB[sub], h1eB[sub]], [h1eB[sub]])
            pend = [epi(ps_, 0), epi(ps_, 1)]
        for g_ in pend:
            for _ in g_:
                pass
        S.emit()
        ep.close()


_W_NAMES = ["ln_mix_g", "w_in", "s5_a_re", "s5_a_im", "s5_log_dt", "s5_b_re", "s5_b_im", "s5_c_re", "s5_c_im", "s5_d",
            "glu_w", "glu_b", "sgu_ln_g", "sgu_ln_b", "sgu_ws", "sgu_b", "out_norm_ssm", "out_norm_sgu", "w_out",
            "ln_ffn_g", "peer_wq", "peer_keys", "peer_u", "peer_v", "ple_norm_g", "ple_w_gate", "ple_w_proj", "final_g"]


def _run(inputs, dbg=None, cores=NCORES):
    k = K(dbg)
    k.build()
    w = {n: np.ascontiguousarray(np.asarray(inputs[n], dtype=np.float32)) for n in _W_NAMES}
    x = np.asarray(inputs["x"], dtype=np.float32).reshape(16 * SEQ, D)
    p = np.asarray(inputs["p"], dtype=np.float32).reshape(16 * SEQ, 256)
    in_maps = []
    for c in range(cores):
        m = {n: w[n] for n in k.din if n in w}
        m["x"] = np.ascontiguousarray(x[c * T:(c + 1) * T])
        m["p"] = np.ascontiguousarray(p[c * T:(c + 1) * T])
        in_maps.append({n: m[n] for n in k.din})
    res = run_bass_kernel_spmd(k.nc, in_maps, core_ids=list(range(cores)))
    return res


def kernel(**inputs):
    res = _run(inputs)
    outs = [np.asarray(r["out"], dtype=np.float32).reshape(NB, SEQ, D) for r in res.results]
    return np.concatenate(outs, axis=0)
```
